# Optimizing a Trainium2 kernel written in Bass

```python
import math
import jax, jax.numpy as jnp
from jax import lax
import numpy as np

D_MODEL = 1024
BATCH = 16
SEQ = 2048
DEPTH = 1

CHUNK = 64
Q_BLOCK = 128
HEAD_DIM_A = 64
N_HEADS_A = D_MODEL // (2 * HEAD_DIM_A)
ATTN_WIDTH = N_HEADS_A * 2 * HEAD_DIM_A
ROPE_THETA = 10000.0
POOL_WINDOWS = (2, 4, 8, 16)
POOL_GROUPS = len(POOL_WINDOWS)
POOL_WIDTH = D_MODEL // 2
POOL_GROUP_DIM = POOL_WIDTH // POOL_GROUPS
N_BRANCHES = 2
IN_WIDTH = 3 * ATTN_WIDTH + POOL_WIDTH + N_BRANCHES * D_MODEL
SPLIT_POINTS = (ATTN_WIDTH, 2 * ATTN_WIDTH, 3 * ATTN_WIDTH, 3 * ATTN_WIDTH + POOL_WIDTH)
N_GROUPS = 4
EXPERTS_PER_GROUP = 8
N_EXPERTS = N_GROUPS * EXPERTS_PER_GROUP
TOP_K = 2
EXPERT_FF = D_MODEL // 2
MOE_BLOCK = 256
LN_EPS = 1e-5
RMS_EPS = 1e-5
ALPHA = (2.0 * DEPTH) ** 0.25
BETA = (8.0 * DEPTH) ** -0.25
NEG_INF = -1e30

kernel_name = 'hybrid_diffattn_pool_hmoe_deepnorm'


def _layer_norm(x, gain, bias):
    xf = x.astype(jnp.float32)
    mu = jnp.mean(xf, axis=-1, keepdims=True)
    var = jnp.mean(jnp.square(xf - mu), axis=-1, keepdims=True)
    y = (xf - mu) * lax.rsqrt(var + LN_EPS)
    return (y * gain.astype(jnp.float32) + bias.astype(jnp.float32)).astype(x.dtype)


def _rope(t, positions):
    half = HEAD_DIM_A // 2
    inv_freq = ROPE_THETA ** (-jnp.arange(half, dtype=jnp.float32) * (2.0 / HEAD_DIM_A))
    ang = positions.astype(jnp.float32)[..., None] * inv_freq
    cos = jnp.cos(ang)[:, :, None, None, :].astype(t.dtype)
    sin = jnp.sin(ang)[:, :, None, None, :].astype(t.dtype)
    t1, t2 = t[..., :half], t[..., half:]
    return jnp.concatenate([t1 * cos - t2 * sin, t2 * cos + t1 * sin], axis=-1)


def _diff_attention(q, k, v, lam):
    B, S = q.shape[0], q.shape[1]
    nb = S // Q_BLOCK
    scale = HEAD_DIM_A ** -0.5
    q_blocks = q.reshape(B, nb, Q_BLOCK, N_HEADS_A, 2, HEAD_DIM_A).transpose(1, 0, 3, 4, 2, 5)
    k_t = k.transpose(0, 2, 3, 1, 4)
    v_t = v.transpose(0, 2, 1, 3)
    key_chunk = jnp.arange(S) // CHUNK

    def one_block(args):
        q_blk, blk = args
        s = jnp.einsum('bhmqd,bhmkd->bhmqk', q_blk, k_t).astype(jnp.float32) * scale
        q_chunk = (blk * Q_BLOCK + jnp.arange(Q_BLOCK)) // CHUNK
        allowed = key_chunk[None, :] <= q_chunk[:, None]
        p = jax.nn.softmax(jnp.where(allowed, s, NEG_INF), axis=-1)
        a = p[:, :, 0] - lam * p[:, :, 1]
        return jnp.einsum('bhqk,bhkv->bhqv', a.astype(v_t.dtype), v_t)

    o = lax.map(one_block, (q_blocks, jnp.arange(nb)))
    return o.transpose(1, 0, 3, 2, 4).reshape(B, S, N_HEADS_A, 2 * HEAD_DIM_A)


def _multiscale_pool(u, w_pool, pool_scale):
    B, S = u.shape[0], u.shape[1]
    groups = u.astype(jnp.float32).reshape(B, S, POOL_GROUPS, POOL_GROUP_DIM)
    cs = jnp.cumsum(groups, axis=1)
    t = jnp.arange(S)
    means = []
    for g, w in enumerate(POOL_WINDOWS):
        c = cs[:, :, g]
        lagged = jnp.pad(c, ((0, 0), (w, 0), (0, 0)))[:, :S]
        count = jnp.minimum(t + 1, w).astype(jnp.float32)[None, :, None]
        means.append((c - lagged) / count)
    pooled = (jnp.stack(means, axis=2) - groups).astype(u.dtype)
    mixed = jnp.einsum('bsgc,gcd->bsgd', pooled, w_pool)
    return mixed.reshape(B, S, POOL_WIDTH) * pool_scale


def _token_mixer(h, positions, w_in, gate_bias, lam_vecs, subln_gain, lambda_init,
                 w_pool, pool_scale, w_branch_a, w_branch_b, w_out):
    B, S = h.shape[0], h.shape[1]
    proj = h @ w_in
    q, k, v, u, gates = jnp.split(proj, SPLIT_POINTS, axis=-1)
    q = _rope(q.reshape(B, S, N_HEADS_A, 2, HEAD_DIM_A), positions)
    k = _rope(k.reshape(B, S, N_HEADS_A, 2, HEAD_DIM_A), positions)
    v = v.reshape(B, S, N_HEADS_A, 2 * HEAD_DIM_A)
    lv = lam_vecs.astype(jnp.float32)
    lam = jnp.exp(jnp.sum(lv[0] * lv[1])) - jnp.exp(jnp.sum(lv[2] * lv[3])) + lambda_init
    o = _diff_attention(q, k, v, lam).astype(jnp.float32)
    o = o * lax.rsqrt(jnp.mean(jnp.square(o), axis=-1, keepdims=True) + RMS_EPS)
    o = o * subln_gain.astype(jnp.float32) * (1.0 - lambda_init)
    y_a = o.astype(h.dtype).reshape(B, S, ATTN_WIDTH) @ w_branch_a
    y_b = _multiscale_pool(u, w_pool, pool_scale) @ w_branch_b
    g = jax.nn.sigmoid((gates + gate_bias).astype(jnp.float32)).reshape(B, S, N_BRANCHES, D_MODEL)
    merged = g[:, :, 0] * y_a.astype(jnp.float32) + g[:, :, 1] * y_b.astype(jnp.float32)
    return merged.astype(h.dtype) @ w_out


def _hierarchical_moe(h, w_router_group, b_router_group, w_router_expert, b_router_expert,
                      w_gate_up, w_down):
    B, S, D = h.shape
    T = B * S
    x2 = h.reshape(T, D)
    g_prob = jax.nn.softmax((x2 @ w_router_group + b_router_group).astype(jnp.float32), axis=-1)
    g_p, g_idx = lax.top_k(g_prob, 1)
    e_logit = (x2 @ w_router_expert + b_router_expert).astype(jnp.float32)
    e_logit = e_logit.reshape(T, N_GROUPS, EXPERTS_PER_GROUP)
    e_logit = jnp.take_along_axis(e_logit, g_idx[:, :, None], axis=1)[:, 0]
    e_p, e_local = lax.top_k(jax.nn.softmax(e_logit, axis=-1), TOP_K)
    e_p = e_p / jnp.sum(e_p, axis=-1, keepdims=True)
    weights = g_p * e_p
    experts = g_idx * EXPERTS_PER_GROUP + e_local

    A = T * TOP_K
    n_blocks = -(-A // MOE_BLOCK) + N_EXPERTS
    P = n_blocks * MOE_BLOCK
    flat_e = experts.reshape(A)
    flat_w = weights.reshape(A)
    flat_tok = jnp.repeat(jnp.arange(T, dtype=jnp.int32), TOP_K)
    order = jnp.argsort(flat_e)
    sorted_e = flat_e[order]
    counts = jnp.bincount(flat_e, length=N_EXPERTS)
    padded = ((counts + MOE_BLOCK - 1) // MOE_BLOCK) * MOE_BLOCK
    ends = jnp.cumsum(padded)
    start_padded = ends - padded
    start = jnp.cumsum(counts) - counts
    dest = start_padded[sorted_e] + (jnp.arange(A) - start[sorted_e])
    row_tok = jnp.full((P,), T, dtype=jnp.int32).at[dest].set(flat_tok[order])
    row_w = jnp.zeros((P,), jnp.float32).at[dest].set(flat_w[order])
    block_expert = jnp.minimum(
        jnp.searchsorted(ends, jnp.arange(n_blocks) * MOE_BLOCK, side='right'), N_EXPERTS - 1)
    x_pad = jnp.concatenate([x2, jnp.zeros((1, D), x2.dtype)], axis=0)
    xb = x_pad[row_tok].reshape(n_blocks, MOE_BLOCK, D)

    def run_block(args):
        xs, e = args
        gate, up = jnp.split(xs @ w_gate_up[e], 2, axis=-1)
        return (jax.nn.silu(gate) * up) @ w_down[e]

    yb = lax.map(run_block, (xb, block_expert)).reshape(P, D)
    out = jax.ops.segment_sum(yb.astype(jnp.float32) * row_w[:, None], row_tok, num_segments=T + 1)
    return out[:T].astype(h.dtype).reshape(B, S, D)


def setup_inputs(seed: int = 0) -> dict:
    key = jax.random.key(seed)
    ks = jax.random.split(key, 24)
    nrm = lambda k, shape, s: jax.random.normal(k, shape, jnp.float32) * s
    L, D = DEPTH, D_MODEL
    x = jax.random.normal(ks[0], (BATCH, SEQ, D), jnp.float32)
    offset = jax.random.randint(ks[1], (BATCH, 1), 0, 4096, dtype=jnp.int32)
    positions = offset + jnp.arange(SEQ, dtype=jnp.int32)[None, :]
    w_q = nrm(ks[2], (L, D, ATTN_WIDTH), D ** -0.5)
    w_k = nrm(ks[3], (L, D, ATTN_WIDTH), D ** -0.5)
    w_v = nrm(ks[4], (L, D, ATTN_WIDTH), BETA * D ** -0.5)
    w_u = nrm(ks[5], (L, D, POOL_WIDTH), D ** -0.5)
    w_g = nrm(ks[6], (L, D, N_BRANCHES * D), D ** -0.5)
    w_in = jnp.concatenate([w_q, w_k, w_v, w_u, w_g], axis=-1)
    return {
        'x': x,
        'positions': positions,
        'w_in': w_in,
        'gate_bias': nrm(ks[7], (L, N_BRANCHES * D), 0.01),
        'lam_vecs': nrm(ks[8], (L, 4, HEAD_DIM_A), 0.1),
        'subln_gain': 1.0 + nrm(ks[9], (L, 2 * HEAD_DIM_A), 0.02),
        'w_pool': nrm(ks[10], (L, POOL_GROUPS, POOL_GROUP_DIM, POOL_GROUP_DIM), POOL_GROUP_DIM ** -0.5),
        'pool_scale': 1.0 + nrm(ks[11], (L, POOL_WIDTH), 0.02),
        'w_branch_a': nrm(ks[12], (L, ATTN_WIDTH, D), BETA * ATTN_WIDTH ** -0.5),
        'w_branch_b': nrm(ks[13], (L, POOL_WIDTH, D), BETA * POOL_WIDTH ** -0.5),
        'w_out': nrm(ks[14], (L, D, D), BETA * D ** -0.5),
        'ln1_gain': 1.0 + nrm(ks[15], (L, D), 0.02),
        'ln1_bias': nrm(ks[16], (L, D), 0.01),
        'w_router_group': nrm(ks[17], (L, D, N_GROUPS), D ** -0.5),
        'b_router_group': nrm(ks[18], (L, N_GROUPS), 0.01),
        'w_router_expert': nrm(ks[19], (L, D, N_EXPERTS), D ** -0.5),
        'b_router_expert': nrm(ks[20], (L, N_EXPERTS), 0.01),
        'w_gate_up': nrm(ks[21], (L, N_EXPERTS, D, 2 * EXPERT_FF), D ** -0.5),
        'w_down': nrm(ks[22], (L, N_EXPERTS, EXPERT_FF, D), BETA * EXPERT_FF ** -0.5),
        'ln2_gain': 1.0 + nrm(ks[23], (L, D), 0.02),
        'ln2_bias': nrm(jax.random.fold_in(ks[23], 1), (L, D), 0.01),
    }


def reference(x, positions, w_in, gate_bias, lam_vecs, subln_gain, w_pool, pool_scale,
              w_branch_a, w_branch_b, w_out, ln1_gain, ln1_bias, w_router_group,
              b_router_group, w_router_expert, b_router_expert, w_gate_up, w_down,
              ln2_gain, ln2_bias):
    for layer in range(DEPTH):
        lambda_init = 0.8 - 0.6 * math.exp(-0.3 * layer)
        mix = _token_mixer(x, positions, w_in[layer], gate_bias[layer], lam_vecs[layer],
                           subln_gain[layer], lambda_init, w_pool[layer], pool_scale[layer],
                           w_branch_a[layer], w_branch_b[layer], w_out[layer])
        x = _layer_norm(ALPHA * x + mix, ln1_gain[layer], ln1_bias[layer])
        ffn = _hierarchical_moe(x, w_router_group[layer], b_router_group[layer],
                                w_router_expert[layer], b_router_expert[layer],
                                w_gate_up[layer], w_down[layer])
        x = _layer_norm(ALPHA * x + ffn, ln2_gain[layer], ln2_bias[layer])
    return x
```

```python
import contextlib
import math
import numpy as np
import concourse.bass as bass
import concourse.mybir as mybir
from concourse.bass_utils import run_bass_kernel_spmd

F32 = mybir.dt.float32
BF16 = mybir.dt.bfloat16
I32 = mybir.dt.int32
AF = mybir.ActivationFunctionType
ALU = mybir.AluOpType
AX = mybir.AxisListType

NCORES = 8
NSEQ = 2
S = 2048
D = 1024
H = 8
NT = S // 128
NE = 32
CAP = 384
NBLK = CAP // 128
NSLOT = NE * CAP + 128
DUMP = NE * CAP
ALPHA = 2.0 ** 0.25
LAMBDA_INIT = 0.2
LN_EPS = 1e-5
RMS_EPS = 1e-5
IN_W = 5632
TWO_PI = 2.0 * math.pi
C1 = 6.28125
C2 = TWO_PI - C1

SEM_LIMIT = 30000
DMA_RING = 6
DMA_RING_Q = {"poolc": 2}
CONV_EVERY = 13


class Prog:
    def __init__(self, nc, stack):
        self.nc = nc
        self.stack = stack
        self.eng = {"pe": nc.tensor, "dve": nc.vector, "act": nc.scalar,
                    "pool": nc.gpsimd, "sp": nc.sync, "poolc": nc.gpsimd}
        self.cnt = {}
        self.sems = {}
        self.seen = {}
        self.res = {}
        self.dma_n = {}
        self.dma_ring = {}
        self.dma_ring_cnt = {}
        self.ninst = 0
        self.recording = None
        for s in ["pe", "dve", "act", "pool"]:
            self.cnt[s] = 0
            self.sems[s] = []
            self._new_sem(s)

    def _alloc_sem(self, name):
        return self.stack.enter_context(self.nc.semaphore(name))

    def _new_sem(self, s):
        sem = self._alloc_sem("s_%s_%d" % (s, len(self.sems[s])))
        self.sems[s].append((sem, self.cnt[s]))

    def _wait(self, engname, tok):
        stream, idx = tok
        key = (engname, stream)
        if self.seen.get(key, 0) >= idx:
            return
        if stream.startswith("dma"):
            self.seen[key] = idx
            q, r = stream.split(":")[1:]
            self.eng[engname].wait_ge(self.dma_ring[q][int(r)], 16 * idx)
            self.ninst += 1
            return
        if stream == engname and engname == "pe":
            return
        self.seen[key] = idx
        for sem, base in reversed(self.sems[stream]):
            if idx > base:
                self.eng[engname].wait_ge(sem, idx - base)
                self.ninst += 1
                return
        raise RuntimeError("bad token")

    def _deps(self, reads, writes):
        deps = []
        for r in reads:
            st = self.res.get(r)
            if st and st[0] is not None:
                deps.append(st[0])
        for w in writes:
            st = self.res.get(w)
            if st:
                if st[0] is not None:
                    deps.append(st[0])
                deps.extend(st[1])
        return deps

    def _commit(self, tok, reads, writes):
        for r in reads:
            st = self.res.setdefault(r, [None, []])
            st[1].append(tok)
        for w in writes:
            self.res[w] = [tok, []]

    def _excl(self, reads, writes):
        r2 = [r for r in reads if not (isinstance(r, str) and r.startswith("B") and r[1:].isdigit())]
        w2 = list(writes) + [r for r in reads if (isinstance(r, str) and r.startswith("B") and r[1:].isdigit())]
        return r2, w2

    def op(self, engname, fn, reads=(), writes=()):
        if self.recording is not None:
            rec = _Rec()
            fn(rec)
            self.recording.append(("op", engname, rec.call, tuple(reads), tuple(writes)))
            return None
        reads, writes = self._excl(reads, writes)
        for d in self._deps(reads, writes):
            self._wait(engname, d)
        inst = fn(self.eng[engname])
        if self.cnt[engname] - self.sems[engname][-1][1] >= SEM_LIMIT:
            self._new_sem(engname)
        sem, base = self.sems[engname][-1]
        inst.then_inc(sem, 1)
        self.cnt[engname] += 1
        self.ninst += 1
        tok = (engname, self.cnt[engname])
        self._commit(tok, reads, writes)
        return tok

    def dma(self, q, fn, reads=(), writes=()):
        if self.recording is not None:
            rec = _Rec()
            fn(rec)
            self.recording.append(("dma", q, rec.call, tuple(reads), tuple(writes)))
            return None
        nring = DMA_RING_Q.get(q, DMA_RING)
        if q not in self.dma_ring:
            self.dma_ring[q] = [self._alloc_sem("d_%s_%d" % (q, i)) for i in range(nring)]
            self.dma_ring_cnt[q] = [0] * nring
            self.dma_n[q] = 0
        r = self.dma_n[q] % nring
        self.dma_n[q] += 1
        stream = "dma:%s:%d" % (q, r)
        if self.dma_ring_cnt[q][r] > 0:
            self._wait(q, (stream, self.dma_ring_cnt[q][r]))
        for d in self._deps(reads, writes):
            self._wait(q, d)
        inst = fn(self.eng[q])
        inst.then_inc(self.dma_ring[q][r], 16)
        self.dma_ring_cnt[q][r] += 1
        self.ninst += 1
        tok = (stream, self.dma_ring_cnt[q][r])
        self._commit(tok, reads, writes)
        return tok

    def record(self, fn, *args):
        assert self.recording is None
        self.recording = []
        try:
            fn(*args)
            return self.recording
        finally:
            self.recording = None

    def replay(self, item):
        kind, eng, call, reads, writes = item
        name, a, kw = call
        f = lambda e: getattr(e, name)(*a, **kw)
        if kind == "op":
            return self.op(eng, f, reads, writes)
        return self.dma(eng, f, reads, writes)

    def barrier(self):
        toks = []
        for s in ["pe", "dve", "act", "pool"]:
            if self.cnt[s] > 0:
                toks.append((s, self.cnt[s]))
        for q in self.dma_ring:
            for r in range(len(self.dma_ring[q])):
                if self.dma_ring_cnt[q][r] > 0:
                    toks.append(("dma:%s:%d" % (q, r), self.dma_ring_cnt[q][r]))
        for e in ["pe", "dve", "act", "pool", "sp"]:
            for t in toks:
                if e == "pe" and t[0] == "pe":
                    continue
                self._wait(e, t)
        self.res = {}


class _Rec:
    def __init__(self):
        self.call = None

    def __getattr__(self, name):
        def f(*a, **kw):
            self.call = (name, a, kw)
            return self
        return f


def _consts():
    ident = np.eye(128, dtype=np.float32)
    prot = np.zeros((128, 128), np.float32)
    for pp in range(128):
        if (pp % 64) < 32:
            prot[pp + 32, pp] = -1.0
        else:
            prot[pp - 32, pp] = 1.0
    tri = np.triu(np.ones((128, 128), np.float32), 1)
    ones = np.ones((128, 128), np.float32)
    cmat = np.stack([ident, prot, tri, ones], axis=1)
    half = 32
    inv_freq = (10000.0 ** (-np.arange(half, dtype=np.float32) * np.float32(2.0 / 64))).astype(np.float32)
    cvec = np.zeros((128, 128), np.float32)
    cvec[:, 0] = inv_freq[np.arange(128) % 32]
    for g, w in enumerate((2, 4, 8, 16)):
        for t in range(16):
            cvec[:, 16 + g * 16 + t] = (w / (t + 1.0)) if t < w - 1 else 1.0
    cvec[:, 80:112] = (np.arange(NE, dtype=np.float32) * CAP - DUMP)[None, :]
    cvec[64:, 112] = -30000.0
    return np.ascontiguousarray(cmat), cvec


def build_nc(stop=None):
    nc = bass.Bass("TRN2", target_bir_lowering=False)

    def dram(name, shape, dt, kind="ExternalInput"):
        return nc.dram_tensor(name, shape, dt, kind=kind).ap()

    x = dram("x", [NSEQ, S, D], F32)
    pos = dram("pos", [NSEQ, S], I32)
    w_in = dram("w_in", [D, IN_W], F32)
    gate_bias = dram("gate_bias", [128, 16], F32)
    lam_vecs = dram("lam_vecs", [1, 256], F32)
    subln_gain = dram("subln_gain", [1, 128], F32)
    w_pool = dram("w_pool", [4, 128, 128], F32)
    pool_scale = dram("pool_scale", [128, 4], F32)
    w_a = dram("w_a", [D, D], F32)
    w_b = dram("w_b", [512, D], F32)
    w_out = dram("w_out", [D, D], F32)
    ln1_g = dram("ln1_g", [1, D], F32)
    ln1_b = dram("ln1_b", [1, D], F32)
    w_rt = dram("w_rt", [D, 36], F32)
    b_rt = dram("b_rt", [1, 36], F32)
    w_gu = dram("w_gu", [NE, D, D], F32)
    w_dn = dram("w_dn", [NE, 512, D], F32)
    ln2_g = dram("ln2_g", [1, D], F32)
    ln2_b = dram("ln2_b", [1, D], F32)
    cmat = dram("cmat", [128, 4, 128], F32)
    cvec = dram("cvec", [128, 128], F32)
    out = dram("out", [NSEQ, S, D], F32, kind="ExternalOutput")
    h_scr = dram("h_scr", [NSEQ * S, D], F32, kind="Internal")
    xs = dram("xs", [NSLOT, D], BF16, kind="Internal")
    ys = dram("ys", [NSLOT, D], F32, kind="Internal")
    wgu_bf = dram("wgu_bf", [NE, D, D], BF16, kind="Internal")
    wdn_bf = dram("wdn_bf", [NE, 512, D], BF16, kind="Internal")
    dbg = {}
    if stop in ("attn", "a1", "rope", "v", "qk"):
        dbg["oT"] = dram("dbg_oT", [NSEQ, 128, H, S], BF16, kind="ExternalOutput")
    if stop == "mix":
        dbg["mergedT"] = dram("dbg_mergedT", [NSEQ, 128, 8, S], BF16, kind="ExternalOutput")
    if stop in ("ln1",):
        dbg["h"] = dram("dbg_h", [NSEQ * S, D], F32, kind="ExternalOutput")
    if stop in ("ln1", "moe"):
        dbg["slots"] = dram("dbg_slots", [128, 32, 2], I32, kind="ExternalOutput")
        dbg["wts"] = dram("dbg_wts", [128, 32, 2], F32, kind="ExternalOutput")
    if stop == "moe":
        dbg["ys"] = dram("dbg_ys", [NSLOT, D], F32, kind="ExternalOutput")

    w_in_k = w_in.rearrange("(kc k) n -> k kc n", k=128)
    w_a_k = w_a.rearrange("(kc k) n -> k kc n", k=128)
    w_b_k = w_b.rearrange("(kc k) n -> k kc n", k=128)
    w_out_k = w_out.rearrange("(kc k) n -> k kc n", k=128)
    w_rt_k = w_rt.rearrange("(kc k) n -> k kc n", k=128)

    with contextlib.ExitStack() as st:
        P = Prog(nc, st)

        uniq = [0]

        def sb(stack, name, shape, dt):
            uniq[0] += 1
            return stack.enter_context(nc.sbuf_tensor("%s_u%d" % (name, uniq[0]), shape, dt))

        B = [st.enter_context(nc.psum_tensor("bank%d" % i, [128, 512], F32)) for i in range(8)]
        BN = ["B%d" % i for i in range(8)]

        def bank_bf(i):
            return B[i][:].bitcast(BF16)

        cm_f = sb(st, "cm_f", [128, 4, 128], F32)
        cm_b = sb(st, "cm_b", [128, 4, 128], BF16)
        cv = sb(st, "cv", [128, 128], F32)
        lvb = sb(st, "lvb", [128, 256], F32)
        gainb = sb(st, "gainb", [128, 128], F32)
        gbias = sb(st, "gbias", [128, 16], F32)
        pscale = sb(st, "pscale", [128, 4], F32)
        wpool_b = sb(st, "wpool_b", [128, 4, 128], BF16)
        wrt = sb(st, "wrt", [128, 8, 36], F32)
        brt = sb(st, "brt", [128, 36], F32)
        small = sb(st, "small", [128, 16], F32)
        negh = sb(st, "negh", [128, 4], F32)
        cnt = sb(st, "cnt", [128, 32], F32)
        slots_f = sb(st, "slots_f", [128, 32, 2], F32)
        slots_i = sb(st, "slots_i", [128, 32, 2], I32)
        wts = sb(st, "wts", [128, 32, 2], F32)
        zrow = sb(st, "zrow", [128, 1024], F32)
        zb3 = sb(st, "zb3", [128, 1, 1024], BF16)
        xT = sb(st, "xT", [128, 8, S], BF16)
        oT = sb(st, "oT", [128, 8, S], BF16)

        ident_f = cm_f[:, 0, :]
        ident_b = cm_b[:, 0, :]
        prot_b = cm_b[:, 1, :]
        tri_b = cm_b[:, 2, :]
        ones_b = cm_b[:, 3, :]
        invf = cv[:, 0:1]
        eoffmd = cv[:, 80:112]
        lamneg = small[:, 0:1]

        P.dma("sp", lambda e: e.dma_start(out=cm_f[:], in_=cmat[:, :, :]), writes=["cm_f"])
        P.dma("pool", lambda e: e.dma_start(out=cm_b[:], in_=cmat[:, :, :]), writes=["cm_b"])
        P.dma("sp", lambda e: e.dma_start(out=cv[:], in_=cvec[:, :]), writes=["cv"])
        P.dma("sp", lambda e: e.dma_start(out=lvb[:], in_=lam_vecs[0:1, :].to_broadcast([128, 256])), writes=["lvb"])
        P.dma("sp", lambda e: e.dma_start(out=gainb[:], in_=subln_gain[0:1, :].to_broadcast([128, 128])), writes=["gainb"])
        P.dma("sp", lambda e: e.dma_start(out=brt[:], in_=b_rt[0:1, :].to_broadcast([128, 36])), writes=["brt"])
        P.dma("sp", lambda e: e.dma_start(out=gbias[:], in_=gate_bias[:, :]), writes=["gbias"])
        P.dma("sp", lambda e: e.dma_start(out=pscale[:], in_=pool_scale[:, :]), writes=["pscale"])
        P.dma("pool", lambda e: e.dma_start(out=wpool_b[:], in_=w_pool.rearrange("g c d -> c g d")), writes=["wpool_b"])
        P.dma("sp", lambda e: e.dma_start(out=wrt[:], in_=w_rt_k), writes=["wrt"])
        P.op("dve", lambda e: e.memset(cnt[:], 0.0), writes=["cnt"])
        P.op("dve", lambda e: e.memset(slots_i[:], 0), writes=["slots_i_init"])
        P.op("dve", lambda e: e.memset(wts[:], 0.0), writes=["wts_init"])
        P.op("dve", lambda e: e.memset(zrow[:], 0.0), writes=["zrow"])
        P.op("dve", lambda e: e.memset(negh[:], -0.5), writes=["negh"])
        P.op("dve", lambda e: e.tensor_scalar(gainb[:], gainb[:], 1.0 - LAMBDA_INIT, None, op0=ALU.mult),
             reads=["gainb"], writes=["gainb"])
        P.op("dve", lambda e: e.memset(zb3[:], 0.0), writes=["zb3"])
        xs_v = xs.rearrange("(n p) d -> p n d", p=128)
        n_tot = NSLOT // 128
        xs_init = [("xs_init", n0) for n0 in range(0, n_tot, 25)]

        def emit_xs_zero_fill():
            for n0 in range(0, n_tot, 25):
                n1 = min(n_tot, n0 + 25)
                P.dma("sp", lambda e: e.dma_start(out=xs_v[:, n0:n1, :], in_=zb3[:].to_broadcast([128, n1 - n0, 1024])),
                      reads=["zb3"], writes=[("xs_init", n0)])

        P.dma("sp", lambda e: e.dma_start(out=ys[DUMP:DUMP + 128, :], in_=zrow[:]), reads=["zrow"], writes=["ys_dump"])
        P.op("dve", lambda e: e.tensor_tensor(lvb[:, 0:64], lvb[:, 0:64], lvb[:, 64:128], op=ALU.mult),
             reads=["lvb"], writes=["lvb"])
        P.op("dve", lambda e: e.tensor_tensor(lvb[:, 128:192], lvb[:, 128:192], lvb[:, 192:256], op=ALU.mult),
             reads=["lvb"], writes=["lvb"])
        P.op("dve", lambda e: e.reduce_sum(small[:, 1:2], lvb[:, 0:64], axis=AX.X), reads=["lvb"], writes=["small"])
        P.op("dve", lambda e: e.reduce_sum(small[:, 2:3], lvb[:, 128:192], axis=AX.X), reads=["lvb", "small"], writes=["small"])
        P.op("act", lambda e: e.activation(out=small[:, 3:5], in_=small[:, 1:3], func=AF.Exp), reads=["small"], writes=["small"])
        P.op("dve", lambda e: e.tensor_tensor(small[:, 5:6], small[:, 4:5], small[:, 3:4], op=ALU.subtract),
             reads=["small"], writes=["small"])
        P.op("dve", lambda e: e.tensor_scalar(small[:, 0:1], small[:, 5:6], -LAMBDA_INIT, None, op0=ALU.add),
             reads=["small"], writes=["small"])

        ev_flip = [0]

        def evac(out_ap, in_ap, reads, writes, eng=None):
            if eng is None:
                eng = "act" if ev_flip[0] % 2 == 0 else "dve"
                ev_flip[0] += 1
            if eng == "act":
                return P.op("act", lambda e: e.activation(out=out_ap, in_=in_ap, func=AF.Copy), reads=reads, writes=writes)
            return P.op("dve", lambda e: e.tensor_copy(out_ap, in_ap), reads=reads, writes=writes)

        def pipeline(n_items, stages):
            for step in range(n_items + len(stages) - 1):
                lists = []
                for si, f in enumerate(stages):
                    k = step - si
                    if 0 <= k < n_items:
                        lists.append(P.record(f, k))
                pos = [0] * len(lists)
                left = sum(len(l) for l in lists)
                while left:
                    for li, l in enumerate(lists):
                        if pos[li] < len(l):
                            P.replay(l[pos[li]])
                            pos[li] += 1
                            left -= 1

        conv_jobs = []
        for e_ in range(NE):
            conv_jobs.append((wgu_bf[e_, 0:512, :], w_gu[e_, 0:512, :], ("wcv", e_, 0)))
            conv_jobs.append((wgu_bf[e_, 512:1024, :], w_gu[e_, 512:1024, :], ("wcv", e_, 1)))
            conv_jobs.append((wdn_bf[e_, :, :], w_dn[e_, :, :], ("wcv", e_, 2)))
        conv_next = [0]
        conv_tick = [0]

        def conv_issue(n=1):
            for _ in range(n):
                if conv_next[0] < len(conv_jobs):
                    o_, i_, nm_ = conv_jobs[conv_next[0]]
                    conv_next[0] += 1
                    P.dma("poolc", lambda e: e.dma_start(out=o_, in_=i_), writes=[nm_])

        for s in range(NSEQ):
            s2 = contextlib.ExitStack()
            with s2:
                V = sb(s2, "V", [128, NT, H, 129], BF16)
                cosT = sb(s2, "cosT", [128, S], F32)
                sinT = sb(s2, "sinT", [128, S], F32)
                with contextlib.ExitStack() as s2a:
                    xt = [sb(s2a, "xt%d" % i, [128, D], F32) for i in range(2)]
                    posi = sb(s2a, "posi", [128, S], I32)
                    ang = sb(s2a, "ang", [128, S], F32)
                    ras = [sb(s2a, "ra%d" % i, [128, S], F32) for i in range(2)]
                    rk = sb(s2a, "rk", [128, S], F32)
                    ki = sb(s2a, "ki", [128, S], I32)
                    wv = sb(s2a, "wv", [128, 8, D], BF16)
                    for kc2 in range(2):
                        P.dma("pool", lambda e: e.dma_start(out=wv[:, kc2 * 4:(kc2 + 1) * 4, :],
                                                            in_=w_in_k[:, kc2 * 4:(kc2 + 1) * 4, 2048:3072]),
                              writes=[("wv", kc2)])
                    P.dma("sp", lambda e: e.dma_start(out=posi[:], in_=pos[s:s + 1, :].to_broadcast([128, S])), writes=["posi"])
                    P.op("pool", lambda e: e.memset(V[:, :, :, 128:129], 1.0), writes=["Vones"])
                    P.op("dve", lambda e: e.tensor_copy(ang[:], posi[:]), reads=["posi"], writes=["ang"])
                    P.op("dve", lambda e: e.tensor_scalar(ang[:], ang[:], invf, None, op0=ALU.mult), reads=["ang", "cv"], writes=["ang"])
                    for which in range(2):
                        ra, ran = ras[which], "ra%d" % which
                        shift = 0.0 if which == 0 else 0.5 * math.pi
                        P.op("dve", lambda e: e.tensor_scalar(ra[:], ang[:], shift, None, op0=ALU.add), reads=["ang"], writes=[ran])
                        P.op("dve", lambda e: e.tensor_scalar(rk[:], ra[:], 1.0 / TWO_PI, None, op0=ALU.mult), reads=[ran], writes=["rk"])
                        P.op("dve", lambda e: e.tensor_copy(ki[:], rk[:]), reads=["rk"], writes=["ki"])
                        P.op("dve", lambda e: e.tensor_copy(rk[:], ki[:]), reads=["ki"], writes=["rk"])
                        P.op("dve", lambda e: e.scalar_tensor_tensor(ra[:], rk[:], -C1, ra[:], op0=ALU.mult, op1=ALU.add),
                             reads=["rk", ran], writes=[ran])
                        P.op("dve", lambda e: e.scalar_tensor_tensor(ra[:], rk[:], -C2, ra[:], op0=ALU.mult, op1=ALU.add),
                             reads=["rk", ran], writes=[ran])
                        P.op("dve", lambda e: e.tensor_scalar(rk[:], ra[:], math.pi, -TWO_PI, op0=ALU.is_gt, op1=ALU.mult),
                             reads=[ran], writes=["rk"])
                        P.op("dve", lambda e: e.tensor_tensor(ra[:], ra[:], rk[:], op=ALU.add), reads=[ran, "rk"], writes=[ran])
                        P.op("dve", lambda e: e.tensor_scalar(rk[:], ra[:], -math.pi, TWO_PI, op0=ALU.is_lt, op1=ALU.mult),
                             reads=[ran], writes=["rk"])
                        P.op("dve", lambda e: e.tensor_tensor(ra[:], ra[:], rk[:], op=ALU.add), reads=[ran, "rk"], writes=[ran])
                        P.op("dve", lambda e: e.tensor_scalar(ra[:], ra[:], 3.1415925, -3.1415925, op0=ALU.min, op1=ALU.max),
                             reads=[ran], writes=[ran])
                    for t in range(NT):
                        xb_ = xt[t % 2]
                        xn = "xt%d" % (t % 2)
                        P.dma("sp", lambda e: e.dma_start(out=xb_[:], in_=x[s, t * 128:(t + 1) * 128, :]), writes=[xn])
                        for hf in range(2):
                            bk = (t * 2 + hf) % 4
                            for j in range(4):
                                kc = hf * 4 + j
                                P.op("pe", lambda e: e.transpose(B[bk][:, j * 128:(j + 1) * 128], xb_[:, kc * 128:(kc + 1) * 128], ident_f),
                                     reads=[xn, "cm_f"], writes=[BN[bk]])
                            evac(xT[:, hf * 4:(hf + 1) * 4, t * 128:(t + 1) * 128],
                                 B[bk][:].rearrange("p (a b) -> p a b", a=4), reads=[BN[bk]], writes=[("xT", t)], eng="act")
                    P.op("act", lambda e: e.activation(out=sinT[:], in_=ras[0][:], func=AF.Sin), reads=["ra0"], writes=["sinT"])
                    P.op("act", lambda e: e.activation(out=cosT[:], in_=ras[1][:], func=AF.Sin), reads=["ra1"], writes=["cosT"])
                    if stop == "a1":
                        P.dma("sp", lambda e: e.dma_start(out=dbg["oT"][s], in_=xT[:]), reads=[("xT", t_) for t_ in range(NT)], writes=["dbg"])
                    for t in range(NT):
                        for hf in range(2):
                            bk = 4 + (t * 2 + hf) % 4
                            for kc in range(8):
                                P.op("pe", lambda e: e.matmul(B[bk][:], lhsT=xT[:, kc, t * 128:(t + 1) * 128],
                                                              rhs=wv[:, kc, hf * 512:(hf + 1) * 512],
                                                              start=(kc == 0), stop=(kc == 7)),
                                     reads=[("xT", t), ("wv", kc // 4)], writes=[BN[bk]])
                            evac(V[:, t, hf * 4:(hf + 1) * 4, 0:128], B[bk][:].rearrange("p (a b) -> p a b", a=4),
                                 reads=[BN[bk]], writes=[("V", t)])
                    P.barrier()
                if s == 0:
                    emit_xs_zero_fill()
                if stop in ("a1", "rope", "v"):
                    P.barrier()
                    continue
                with contextlib.ExitStack() as s2c:
                    wq = [sb(s2c, "wq%d" % i, [128, 8, 128], BF16) for i in range(2)]
                    wk = [sb(s2c, "wk%d" % i, [128, 8, 128], BF16) for i in range(2)]
                    qT = [sb(s2c, "qT%d" % i, [128, S], BF16) for i in range(2)]
                    kz = [[sb(s2c, "kz%d_%d" % (i, m), [128, S], BF16) for m in range(2)] for i in range(2)]
                    qb = [sb(s2c, "qb%d" % i, [128, 512], BF16) for i in range(2)]
                    t1 = [sb(s2c, "t1_%d" % i, [128, 512], F32) for i in range(2)]
                    t2 = [sb(s2c, "t2_%d" % i, [128, 512], F32) for i in range(2)]
                    PT = [sb(s2c, "PT%d" % i, [128, 512], BF16) for i in range(4)]
                    accs = [sb(s2c, "accs%d" % i, [128, 1032], F32) for i in range(2)]
                    rec3 = sb(s2c, "rec3", [128, 4, 2, 1], F32)
                    O4 = sb(s2c, "O4", [128, 4, 128], F32)
                    T4 = sb(s2c, "T4", [128, 4, 128], F32)
                    ss = sb(s2c, "ss", [128, 4], F32)
                    ssv = sb(s2c, "ssv", [128, 4], F32)
                    rstd3 = sb(s2c, "rstd3", [128, 4, 1], F32)
                    onb = [sb(s2c, "onb%d" % i, [128, 4, 128], BF16) for i in range(2)]
                    rope_i = [0]
                    NG = S // 512
                    for i_ in range(2):
                        P.op("pool", lambda e: e.memset(kz[i_][0][64:128, :], 0.0), writes=[("kzz", i_, 0)])
                        P.op("pool", lambda e: e.memset(kz[i_][1][0:64, :], 0.0), writes=[("kzz", i_, 1)])

                    def load_head_w(h):
                        b = h % 2
                        P.dma("pool", lambda e: e.dma_start(out=wq[b][:], in_=w_in_k[:, :, h * 128:(h + 1) * 128]),
                              writes=["wq%d" % b])
                        P.dma("pool", lambda e: e.dma_start(out=wk[b][:], in_=w_in_k[:, :, 1024 + h * 128:1024 + (h + 1) * 128]),
                              writes=["wk%d" % b])

                    def proj_units(h):
                        hb = h % 2
                        units = []
                        for which in range(2):
                            w_t = (wq if which == 0 else wk)[hb]
                            wname = ("wq%d" if which == 0 else "wk%d") % hb
                            for tg in range(NG):
                                st_ = {}

                                def part_a(w_t=w_t, wname=wname, tg=tg, st_=st_):
                                    i = rope_i[0]
                                    rope_i[0] += 1
                                    st_["i"] = i
                                    r = i % 2
                                    pb = 3 if i % 2 == 0 else 7
                                    tsl = slice(tg * 512, (tg + 1) * 512)
                                    for kc in range(8):
                                        P.op("pe", lambda e: e.matmul(B[pb][:], lhsT=w_t[:, kc, :], rhs=xT[:, kc, tsl],
                                                                      start=(kc == 0), stop=(kc == 7)),
                                             reads=[wname], writes=[BN[pb]])
                                    P.op("act", lambda e: e.activation(out=qb[r][:], in_=B[pb][:], func=AF.Copy),
                                         reads=[BN[pb]], writes=["qb%d" % r])
                                    P.op("dve", lambda e: e.tensor_tensor(t1[r][:], B[pb][:], cosT[:, tsl], op=ALU.mult),
                                         reads=[BN[pb], "cosT"], writes=["t1_%d" % r])

                                def part_b(which=which, tg=tg, st_=st_, hb=hb):
                                    i = st_["i"]
                                    r = i % 2
                                    pb = 3 if i % 2 == 0 else 7
                                    tsl = slice(tg * 512, (tg + 1) * 512)
                                    P.op("pe", lambda e: e.matmul(B[pb][:], lhsT=prot_b, rhs=qb[r][:], start=True, stop=True),
                                         reads=["qb%d" % r, "cm_b"], writes=[BN[pb]])
                                    P.op("dve", lambda e: e.tensor_tensor(t2[r][:], B[pb][:], sinT[:, tsl], op=ALU.mult),
                                         reads=[BN[pb], "sinT"], writes=["t2_%d" % r])
                                    if which == 0:
                                        P.op("dve", lambda e: e.tensor_tensor(qT[hb][:, tsl], t1[r][:], t2[r][:], op=ALU.add),
                                             reads=["t1_%d" % r, "t2_%d" % r], writes=[("qT%d" % hb, tg)])
                                    else:
                                        for m in range(2):
                                            ps_ = slice(m * 64, (m + 1) * 64)
                                            P.op("dve", lambda e: e.tensor_tensor(kz[hb][m][ps_, tsl], t1[r][ps_, :], t2[r][ps_, :], op=ALU.add),
                                                 reads=["t1_%d" % r, "t2_%d" % r, ("kzz", hb, m)], writes=[("kz%d_%d" % (hb, m), tg)])
                                units.append(part_a)
                                units.append(part_b)
                        return units

                    LOOK = 2
                    gstep = [0]
                    deferred = []

                    def run_due(force=False):
                        keep = []
                        for due, fn in deferred:
                            if force or due <= gstep[0]:
                                fn()
                            else:
                                keep.append((due, fn))
                        deferred[:] = keep

                    def defer(delay, fn):
                        deferred.append((gstep[0] + delay, fn))

                    grp_i = [0]
                    load_head_w(0)
                    for u in proj_units(0):
                        u()
                    for h in range(H):
                        hb = h % 2
                        if h + 1 < H:
                            load_head_w(h + 1)
                            nxt = proj_units(h + 1)
                        else:
                            nxt = []
                        qn = "qT%d" % hb
                        steps = []
                        for g in range(NG):
                            for j in range(4 * g + 4):
                                for m in range(2):
                                    steps.append((g, j, m))
                        nsteps = len(steps)
                        unit_at = {}
                        if nxt:
                            gap = max(1, (nsteps - 8) // len(nxt))
                            for ui, u in enumerate(nxt):
                                unit_at.setdefault(4 + ui * gap, []).append(u)
                        started = {}
                        pv_pending = []

                        def emit_pv(g, j, m, pt, ptn, q0):
                            for il in range(q0 - 4 * g, 4):
                                a = il * 2 + m
                                ab = 4 + a // 3
                                col = (a % 3) * 129
                                c0 = (il - (q0 - 4 * g)) * 128
                                first = (g, ab) not in started
                                started[(g, ab)] = True
                                P.op("pe", lambda e: e.matmul(B[ab][:, col:col + 129], lhsT=pt[:, c0:c0 + 128],
                                                              rhs=V[:, j, h, :], start=first, stop=(j == 4 * g + il),
                                                              skip_group_check=True),
                                     reads=[ptn, (ptn, "m"), ("V", j), "Vones"], writes=[BN[ab]])
                            if j == 4 * g + 3 and m == 1:
                                emit_group_end(g)

                        def emit_group_end(g, h=h):
                            gi = grp_i[0]
                            grp_i[0] += 1
                            ac = accs[gi % 2]
                            acn = "accs%d" % (gi % 2)
                            evac(ac[:, 0:387], B[4][:, 0:387], reads=[BN[4]], writes=[(acn, 0)], eng="dve")
                            evac(ac[:, 387:774], B[5][:, 0:387], reads=[BN[5]], writes=[(acn, 1)], eng="act")
                            evac(ac[:, 774:1032], B[6][:, 0:258], reads=[BN[6]], writes=[(acn, 2)], eng="dve")
                            acv = ac[:, :].rearrange("p (i m c) -> p i m c", i=4, m=2)
                            acr = [(acn, 0), (acn, 1), (acn, 2)]

                            def norm_1():
                                P.op("dve", lambda e: e.reciprocal(rec3[:, :, :, 0], acv[:, :, :, 128]), reads=acr, writes=["rec"])
                                P.op("dve", lambda e: e.tensor_scalar(rec3[:, :, 1, 0], rec3[:, :, 1, 0], lamneg, None, op0=ALU.mult),
                                     reads=["rec", "small"], writes=["rec"])
                                P.op("dve", lambda e: e.tensor_tensor(O4[:], acv[:, :, 0, 0:128], rec3[:, :, 0, :].to_broadcast([128, 4, 128]), op=ALU.mult),
                                     reads=acr + ["rec"], writes=["O4"])
                                P.op("dve", lambda e: e.tensor_tensor(T4[:], acv[:, :, 1, 0:128], rec3[:, :, 1, :].to_broadcast([128, 4, 128]), op=ALU.mult),
                                     reads=acr + ["rec"], writes=["T4"])

                            def norm_2():
                                P.op("dve", lambda e: e.tensor_tensor(O4[:], O4[:], T4[:], op=ALU.add), reads=["O4", "T4"], writes=["O4"])
                                P.op("dve", lambda e: e.tensor_tensor(T4[:], O4[:], O4[:], op=ALU.mult), reads=["O4"], writes=["T4"])
                                P.op("dve", lambda e: e.reduce_sum(ss[:], T4[:], axis=AX.X), reads=["T4"], writes=["ss"])
                                P.op("dve", lambda e: e.tensor_scalar(ssv[:], ss[:], 1.0 / 128.0, RMS_EPS, op0=ALU.mult, op1=ALU.add),
                                     reads=["ss"], writes=["ssv"])
                                P.op("pool", lambda e: e.tensor_tensor(rstd3[:, :, 0], ssv[:], negh[:], op=ALU.pow), reads=["ssv", "negh"], writes=["rstd"])

                            def norm_on():
                                ob = onb[gi % 2]
                                obn = "onb%d" % (gi % 2)
                                P.op("dve", lambda e: e.tensor_tensor(O4[:], O4[:], rstd3[:].to_broadcast([128, 4, 128]), op=ALU.mult),
                                     reads=["O4", "rstd"], writes=["O4"])
                                P.op("dve", lambda e: e.tensor_tensor(ob[:], O4[:], gainb[:, None, :].to_broadcast([128, 4, 128]), op=ALU.mult),
                                     reads=["O4", "gainb"], writes=[obn])

                            def norm_b():
                                ob = onb[gi % 2]
                                obn = "onb%d" % (gi % 2)
                                for il in range(4):
                                    P.op("pe", lambda e: e.transpose(bank_bf(7)[:, il * 128:(il + 1) * 128], ob[:, il, :], ident_b),
                                         reads=[obn, "cm_b"], writes=[BN[7]])
                                evac(oT[:, h, g * 512:(g + 1) * 512], bank_bf(7)[:, 0:512], reads=[BN[7]], writes=[("oT", g)])

                            defer(1, norm_1)
                            defer(3, norm_2)
                            defer(6, norm_on)
                            defer(11, norm_b)

                        for si, (g, j, m) in enumerate(steps):
                            gstep[0] += 1
                            run_due()
                            conv_tick[0] += 1
                            if conv_tick[0] % CONV_EVERY == 0:
                                conv_issue()
                            for u in unit_at.get(si, []):
                                u()
                            q0 = max(j, 4 * g)
                            N = (4 * g + 4 - q0) * 128
                            qsl = slice(q0 * 128, (4 * g + 4) * 128)
                            i = gstep[0]
                            bk = i % 3
                            pt = PT[i % 4]
                            ptn = "PT%d" % (i % 4)
                            P.op("pe", lambda e: e.matmul(B[bk][:, 0:N], lhsT=kz[hb][m][:, j * 128:(j + 1) * 128],
                                                          rhs=qT[hb][:, qsl], start=True, stop=True),
                                 reads=[("kz%d_%d" % (hb, m), j // 4), ("kzz", hb, m), (qn, g)], writes=[BN[bk]])
                            if j >= 4 * g:
                                P.op("act", lambda e: e.activation(out=pt[:, 0:64], in_=B[bk][:, 0:64], func=AF.Exp, scale=0.125, bias=cv[:, 112:113]),
                                     reads=[BN[bk], "cv"], writes=[(ptn, "m")])
                                P.op("act", lambda e: e.activation(out=pt[:, 64:N], in_=B[bk][:, 64:N], func=AF.Exp, scale=0.125),
                                     reads=[BN[bk]], writes=[ptn])
                            else:
                                P.op("act", lambda e: e.activation(out=pt[:, 0:N], in_=B[bk][:, 0:N], func=AF.Exp, scale=0.125),
                                     reads=[BN[bk]], writes=[ptn, (ptn, "m")])
                            pv_pending.append((g, j, m, pt, ptn, q0))
                            if len(pv_pending) > LOOK:
                                emit_pv(*pv_pending.pop(0))
                        while pv_pending:
                            emit_pv(*pv_pending.pop(0))
                        for si_ in sorted(unit_at):
                            if si_ >= nsteps:
                                for u in unit_at[si_]:
                                    u()
                    run_due(force=True)
                    if s == NSEQ - 1:
                        conv_issue(len(conv_jobs))
                    P.barrier()
            if stop in ("attn", "qk"):
                P.dma("sp", lambda e: e.dma_start(out=dbg["oT"][s], in_=oT[:]), reads=[], writes=["dbg"])
                P.barrier()
                continue
            with contextlib.ExitStack() as s3:
                mergedT = sb(s3, "mergedT", [128, 8, S], BF16)
                mixedT = sb(s3, "mixedT", [128, 4, S], BF16)
                NG = S // 512
                with contextlib.ExitStack() as s3a:
                    wu = sb(s3a, "wu", [128, 8, 512], BF16)
                    uT = sb(s3a, "uT", [128, 4, S], F32)
                    Tp = [sb(s3a, "Tp%d" % i, [128, S], F32) for i in range(2)]
                    pooledT = sb(s3a, "pooledT", [128, 4, S], BF16)
                    for kc2 in range(2):
                        P.dma("pool", lambda e: e.dma_start(out=wu[:, kc2 * 4:(kc2 + 1) * 4, :],
                                                            in_=w_in_k[:, kc2 * 4:(kc2 + 1) * 4, 3072:3584]),
                              writes=[("wu", kc2)])
                    bi = 0
                    for g in range(4):
                        for tg in range(NG):
                            bk = bi % 4
                            bi += 1
                            tsl = slice(tg * 512, (tg + 1) * 512)
                            for kc in range(8):
                                P.op("pe", lambda e: e.matmul(B[bk][:], lhsT=wu[:, kc, g * 128:(g + 1) * 128], rhs=xT[:, kc, tsl],
                                                              start=(kc == 0), stop=(kc == 7)),
                                     reads=[("wu", kc // 4)], writes=[BN[bk]])
                            evac(uT[:, g, tsl], B[bk][:], reads=[BN[bk]], writes=[("uT", g)])
                    for g, w in enumerate((2, 4, 8, 16)):
                        cur, cur_r = uT[:, g, :], [("uT", g)]
                        k = 0
                        sh = 1
                        while sh < w:
                            dst, dstn = Tp[k % 2], "Tp%d" % (k % 2)
                            eng = "dve"
                            P.op(eng, lambda e: e.tensor_tensor(dst[:, sh:S], cur[:, sh:S], cur[:, 0:S - sh], op=ALU.add),
                                 reads=cur_r, writes=[dstn])
                            P.op(eng, lambda e: e.tensor_copy(dst[:, 0:sh], cur[:, 0:sh]), reads=cur_r, writes=[dstn + "h"])
                            cur, cur_r = dst[:, :], [dstn, dstn + "h"]
                            k += 1
                            sh *= 2
                        P.op("dve", lambda e: e.tensor_tensor(cur[:, 0:16], cur[:, 0:16], cv[:, 16 + g * 16:32 + g * 16], op=ALU.mult),
                             reads=cur_r + ["cv"], writes=cur_r)
                        P.op("dve", lambda e: e.scalar_tensor_tensor(pooledT[:, g, :], cur, 1.0 / w, uT[:, g, :], op0=ALU.mult, op1=ALU.subtract),
                             reads=cur_r + [("uT", g)], writes=[("pooledT", g)])
                    for g in range(4):
                        for tg in range(NG):
                            bk = bi % 4
                            bi += 1
                            tsl = slice(tg * 512, (tg + 1) * 512)
                            P.op("pe", lambda e: e.matmul(B[bk][:], lhsT=wpool_b[:, g, :], rhs=pooledT[:, g, tsl], start=True, stop=True),
                                 reads=[("pooledT", g), "wpool_b"], writes=[BN[bk]])
                            P.op("dve", lambda e: e.tensor_scalar(mixedT[:, g, tsl], B[bk][:], pscale[:, g:g + 1], None, op0=ALU.mult),
                                 reads=[BN[bk], "pscale"], writes=[("mixedT", g)])
                    P.barrier()
                with contextlib.ExitStack() as s3b:
                    wa_c = [sb(s3b, "wa_c%d" % i, [128, 8, 128], BF16) for i in range(2)]
                    wb_c = [sb(s3b, "wb_c%d" % i, [128, 4, 128], BF16) for i in range(2)]
                    wga_c = [sb(s3b, "wga_c%d" % i, [128, 8, 128], BF16) for i in range(2)]
                    wgb_c = [sb(s3b, "wgb_c%d" % i, [128, 8, 128], BF16) for i in range(2)]
                    ga = [sb(s3b, "ga%d" % i, [128, 512], F32) for i in range(2)]
                    gb = [sb(s3b, "gb%d" % i, [128, 512], F32) for i in range(2)]
                    m1 = [sb(s3b, "m1_%d" % i, [128, 512], F32) for i in range(2)]
                    m2 = [sb(s3b, "m2_%d" % i, [128, 512], F32) for i in range(2)]

                    def load_chunk_w(c):
                        b = c % 2
                        cs = slice(c * 128, (c + 1) * 128)
                        P.dma("pool", lambda e: e.dma_start(out=wa_c[b][:], in_=w_a_k[:, :, cs]), writes=["wa_c%d" % b])
                        P.dma("pool", lambda e: e.dma_start(out=wb_c[b][:], in_=w_b_k[:, :, cs]), writes=["wb_c%d" % b])
                        P.dma("pool", lambda e: e.dma_start(out=wga_c[b][:], in_=w_in_k[:, :, 3584 + c * 128:3584 + (c + 1) * 128]),
                              writes=["wga_c%d" % b])
                        P.dma("pool", lambda e: e.dma_start(out=wgb_c[b][:], in_=w_in_k[:, :, 4608 + c * 128:4608 + (c + 1) * 128]),
                              writes=["wgb_c%d" % b])

                    load_chunk_w(0)
                    it = 0
                    for c in range(8):
                        cb = c % 2
                        if c + 1 < 8:
                            load_chunk_w(c + 1)
                        for tg in range(NG):
                            r = it % 2
                            it += 1
                            b0 = r * 4
                            tsl = slice(tg * 512, (tg + 1) * 512)
                            for hh_ in range(8):
                                P.op("pe", lambda e: e.matmul(B[b0][:], lhsT=wa_c[cb][:, hh_, :], rhs=oT[:, hh_, tsl],
                                                              start=(hh_ == 0), stop=(hh_ == 7)),
                                     reads=["wa_c%d" % cb], writes=[BN[b0]])
                            for g in range(4):
                                P.op("pe", lambda e: e.matmul(B[b0 + 1][:], lhsT=wb_c[cb][:, g, :], rhs=mixedT[:, g, tsl],
                                                              start=(g == 0), stop=(g == 3)),
                                     reads=["wb_c%d" % cb], writes=[BN[b0 + 1]])
                            for kc in range(8):
                                P.op("pe", lambda e: e.matmul(B[b0 + 2][:], lhsT=wga_c[cb][:, kc, :], rhs=xT[:, kc, tsl],
                                                              start=(kc == 0), stop=(kc == 7)),
                                     reads=["wga_c%d" % cb], writes=[BN[b0 + 2]])
                            for kc in range(8):
                                P.op("pe", lambda e: e.matmul(B[b0 + 3][:], lhsT=wgb_c[cb][:, kc, :], rhs=xT[:, kc, tsl],
                                                              start=(kc == 0), stop=(kc == 7)),
                                     reads=["wgb_c%d" % cb], writes=[BN[b0 + 3]])
                            P.op("act", lambda e: e.activation(out=ga[r][:], in_=B[b0 + 2][:], func=AF.Sigmoid, bias=gbias[:, c:c + 1]),
                                 reads=[BN[b0 + 2], "gbias"], writes=["ga%d" % r])
                            P.op("act", lambda e: e.activation(out=gb[r][:], in_=B[b0 + 3][:], func=AF.Sigmoid, bias=gbias[:, 8 + c:9 + c]),
                                 reads=[BN[b0 + 3], "gbias"], writes=["gb%d" % r])
                            P.op("dve", lambda e: e.tensor_tensor(m1[r][:], B[b0][:], ga[r][:], op=ALU.mult),
                                 reads=[BN[b0], "ga%d" % r], writes=["m1_%d" % r])
                            P.op("dve", lambda e: e.tensor_tensor(m2[r][:], B[b0 + 1][:], gb[r][:], op=ALU.mult),
                                 reads=[BN[b0 + 1], "gb%d" % r], writes=["m2_%d" % r])
                            P.op("pool", lambda e: e.tensor_tensor(mergedT[:, c, tsl], m1[r][:], m2[r][:], op=ALU.add),
                                 reads=["m1_%d" % r, "m2_%d" % r], writes=[("mergedT", c)])
                    P.barrier()
                if stop == "mix":
                    P.dma("sp", lambda e: e.dma_start(out=dbg["mergedT"][s], in_=mergedT[:]), reads=[], writes=["dbg"])
                    P.barrier()
                    continue
                with contextlib.ExitStack() as s3c:
                    wout = sb(s3c, "wout", [128, 8, D], BF16)
                    g1 = sb(s3c, "g1", [128, D], F32)
                    b1 = sb(s3c, "b1", [128, D], F32)
                    xt = [sb(s3c, "xt%d" % i, [128, D], F32) for i in range(2)]
                    zs = [sb(s3c, "z%d" % i, [128, D], F32) for i in range(3)]
                    hh = [sb(s3c, "hh%d" % i, [128, D], F32) for i in range(3)]
                    hbf = [sb(s3c, "hbf%d" % i, [128, D], BF16) for i in range(4)]
                    hTs = [sb(s3c, "hT%d" % i, [128, 8, 128], F32) for i in range(2)]
                    statss = [sb(s3c, "stats%d" % i, [128, 2, 6], F32) for i in range(3)]
                    mvs = [sb(s3c, "mv%d" % i, [128, 4], F32) for i in range(3)]
                    rrs = [sb(s3c, "rr%d" % i, [128, 16], F32) for i in range(2)]
                    nm = sb(s3c, "nm", [128, 3], F32)
                    junkA = sb(s3c, "junkA", [128, D], F32)
                    lg = sb(s3c, "lg", [128, 36], F32)
                    ge = sb(s3c, "ge", [128, 4], F32)
                    gone = sb(s3c, "gone", [128, 4], F32)
                    mneg = sb(s3c, "mneg", [128, 4, 1], F32)
                    elms = [sb(s3c, "elm%d" % i, [128, 4, 8], F32) for i in range(2)]
                    elm2 = sb(s3c, "elm2", [128, 32], F32)
                    oh1s = [sb(s3c, "oh1_%d" % i, [128, 32], F32) for i in range(3)]
                    oh2s = [sb(s3c, "oh2_%d" % i, [128, 32], F32) for i in range(2)]
                    Mbs = [sb(s3c, "Mb%d" % i, [128, 32], BF16) for i in range(2)]
                    posin = sb(s3c, "posin", [128, 32], F32)
                    valid = sb(s3c, "valid", [128, 32], F32)
                    smv = sb(s3c, "smv", [128, 32], F32)
                    tmp32 = sb(s3c, "tmp32", [128, 32], F32)
                    for kc2 in range(2):
                        P.dma("pool", lambda e: e.dma_start(out=wout[:, kc2 * 4:(kc2 + 1) * 4, :], in_=w_out_k[:, kc2 * 4:(kc2 + 1) * 4, :]),
                              writes=[("wout", kc2)])
                    P.dma("sp", lambda e: e.dma_start(out=g1[:], in_=ln1_g[0:1, :].to_broadcast([128, D])), writes=["g1"])
                    P.dma("sp", lambda e: e.dma_start(out=b1[:], in_=ln1_b[0:1, :].to_broadcast([128, D])), writes=["b1"])

                    def L0a(t):
                        r = t % 2
                        xb_, xn = xt[r], "xt%d" % r
                        P.dma("sp", lambda e: e.dma_start(out=xb_[:], in_=x[s, t * 128:(t + 1) * 128, :]), writes=[xn])
                        for hf in range(2):
                            bk = (0, 1)[hf] if r == 0 else (6, 7)[hf]
                            for kc in range(8):
                                P.op("pe", lambda e: e.matmul(B[bk][:], lhsT=mergedT[:, kc, t * 128:(t + 1) * 128],
                                                              rhs=wout[:, kc, hf * 512:(hf + 1) * 512], start=(kc == 0), stop=(kc == 7)),
                                     reads=[("wout", kc // 4)], writes=[BN[bk]])

                    def L0b(t):
                        r = t % 2
                        q3 = t % 3
                        xb_, xn = xt[r], "xt%d" % r
                        z, stats, mv = zs[q3], statss[q3], mvs[q3]
                        zn = "z%d" % q3
                        for hf in range(2):
                            bk = (0, 1)[hf] if r == 0 else (6, 7)[hf]
                            P.op("dve", lambda e: e.scalar_tensor_tensor(z[:, hf * 512:(hf + 1) * 512], xb_[:, hf * 512:(hf + 1) * 512], ALPHA, B[bk][:],
                                                                         op0=ALU.mult, op1=ALU.add),
                                 reads=[xn, BN[bk]], writes=[(zn, hf)])
                        P.op("act", lambda e: e.activation(out=junkA[:], in_=z[:], func=AF.Copy, accum_out=stats[:, 0, 0:1]),
                             reads=[(zn, 0), (zn, 1)], writes=["junkA", ("stats", q3, 0)])
                        P.op("act", lambda e: e.activation(out=junkA[:], in_=z[:], func=AF.Square, accum_out=stats[:, 0, 1:2]),
                             reads=[(zn, 0), (zn, 1)], writes=["junkA", ("stats", q3, 1)])
                        P.op("dve", lambda e: e.tensor_scalar(mv[:, 0:1], stats[:, 0, 0:1], 1.0 / D, None, op0=ALU.mult), reads=[("stats", q3, 0)], writes=[("mv", q3)])
                        P.op("dve", lambda e: e.tensor_tensor(mv[:, 1:2], mv[:, 0:1], mv[:, 0:1], op=ALU.mult), reads=[("mv", q3)], writes=[("mvq", q3)])
                        P.op("dve", lambda e: e.scalar_tensor_tensor(mv[:, 2:3], stats[:, 0, 1:2], 1.0 / D, mv[:, 1:2], op0=ALU.mult, op1=ALU.subtract),
                             reads=[("stats", q3, 1), ("mvq", q3)], writes=[("mvv", q3)])
                        P.op("dve", lambda e: e.tensor_scalar(mv[:, 2:3], mv[:, 2:3], LN_EPS, None, op0=ALU.add), reads=[("mvv", q3)], writes=[("mv2", q3)])

                    def L1a(t):
                        q3 = t % 3
                        z, mv = zs[q3], mvs[q3]
                        zn = "z%d" % q3
                        hb_, hn = hh[q3], "hh%d" % q3
                        P.op("pool", lambda e: e.tensor_tensor(mv[:, 3:4], mv[:, 2:3], negh[:, 0:1], op=ALU.pow), reads=[("mv2", q3), "negh"], writes=[("mv3", q3)])
                        P.op("dve", lambda e: e.scalar_tensor_tensor(nm[:, q3:q3 + 1], mv[:, 0:1], -1.0, mv[:, 3:4], op0=ALU.mult, op1=ALU.mult),
                             reads=[("mv", q3), ("mv3", q3)], writes=[("nm", q3)])
                        P.op("act", lambda e: e.activation(out=hb_[:], in_=z[:], func=AF.Identity, scale=mv[:, 3:4], bias=nm[:, q3:q3 + 1]),
                             reads=[(zn, 0), (zn, 1), ("mv3", q3), ("nm", q3)], writes=[hn])
                        P.op("dve", lambda e: e.tensor_tensor(hb_[:], hb_[:], g1[:], op=ALU.mult), reads=[hn, "g1"], writes=[hn])
                        P.op("dve", lambda e: e.tensor_tensor(hb_[:], hb_[:], b1[:], op=ALU.add), reads=[hn, "b1"], writes=[hn])

                    def L1b(t):
                        gt = s * NT + t
                        r = t % 2
                        r3 = t % 3
                        hb_, hn = hh[r3], "hh%d" % r3
                        P.dma("sp", lambda e: e.dma_start(out=h_scr[gt * 128:(gt + 1) * 128, :], in_=hb_[:]), reads=[hn], writes=[("h_scr", gt)])
                        if "h" in dbg:
                            P.dma("sp", lambda e: e.dma_start(out=dbg["h"][gt * 128:(gt + 1) * 128, :], in_=hb_[:]), reads=[hn], writes=[("dbg_h", gt)])
                        P.op("act", lambda e: e.activation(out=hbf[t % 4][:], in_=hb_[:], func=AF.Copy), reads=[hn], writes=["hbf%d" % (t % 4)])
                        for hf in range(2):
                            bk = 2 + hf
                            for j in range(4):
                                kc = hf * 4 + j
                                P.op("pe", lambda e: e.transpose(B[bk][:, j * 128:(j + 1) * 128], hb_[:, kc * 128:(kc + 1) * 128], ident_f),
                                     reads=[hn, "cm_f"], writes=[BN[bk]])
                            evac(hTs[r][:, hf * 4:(hf + 1) * 4, :], B[bk][:].rearrange("p (a b) -> p a b", a=4), reads=[BN[bk]], writes=[("hT", r, hf)], eng="act")

                    def L2a1(t):
                        gt = s * NT + t
                        r = t % 2
                        hT = hTs[r]
                        rr = rrs[r]
                        elm = elms[r]
                        elm_f = elm[:].rearrange("p a b -> p (a b)")
                        oh1 = oh1s[t % 3]
                        o1n = "oh1_%d" % (t % 3)
                        for kc in range(8):
                            P.op("pe", lambda e: e.matmul(B[4][:, 0:36], lhsT=hT[:, kc, :], rhs=wrt[:, kc, :], start=(kc == 0), stop=(kc == 7)),
                                 reads=[("hT", r, kc // 4), "wrt"], writes=[BN[4]])
                        P.op("dve", lambda e: e.tensor_tensor(lg[:], B[4][:, 0:36], brt[:], op=ALU.add), reads=[BN[4], "brt"], writes=["lg"])
                        P.op("dve", lambda e: e.reduce_max(rr[:, 0:1], lg[:, 0:4], axis=AX.X), reads=["lg"], writes=[("rr0", r)])
                        P.op("dve", lambda e: e.tensor_scalar(rr[:, 1:2], rr[:, 0:1], -1.0, None, op0=ALU.mult), reads=[("rr0", r)], writes=[("rr1", r)])
                        P.op("act", lambda e: e.activation(out=ge[:], in_=lg[:, 0:4], func=AF.Exp, bias=rr[:, 1:2]), reads=["lg", ("rr1", r)], writes=["ge"])
                        P.op("dve", lambda e: e.reduce_sum(rr[:, 2:3], ge[:], axis=AX.X), reads=["ge"], writes=[("rr2", r)])
                        P.op("dve", lambda e: e.reciprocal(rr[:, 3:4], rr[:, 2:3]), reads=[("rr2", r)], writes=[("rr3", r)])
                        P.op("dve", lambda e: e.tensor_scalar(gone[:], lg[:, 0:4], rr[:, 0:1], None, op0=ALU.is_equal), reads=["lg", ("rr0", r)], writes=["gone"])
                        P.op("dve", lambda e: e.tensor_scalar(mneg[:, :, 0], gone[:], -1.0, 1e30, op0=ALU.add, op1=ALU.mult), reads=["gone"], writes=["mneg"])
                        P.op("dve", lambda e: e.tensor_tensor(elm[:], lg[:, 4:36].rearrange("p (a b) -> p a b", a=4), mneg[:].to_broadcast([128, 4, 8]), op=ALU.add),
                             reads=["lg", "mneg"], writes=[("elm", r)])
                        P.op("dve", lambda e: e.reduce_max(rr[:, 4:5], elm_f, axis=AX.X), reads=[("elm", r)], writes=[("rr4", r)])
                        P.op("dve", lambda e: e.tensor_scalar(oh1[:], elm_f, rr[:, 4:5], None, op0=ALU.is_equal), reads=[("elm", r), ("rr4", r)], writes=[o1n])

                    def L2a2(t):
                        gt = s * NT + t
                        r = t % 2
                        rr = rrs[r]
                        elm = elms[r]
                        elm_f = elm[:].rearrange("p a b -> p (a b)")
                        oh1, oh2, Mb = oh1s[t % 3], oh2s[r], Mbs[r]
                        o1n, o2n, mbn = "oh1_%d" % (t % 3), "oh2_%d" % r, "Mb%d" % r
                        P.op("dve", lambda e: e.scalar_tensor_tensor(elm2[:], oh1[:], -1e30, elm_f, op0=ALU.mult, op1=ALU.add), reads=[o1n, ("elm", r)], writes=["elm2"])
                        P.op("dve", lambda e: e.reduce_max(rr[:, 5:6], elm2[:], axis=AX.X), reads=["elm2"], writes=[("rr5", r)])
                        P.op("dve", lambda e: e.tensor_scalar(oh2[:], elm2[:], rr[:, 5:6], None, op0=ALU.is_equal), reads=["elm2", ("rr5", r)], writes=[o2n])
                        P.op("dve", lambda e: e.tensor_tensor(rr[:, 6:7], rr[:, 5:6], rr[:, 4:5], op=ALU.subtract), reads=[("rr4", r), ("rr5", r)], writes=[("rr6", r)])
                        P.op("act", lambda e: e.activation(out=rr[:, 7:8], in_=rr[:, 6:7], func=AF.Exp), reads=[("rr6", r)], writes=[("rr7", r)])
                        P.op("dve", lambda e: e.tensor_scalar(rr[:, 8:9], rr[:, 7:8], 1.0, None, op0=ALU.add), reads=[("rr7", r)], writes=[("rr8", r)])
                        P.op("dve", lambda e: e.reciprocal(rr[:, 9:10], rr[:, 8:9]), reads=[("rr8", r)], writes=[("rr9", r)])
                        P.op("dve", lambda e: e.tensor_tensor(wts[:, gt, 0:1], rr[:, 9:10], rr[:, 3:4], op=ALU.mult), reads=[("rr9", r), ("rr3", r)], writes=[("wts", gt, 0)])
                        P.op("dve", lambda e: e.tensor_tensor(rr[:, 10:11], rr[:, 7:8], rr[:, 9:10], op=ALU.mult), reads=[("rr7", r), ("rr9", r)], writes=[("rr10", r)])
                        P.op("dve", lambda e: e.tensor_tensor(wts[:, gt, 1:2], rr[:, 10:11], rr[:, 3:4], op=ALU.mult), reads=[("rr10", r), ("rr3", r)], writes=[("wts", gt, 1)])
                        P.op("dve", lambda e: e.tensor_tensor(Mb[:], oh1[:], oh2[:], op=ALU.add), reads=[o1n, o2n], writes=[mbn])

                    def L2b(t):
                        gt = s * NT + t
                        r = t % 2
                        r3 = t % 3
                        oh1, oh2, Mb = oh1s[t % 3], oh2s[r], Mbs[r]
                        o1n, o2n, mbn = "oh1_%d" % (t % 3), "oh2_%d" % r, "Mb%d" % r
                        r4 = t % 4
                        P.op("pe", lambda e: e.matmul(B[5][:, 0:32], lhsT=tri_b, rhs=Mb[:], start=True, stop=True, skip_group_check=True),
                             reads=[mbn, "cm_b"], writes=[BN[5]])
                        P.op("pe", lambda e: e.matmul(B[5][:, 32:64], lhsT=ones_b, rhs=Mb[:], start=False, stop=True, skip_group_check=True),
                             reads=[mbn, "cm_b"], writes=[BN[5]])
                        P.op("dve", lambda e: e.tensor_tensor(posin[:], B[5][:, 0:32], cnt[:], op=ALU.add), reads=[BN[5], "cnt"], writes=["posin"])
                        P.op("dve", lambda e: e.tensor_scalar(valid[:], posin[:], CAP - 0.5, None, op0=ALU.is_lt), reads=["posin"], writes=["valid"])
                        P.op("dve", lambda e: e.tensor_tensor(smv[:], posin[:], eoffmd, op=ALU.add), reads=["posin", "cv"], writes=["smv"])
                        P.op("dve", lambda e: e.tensor_tensor(smv[:], smv[:], valid[:], op=ALU.mult), reads=["smv", "valid"], writes=["smv"])
                        for k_, ohk, ohn in ((0, oh1, o1n), (1, oh2, o2n)):
                            P.op("dve", lambda e: e.tensor_tensor(tmp32[:], smv[:], ohk[:], op=ALU.mult), reads=["smv", ohn], writes=["tmp32"])
                            P.op("dve", lambda e: e.reduce_sum(slots_f[:, gt, k_:k_ + 1], tmp32[:], axis=AX.X), reads=["tmp32"], writes=[("slots_f", gt, k_)])
                        P.op("dve", lambda e: e.tensor_tensor(cnt[:], cnt[:], B[5][:, 32:64], op=ALU.add), reads=["cnt", BN[5]], writes=["cnt"])
                        P.op("dve", lambda e: e.tensor_scalar(slots_f[:, gt, :], slots_f[:, gt, :], float(DUMP), None, op0=ALU.add),
                             reads=[("slots_f", gt, 0), ("slots_f", gt, 1)], writes=[("slots_f2", gt)])
                        P.op("dve", lambda e: e.tensor_copy(slots_i[:, gt, :], slots_f[:, gt, :]), reads=[("slots_f2", gt)], writes=[("slots_i", gt)])
                        for k_ in range(2):
                            P.dma("pool", lambda e: e.indirect_dma_start(
                                out=xs[:, :], out_offset=bass.IndirectOffsetOnAxis(ap=slots_i[:, gt, k_:k_ + 1], axis=0),
                                in_=hbf[r4][:], in_offset=None), reads=["hbf%d" % r4, ("slots_i", gt)] + xs_init, writes=[("xs", gt, k_)])

                    pipeline(NT, [L0a, L0b, L1a, L1b, L2a1, L2a2, L2b])
                    P.barrier()
        NTOT = NSEQ * NT
        if stop in ("ln1",):
            P.dma("sp", lambda e: e.dma_start(out=dbg["slots"][:, :, :], in_=slots_i[:]), writes=["dbg1"])
            P.dma("sp", lambda e: e.dma_start(out=dbg["wts"][:, :, :], in_=wts[:]), writes=["dbg2"])
            P.barrier()
            return nc
        if stop is not None and stop not in ("moe",):
            P.barrier()
            return nc

        xs_all = [("xs", gt, k_) for gt in range(NTOT) for k_ in range(2)]

        with contextlib.ExitStack() as s4:
            NWB = 3
            wg = [sb(s4, "wg%d" % i, [128, 8, D], BF16) for i in range(NWB)]
            wd = [sb(s4, "wd%d" % i, [128, 4, D], BF16) for i in range(NWB)]
            xsb3 = [sb(s4, "xsb3_%d" % i, [128, NBLK, D], BF16) for i in range(2)]
            xsT3 = [sb(s4, "xsT3_%d" % i, [128, 8, CAP], BF16) for i in range(2)]
            sg = [sb(s4, "sg%d" % i, [128, CAP], F32) for i in range(2)]
            actT3 = [sb(s4, "actT3_%d" % i, [128, 4, CAP], BF16) for i in range(2)]
            yb = [sb(s4, "yb%d" % i, [128, D], F32) for i in range(2)]

            def load_expert(e_):
                b = e_ % NWB
                for kc2 in range(2):
                    P.dma("sp", lambda e: e.dma_start(out=wg[b][:, kc2 * 4:(kc2 + 1) * 4, :],
                                                      in_=wgu_bf[e_].rearrange("(kc k) n -> k kc n", k=128)[:, kc2 * 4:(kc2 + 1) * 4, :]),
                          reads=[("wcv", e_, kc2)], writes=[("wg%d" % b, kc2)])
                P.dma("sp", lambda e: e.dma_start(out=wd[b][:], in_=wdn_bf[e_].rearrange("(kc k) n -> k kc n", k=128)),
                      reads=[("wcv", e_, 2)], writes=["wd%d" % b])

            for e0 in range(min(NWB, NE)):
                load_expert(e0)
            def E0(e_):
                r = e_ % 2
                for blk in range(NBLK):
                    row0 = e_ * CAP + blk * 128
                    P.dma("sp", lambda e: e.dma_start(out=xsb3[r][:, blk, :], in_=xs[row0:row0 + 128, :]), reads=xs_all, writes=[("xsb3", r, blk)])

            def E1(e_):
                r = e_ % 2
                for blk in range(NBLK):
                    bt = (e_ * NBLK + blk) % 2
                    for kc in range(8):
                        P.op("pe", lambda e: e.transpose(bank_bf(bt)[:, kc * 128:(kc + 1) * 128], xsb3[r][:, blk, kc * 128:(kc + 1) * 128], ident_b),
                             reads=[("xsb3", r, blk), "cm_b"], writes=[BN[bt]])
                    evac(xsT3[r][:, :, blk * 128:(blk + 1) * 128], bank_bf(bt)[:, :].rearrange("p (a b) -> p a b", a=8),
                         reads=[BN[bt]], writes=[("xsT3", r, blk)])

            def E2(e_):
                r = e_ % 2
                eb = e_ % NWB
                xr = [("xsT3", r, blk) for blk in range(NBLK)]
                for fc in range(4):
                    bg, bu = ((2, 3), (4, 5))[fc % 2]
                    for col0, bk in ((fc * 128, bg), (512 + fc * 128, bu)):
                        for kc in range(8):
                            P.op("pe", lambda e: e.matmul(B[bk][:, 0:CAP], lhsT=wg[eb][:, kc, col0:col0 + 128], rhs=xsT3[r][:, kc, :],
                                                          start=(kc == 0), stop=(kc == 7)),
                                 reads=xr + [("wg%d" % eb, kc // 4)], writes=[BN[bk]])
                    q_ = fc % 2
                    P.op("act", lambda e: e.activation(out=sg[q_][:], in_=B[bg][:, 0:CAP], func=AF.Silu), reads=[BN[bg]], writes=["sg%d" % q_])
                    P.op("dve", lambda e: e.tensor_tensor(actT3[r][:, fc, :], B[bu][:, 0:CAP], sg[q_][:], op=ALU.mult),
                         reads=[BN[bu], "sg%d" % q_], writes=[("actT3", r, fc)])

            def E3(e_):
                r = e_ % 2
                eb = e_ % NWB
                ar = [("actT3", r, fc) for fc in range(4)]
                for blk in range(NBLK):
                    q_ = (e_ * NBLK + blk) % 2
                    row0 = e_ * CAP + blk * 128
                    for half, bk in ((0, 6), (1, 7)):
                        for fc in range(4):
                            P.op("pe", lambda e: e.matmul(B[bk][:], lhsT=actT3[r][:, fc, blk * 128:(blk + 1) * 128], rhs=wd[eb][:, fc, half * 512:(half + 1) * 512],
                                                          start=(fc == 0), stop=(fc == 3)),
                                 reads=ar + ["wd%d" % eb], writes=[BN[bk]])
                        evac(yb[q_][:, half * 512:(half + 1) * 512], B[bk][:], reads=[BN[bk]], writes=[("yb%d" % q_, half)])
                    P.dma("pool", lambda e: e.dma_start(out=ys[row0:row0 + 128, :], in_=yb[q_][:]),
                          reads=[("yb%d" % q_, 0), ("yb%d" % q_, 1)], writes=[("ys", e_, blk)])
                if e_ + NWB < NE:
                    load_expert(e_ + NWB)

            pipeline(NE, [E0, E1, E2, E3])
            P.barrier()
        ys_all = [("ys", e_, blk) for e_ in range(NE) for blk in range(NBLK)] + ["ys_dump"]
        if stop == "moe":
            P.dma("sp", lambda e: e.dma_start(out=dbg["ys"][:, :], in_=ys[:, :]), writes=["dbg1"])
            P.dma("sp", lambda e: e.dma_start(out=dbg["slots"][:, :, :], in_=slots_i[:]), writes=["dbg2"])
            P.dma("sp", lambda e: e.dma_start(out=dbg["wts"][:, :, :], in_=wts[:]), writes=["dbg3"])
            P.barrier()
            return nc

        with contextlib.ExitStack() as s5:
            g2 = sb(s5, "g2", [128, D], F32)
            b2 = sb(s5, "b2", [128, D], F32)
            y1 = [sb(s5, "y1_%d" % i, [128, D], F32) for i in range(3)]
            y2 = [sb(s5, "y2_%d" % i, [128, D], F32) for i in range(3)]
            ht = [sb(s5, "ht%d" % i, [128, D], F32) for i in range(3)]
            za = [sb(s5, "za%d" % i, [128, D], F32) for i in range(2)]
            zc = [sb(s5, "zc%d" % i, [128, D], F32) for i in range(2)]
            oo = [sb(s5, "oo%d" % i, [128, D], F32) for i in range(2)]
            stats2 = [sb(s5, "stats2_%d" % i, [128, 2, 6], F32) for i in range(2)]
            mv2 = [sb(s5, "mv2_%d" % i, [128, 4], F32) for i in range(2)]
            nm2 = sb(s5, "nm2", [128, 2], F32)
            P.dma("sp", lambda e: e.dma_start(out=g2[:], in_=ln2_g[0:1, :].to_broadcast([128, D])), writes=["g2"])
            P.dma("sp", lambda e: e.dma_start(out=b2[:], in_=ln2_b[0:1, :].to_broadcast([128, D])), writes=["b2"])

            def c0(gt):
                r = gt % 3
                P.dma("pool", lambda e: e.indirect_dma_start(
                    out=y1[r][:], out_offset=None, in_=ys[:, :],
                    in_offset=bass.IndirectOffsetOnAxis(ap=slots_i[:, gt, 0:1], axis=0)), reads=ys_all, writes=["y1_%d" % r])
                P.dma("pool", lambda e: e.indirect_dma_start(
                    out=y2[r][:], out_offset=None, in_=ys[:, :],
                    in_offset=bass.IndirectOffsetOnAxis(ap=slots_i[:, gt, 1:2], axis=0)), reads=ys_all, writes=["y2_%d" % r])
                P.dma("sp", lambda e: e.dma_start(out=ht[r][:], in_=h_scr[gt * 128:(gt + 1) * 128, :]), reads=[("h_scr", gt)], writes=["ht%d" % r])

            def c1(gt):
                r = gt % 3
                q_ = gt % 2
                P.op("act", lambda e: e.activation(out=za[q_][:], in_=ht[r][:], func=AF.Copy, scale=ALPHA), reads=["ht%d" % r], writes=["za%d" % q_])
                P.op("dve", lambda e: e.scalar_tensor_tensor(za[q_][:], y1[r][:], wts[:, gt, 0:1], za[q_][:], op0=ALU.mult, op1=ALU.add),
                     reads=["y1_%d" % r, "za%d" % q_], writes=["za%d" % q_])
                P.op("dve", lambda e: e.scalar_tensor_tensor(zc[q_][:], y2[r][:], wts[:, gt, 1:2], za[q_][:], op0=ALU.mult, op1=ALU.add),
                     reads=["y2_%d" % r, "za%d" % q_], writes=["zc%d" % q_])
                for hf in range(2):
                    P.op("dve", lambda e: e.bn_stats(stats2[q_][:, hf, :], zc[q_][:, hf * 512:(hf + 1) * 512]), reads=["zc%d" % q_], writes=[("stats2", q_, hf)])
                P.op("dve", lambda e: e.bn_aggr(mv2[q_][:, 0:2], stats2[q_][:].rearrange("p a b -> p (a b)")),
                     reads=[("stats2", q_, 0), ("stats2", q_, 1)], writes=[("mv2a", q_)])
                P.op("dve", lambda e: e.tensor_scalar(mv2[q_][:, 2:3], mv2[q_][:, 1:2], LN_EPS, None, op0=ALU.add), reads=[("mv2a", q_)], writes=[("mv2b", q_)])

            def c2(gt):
                q_ = gt % 2
                s_, t_ = gt // NT, gt % NT
                P.op("pool", lambda e: e.tensor_tensor(mv2[q_][:, 3:4], mv2[q_][:, 2:3], negh[:, 0:1], op=ALU.pow), reads=[("mv2b", q_), "negh"], writes=[("mv2c", q_)])
                P.op("dve", lambda e: e.scalar_tensor_tensor(nm2[:, q_:q_ + 1], mv2[q_][:, 0:1], -1.0, mv2[q_][:, 3:4], op0=ALU.mult, op1=ALU.mult),
                     reads=[("mv2a", q_), ("mv2c", q_)], writes=[("nm2", q_)])
                P.op("act", lambda e: e.activation(out=oo[q_][:], in_=zc[q_][:], func=AF.Identity, scale=mv2[q_][:, 3:4], bias=nm2[:, q_:q_ + 1]),
                     reads=["zc%d" % q_, ("mv2c", q_), ("nm2", q_)], writes=["oo%d" % q_])
                P.op("dve", lambda e: e.tensor_tensor(oo[q_][:], oo[q_][:], g2[:], op=ALU.mult), reads=["oo%d" % q_, "g2"], writes=["oo%d" % q_])
                P.op("dve", lambda e: e.tensor_tensor(oo[q_][:], oo[q_][:], b2[:], op=ALU.add), reads=["oo%d" % q_, "b2"], writes=["oo%d" % q_])
                P.dma("sp", lambda e: e.dma_start(out=out[s_, t_ * 128:(t_ + 1) * 128, :], in_=oo[q_][:]), reads=["oo%d" % q_], writes=[("out", gt)])

            pipeline(NTOT, [c0, c1, c2])
            P.barrier()
    return nc


def _in_maps(inputs):
    f = lambda a: np.ascontiguousarray(np.asarray(a))
    cmat, cvec = _consts()
    common = {
        "w_in": f(inputs["w_in"][0]),
        "gate_bias": f(inputs["gate_bias"][0].reshape(16, 128).T),
        "lam_vecs": f(inputs["lam_vecs"][0].reshape(1, 256)),
        "subln_gain": f(inputs["subln_gain"][0].reshape(1, 128)),
        "w_pool": f(inputs["w_pool"][0]),
        "pool_scale": f(inputs["pool_scale"][0].reshape(4, 128).T),
        "w_a": f(inputs["w_branch_a"][0]),
        "w_b": f(inputs["w_branch_b"][0]),
        "w_out": f(inputs["w_out"][0]),
        "ln1_g": f(inputs["ln1_gain"][0].reshape(1, D)),
        "ln1_b": f(inputs["ln1_bias"][0].reshape(1, D)),
        "w_rt": f(np.concatenate([inputs["w_router_group"][0], inputs["w_router_expert"][0]], axis=1)),
        "b_rt": f(np.concatenate([inputs["b_router_group"][0], inputs["b_router_expert"][0]]).reshape(1, 36)),
        "w_gu": f(inputs["w_gate_up"][0]),
        "w_dn": f(inputs["w_down"][0]),
        "ln2_g": f(inputs["ln2_gain"][0].reshape(1, D)),
        "ln2_b": f(inputs["ln2_bias"][0].reshape(1, D)),
        "cmat": cmat,
        "cvec": cvec,
    }
    maps = []
    xx = np.asarray(inputs["x"])
    pp = np.asarray(inputs["positions"]).astype(np.int32)
    for c in range(NCORES):
        m = dict(common)
        m["x"] = f(xx[c * NSEQ:(c + 1) * NSEQ])
        m["pos"] = f(pp[c * NSEQ:(c + 1) * NSEQ])
        maps.append(m)
    return maps


def kernel(**inputs):
    nc = build_nc()
    res = run_bass_kernel_spmd(nc, _in_maps(inputs), core_ids=list(range(NCORES)))
    return np.concatenate([np.asarray(r["out"]) for r in res.results], axis=0).astype(np.float32)
```

```python
import contextlib
import math
import numpy as np
import concourse.bass as bass
import concourse.mybir as mybir
from concourse.bass_utils import run_bass_kernel_spmd

F32 = mybir.dt.float32
BF16 = mybir.dt.bfloat16
I32 = mybir.dt.int32
AF = mybir.ActivationFunctionType
ALU = mybir.AluOpType
AX = mybir.AxisListType

NCORES = 8
NSEQ = 2
S = 2048
D = 1024
H = 8
NT = S // 128
NE = 32
CAP = 384
NBLK = CAP // 128
NSLOT = NE * CAP + 128
DUMP = NE * CAP
ALPHA = 2.0 ** 0.25
LAMBDA_INIT = 0.2
LN_EPS = 1e-5
RMS_EPS = 1e-5
IN_W = 5632
TWO_PI = 2.0 * math.pi
C1 = 6.28125
C2 = TWO_PI - C1

SEM_LIMIT = 30000
DMA_RING = 6
DMA_RING_Q = {"poolc": 2}
CONV_EVERY = 13


class Prog:
    def __init__(self, nc, stack):
        self.nc = nc
        self.stack = stack
        self.eng = {"pe": nc.tensor, "dve": nc.vector, "act": nc.scalar,
                    "pool": nc.gpsimd, "sp": nc.sync, "poolc": nc.gpsimd}
        self.cnt = {}
        self.sems = {}
        self.seen = {}
        self.res = {}
        self.dma_n = {}
        self.dma_ring = {}
        self.dma_ring_cnt = {}
        self.ninst = 0
        self.recording = None
        for s in ["pe", "dve", "act", "pool"]:
            self.cnt[s] = 0
            self.sems[s] = []
            self._new_sem(s)

    def _alloc_sem(self, name):
        return self.stack.enter_context(self.nc.semaphore(name))

    def _new_sem(self, s):
        sem = self._alloc_sem("s_%s_%d" % (s, len(self.sems[s])))
        self.sems[s].append((sem, self.cnt[s]))

    def _wait(self, engname, tok):
        stream, idx = tok
        key = (engname, stream)
        if self.seen.get(key, 0) >= idx:
            return
        if stream.startswith("dma"):
            self.seen[key] = idx
            q, r = stream.split(":")[1:]
            self.eng[engname].wait_ge(self.dma_ring[q][int(r)], 16 * idx)
            self.ninst += 1
            return
        if stream == engname and engname == "pe":
            return
        self.seen[key] = idx
        for sem, base in reversed(self.sems[stream]):
            if idx > base:
                self.eng[engname].wait_ge(sem, idx - base)
                self.ninst += 1
                return
        raise RuntimeError("bad token")

    def _deps(self, reads, writes):
        deps = []
        for r in reads:
            st = self.res.get(r)
            if st and st[0] is not None:
                deps.append(st[0])
        for w in writes:
            st = self.res.get(w)
            if st:
                if st[0] is not None:
                    deps.append(st[0])
                deps.extend(st[1])
        return deps

    def _commit(self, tok, reads, writes):
        for r in reads:
            st = self.res.setdefault(r, [None, []])
            st[1].append(tok)
        for w in writes:
            self.res[w] = [tok, []]

    def _excl(self, reads, writes):
        r2 = [r for r in reads if not (isinstance(r, str) and r.startswith("B") and r[1:].isdigit())]
        w2 = list(writes) + [r for r in reads if (isinstance(r, str) and r.startswith("B") and r[1:].isdigit())]
        return r2, w2

    def op(self, engname, fn, reads=(), writes=()):
        if self.recording is not None:
            rec = _Rec()
            fn(rec)
            self.recording.append(("op", engname, rec.call, tuple(reads), tuple(writes)))
            return None
        reads, writes = self._excl(reads, writes)
        for d in self._deps(reads, writes):
            self._wait(engname, d)
        inst = fn(self.eng[engname])
        if self.cnt[engname] - self.sems[engname][-1][1] >= SEM_LIMIT:
            self._new_sem(engname)
        sem, base = self.sems[engname][-1]
        inst.then_inc(sem, 1)
        self.cnt[engname] += 1
        self.ninst += 1
        tok = (engname, self.cnt[engname])
        self._commit(tok, reads, writes)
        return tok

    def dma(self, q, fn, reads=(), writes=()):
        if self.recording is not None:
            rec = _Rec()
            fn(rec)
            self.recording.append(("dma", q, rec.call, tuple(reads), tuple(writes)))
            return None
        nring = DMA_RING_Q.get(q, DMA_RING)
        if q not in self.dma_ring:
            self.dma_ring[q] = [self._alloc_sem("d_%s_%d" % (q, i)) for i in range(nring)]
            self.dma_ring_cnt[q] = [0] * nring
            self.dma_n[q] = 0
        r = self.dma_n[q] % nring
        self.dma_n[q] += 1
        stream = "dma:%s:%d" % (q, r)
        if self.dma_ring_cnt[q][r] > 0:
            self._wait(q, (stream, self.dma_ring_cnt[q][r]))
        for d in self._deps(reads, writes):
            self._wait(q, d)
        inst = fn(self.eng[q])
        inst.then_inc(self.dma_ring[q][r], 16)
        self.dma_ring_cnt[q][r] += 1
        self.ninst += 1
        tok = (stream, self.dma_ring_cnt[q][r])
        self._commit(tok, reads, writes)
        return tok

    def record(self, fn, *args):
        assert self.recording is None
        self.recording = []
        try:
            fn(*args)
            return self.recording
        finally:
            self.recording = None

    def replay(self, item):
        kind, eng, call, reads, writes = item
        name, a, kw = call
        f = lambda e: getattr(e, name)(*a, **kw)
        if kind == "op":
            return self.op(eng, f, reads, writes)
        return self.dma(eng, f, reads, writes)

    def barrier(self):
        toks = []
        for s in ["pe", "dve", "act", "pool"]:
            if self.cnt[s] > 0:
                toks.append((s, self.cnt[s]))
        for q in self.dma_ring:
            for r in range(len(self.dma_ring[q])):
                if self.dma_ring_cnt[q][r] > 0:
                    toks.append(("dma:%s:%d" % (q, r), self.dma_ring_cnt[q][r]))
        for e in ["pe", "dve", "act", "pool", "sp"]:
            for t in toks:
                if e == "pe" and t[0] == "pe":
                    continue
                self._wait(e, t)
        self.res = {}


class _Rec:
    def __init__(self):
        self.call = None

    def __getattr__(self, name):
        def f(*a, **kw):
            self.call = (name, a, kw)
            return self
        return f


def _consts():
    ident = np.eye(128, dtype=np.float32)
    prot = np.zeros((128, 128), np.float32)
    for pp in range(128):
        if (pp % 64) < 32:
            prot[pp + 32, pp] = -1.0
        else:
            prot[pp - 32, pp] = 1.0
    tri = np.triu(np.ones((128, 128), np.float32), 1)
    ones = np.ones((128, 128), np.float32)
    cmat = np.stack([ident, prot, tri, ones], axis=1)
    half = 32
    inv_freq = (10000.0 ** (-np.arange(half, dtype=np.float32) * np.float32(2.0 / 64))).astype(np.float32)
    cvec = np.zeros((128, 128), np.float32)
    cvec[:, 0] = inv_freq[np.arange(128) % 32]
    for g, w in enumerate((2, 4, 8, 16)):
        for t in range(16):
            cvec[:, 16 + g * 16 + t] = (w / (t + 1.0)) if t < w - 1 else 1.0
    cvec[:, 80:112] = (np.arange(NE, dtype=np.float32) * CAP - DUMP)[None, :]
    cvec[64:, 112] = -30000.0
    return np.ascontiguousarray(cmat), cvec


def build_nc(stop=None):
    nc = bass.Bass("TRN2", target_bir_lowering=False)

    def dram(name, shape, dt, kind="ExternalInput"):
        return nc.dram_tensor(name, shape, dt, kind=kind).ap()

    x = dram("x", [NSEQ, S, D], F32)
    pos = dram("pos", [NSEQ, S], I32)
    w_in = dram("w_in", [D, IN_W], F32)
    gate_bias = dram("gate_bias", [128, 16], F32)
    lam_vecs = dram("lam_vecs", [1, 256], F32)
    subln_gain = dram("subln_gain", [1, 128], F32)
    w_pool = dram("w_pool", [4, 128, 128], F32)
    pool_scale = dram("pool_scale", [128, 4], F32)
    w_a = dram("w_a", [D, D], F32)
    w_b = dram("w_b", [512, D], F32)
    w_out = dram("w_out", [D, D], F32)
    ln1_g = dram("ln1_g", [1, D], F32)
    ln1_b = dram("ln1_b", [1, D], F32)
    w_rt = dram("w_rt", [D, 36], F32)
    b_rt = dram("b_rt", [1, 36], F32)
    w_gu = dram("w_gu", [NE, D, D], F32)
    w_dn = dram("w_dn", [NE, 512, D], F32)
    ln2_g = dram("ln2_g", [1, D], F32)
    ln2_b = dram("ln2_b", [1, D], F32)
    cmat = dram("cmat", [128, 4, 128], F32)
    cvec = dram("cvec", [128, 128], F32)
    out = dram("out", [NSEQ, S, D], F32, kind="ExternalOutput")
    h_scr = dram("h_scr", [NSEQ * S, D], F32, kind="Internal")
    xs = dram("xs", [NSLOT, D], BF16, kind="Internal")
    ys = dram("ys", [NSLOT, D], F32, kind="Internal")
    wgu_bf = dram("wgu_bf", [NE, D, D], BF16, kind="Internal")
    wdn_bf = dram("wdn_bf", [NE, 512, D], BF16, kind="Internal")
    dbg = {}
    if stop in ("attn", "a1", "rope", "v", "qk"):
        dbg["oT"] = dram("dbg_oT", [NSEQ, 128, H, S], BF16, kind="ExternalOutput")
    if stop == "mix":
        dbg["mergedT"] = dram("dbg_mergedT", [NSEQ, 128, 8, S], BF16, kind="ExternalOutput")
    if stop in ("ln1",):
        dbg["h"] = dram("dbg_h", [NSEQ * S, D], F32, kind="ExternalOutput")
    if stop in ("ln1", "moe"):
        dbg["slots"] = dram("dbg_slots", [128, 32, 2], I32, kind="ExternalOutput")
        dbg["wts"] = dram("dbg_wts", [128, 32, 2], F32, kind="ExternalOutput")
    if stop == "moe":
        dbg["ys"] = dram("dbg_ys", [NSLOT, D], F32, kind="ExternalOutput")

    w_in_k = w_in.rearrange("(kc k) n -> k kc n", k=128)
    w_a_k = w_a.rearrange("(kc k) n -> k kc n", k=128)
    w_b_k = w_b.rearrange("(kc k) n -> k kc n", k=128)
    w_out_k = w_out.rearrange("(kc k) n -> k kc n", k=128)
    w_rt_k = w_rt.rearrange("(kc k) n -> k kc n", k=128)

    with contextlib.ExitStack() as st:
        P = Prog(nc, st)

        uniq = [0]

        def sb(stack, name, shape, dt):
            uniq[0] += 1
            return stack.enter_context(nc.sbuf_tensor("%s_u%d" % (name, uniq[0]), shape, dt))

        B = [st.enter_context(nc.psum_tensor("bank%d" % i, [128, 512], F32)) for i in range(8)]
        BN = ["B%d" % i for i in range(8)]

        def bank_bf(i):
            return B[i][:].bitcast(BF16)

        cm_f = sb(st, "cm_f", [128, 4, 128], F32)
        cm_b = sb(st, "cm_b", [128, 4, 128], BF16)
        cv = sb(st, "cv", [128, 128], F32)
        lvb = sb(st, "lvb", [128, 256], F32)
        gainb = sb(st, "gainb", [128, 128], F32)
        gbias = sb(st, "gbias", [128, 16], F32)
        pscale = sb(st, "pscale", [128, 4], F32)
        wpool_b = sb(st, "wpool_b", [128, 4, 128], BF16)
        wrt = sb(st, "wrt", [128, 8, 36], F32)
        brt = sb(st, "brt", [128, 36], F32)
        small = sb(st, "small", [128, 16], F32)
        negh = sb(st, "negh", [128, 4], F32)
        cnt = sb(st, "cnt", [128, 32], F32)
        slots_f = sb(st, "slots_f", [128, 32, 2], F32)
        slots_i = sb(st, "slots_i", [128, 32, 2], I32)
        wts = sb(st, "wts", [128, 32, 2], F32)
        zrow = sb(st, "zrow", [128, 1024], F32)
        zb3 = sb(st, "zb3", [128, 1, 1024], BF16)
        xT = sb(st, "xT", [128, 8, S], BF16)
        oT = sb(st, "oT", [128, 8, S], BF16)

        ident_f = cm_f[:, 0, :]
        ident_b = cm_b[:, 0, :]
        prot_b = cm_b[:, 1, :]
        tri_b = cm_b[:, 2, :]
        ones_b = cm_b[:, 3, :]
        invf = cv[:, 0:1]
        eoffmd = cv[:, 80:112]
        lamneg = small[:, 0:1]

        P.dma("sp", lambda e: e.dma_start(out=cm_f[:], in_=cmat[:, :, :]), writes=["cm_f"])
        P.dma("pool", lambda e: e.dma_start(out=cm_b[:], in_=cmat[:, :, :]), writes=["cm_b"])
        P.dma("sp", lambda e: e.dma_start(out=cv[:], in_=cvec[:, :]), writes=["cv"])
        P.dma("sp", lambda e: e.dma_start(out=lvb[:], in_=lam_vecs[0:1, :].to_broadcast([128, 256])), writes=["lvb"])
        P.dma("sp", lambda e: e.dma_start(out=gainb[:], in_=subln_gain[0:1, :].to_broadcast([128, 128])), writes=["gainb"])
        P.dma("sp", lambda e: e.dma_start(out=brt[:], in_=b_rt[0:1, :].to_broadcast([128, 36])), writes=["brt"])
        P.dma("sp", lambda e: e.dma_start(out=gbias[:], in_=gate_bias[:, :]), writes=["gbias"])
        P.dma("sp", lambda e: e.dma_start(out=pscale[:], in_=pool_scale[:, :]), writes=["pscale"])
        P.dma("pool", lambda e: e.dma_start(out=wpool_b[:], in_=w_pool.rearrange("g c d -> c g d")), writes=["wpool_b"])
        P.dma("sp", lambda e: e.dma_start(out=wrt[:], in_=w_rt_k), writes=["wrt"])
        P.op("dve", lambda e: e.memset(cnt[:], 0.0), writes=["cnt"])
        P.op("dve", lambda e: e.memset(slots_i[:], 0), writes=["slots_i_init"])
        P.op("dve", lambda e: e.memset(wts[:], 0.0), writes=["wts_init"])
        P.op("dve", lambda e: e.memset(zrow[:], 0.0), writes=["zrow"])
        P.op("dve", lambda e: e.memset(negh[:], -0.5), writes=["negh"])
        P.op("dve", lambda e: e.tensor_scalar(gainb[:], gainb[:], 1.0 - LAMBDA_INIT, None, op0=ALU.mult),
             reads=["gainb"], writes=["gainb"])
        P.op("dve", lambda e: e.memset(zb3[:], 0.0), writes=["zb3"])
        xs_v = xs.rearrange("(n p) d -> p n d", p=128)
        n_tot = NSLOT // 128
        xs_init = [("xs_init", n0) for n0 in range(0, n_tot, 25)]

        def emit_xs_zero_fill():
            for n0 in range(0, n_tot, 25):
                n1 = min(n_tot, n0 + 25)
                P.dma("sp", lambda e: e.dma_start(out=xs_v[:, n0:n1, :], in_=zb3[:].to_broadcast([128, n1 - n0, 1024])),
                      reads=["zb3"], writes=[("xs_init", n0)])

        P.dma("sp", lambda e: e.dma_start(out=ys[DUMP:DUMP + 128, :], in_=zrow[:]), reads=["zrow"], writes=["ys_dump"])
        P.op("dve", lambda e: e.tensor_tensor(lvb[:, 0:64], lvb[:, 0:64], lvb[:, 64:128], op=ALU.mult),
             reads=["lvb"], writes=["lvb"])
        P.op("dve", lambda e: e.tensor_tensor(lvb[:, 128:192], lvb[:, 128:192], lvb[:, 192:256], op=ALU.mult),
             reads=["lvb"], writes=["lvb"])
        P.op("dve", lambda e: e.reduce_sum(small[:, 1:2], lvb[:, 0:64], axis=AX.X), reads=["lvb"], writes=["small"])
        P.op("dve", lambda e: e.reduce_sum(small[:, 2:3], lvb[:, 128:192], axis=AX.X), reads=["lvb", "small"], writes=["small"])
        P.op("act", lambda e: e.activation(out=small[:, 3:5], in_=small[:, 1:3], func=AF.Exp), reads=["small"], writes=["small"])
        P.op("dve", lambda e: e.tensor_tensor(small[:, 5:6], small[:, 4:5], small[:, 3:4], op=ALU.subtract),
             reads=["small"], writes=["small"])
        P.op("dve", lambda e: e.tensor_scalar(small[:, 0:1], small[:, 5:6], -LAMBDA_INIT, None, op0=ALU.add),
             reads=["small"], writes=["small"])

        ev_flip = [0]

        def evac(out_ap, in_ap, reads, writes, eng=None):
            if eng is None:
                eng = "act" if ev_flip[0] % 2 == 0 else "dve"
                ev_flip[0] += 1
            if eng == "act":
                return P.op("act", lambda e: e.activation(out=out_ap, in_=in_ap, func=AF.Copy), reads=reads, writes=writes)
            return P.op("dve", lambda e: e.tensor_copy(out_ap, in_ap), reads=reads, writes=writes)

        def pipeline(n_items, stages):
            for step in range(n_items + len(stages) - 1):
                lists = []
                for si, f in enumerate(stages):
                    k = step - si
                    if 0 <= k < n_items:
                        lists.append(P.record(f, k))
                pos = [0] * len(lists)
                left = sum(len(l) for l in lists)
                while left:
                    for li, l in enumerate(lists):
                        if pos[li] < len(l):
                            P.replay(l[pos[li]])
                            pos[li] += 1
                            left -= 1

        conv_jobs = []
        for e_ in range(NE):
            conv_jobs.append((wgu_bf[e_, 0:512, :], w_gu[e_, 0:512, :], ("wcv", e_, 0)))
            conv_jobs.append((wgu_bf[e_, 512:1024, :], w_gu[e_, 512:1024, :], ("wcv", e_, 1)))
            conv_jobs.append((wdn_bf[e_, :, :], w_dn[e_, :, :], ("wcv", e_, 2)))
        conv_next = [0]
        conv_tick = [0]

        def conv_issue(n=1):
            for _ in range(n):
                if conv_next[0] < len(conv_jobs):
                    o_, i_, nm_ = conv_jobs[conv_next[0]]
                    conv_next[0] += 1
                    P.dma("poolc", lambda e: e.dma_start(out=o_, in_=i_), writes=[nm_])

        for s in range(NSEQ):
            s2 = contextlib.ExitStack()
            with s2:
                V = sb(s2, "V", [128, NT, H, 129], BF16)
                cosT = sb(s2, "cosT", [128, S], F32)
                sinT = sb(s2, "sinT", [128, S], F32)
                with contextlib.ExitStack() as s2a:
                    xt = [sb(s2a, "xt%d" % i, [128, D], F32) for i in range(2)]
                    posi = sb(s2a, "posi", [128, S], I32)
                    ang = sb(s2a, "ang", [128, S], F32)
                    ras = [sb(s2a, "ra%d" % i, [128, S], F32) for i in range(2)]
                    rk = sb(s2a, "rk", [128, S], F32)
                    ki = sb(s2a, "ki", [128, S], I32)
                    wv = sb(s2a, "wv", [128, 8, D], BF16)
                    for kc2 in range(2):
                        P.dma("pool", lambda e: e.dma_start(out=wv[:, kc2 * 4:(kc2 + 1) * 4, :],
                                                            in_=w_in_k[:, kc2 * 4:(kc2 + 1) * 4, 2048:3072]),
                              writes=[("wv", kc2)])
                    P.dma("sp", lambda e: e.dma_start(out=posi[:], in_=pos[s:s + 1, :].to_broadcast([128, S])), writes=["posi"])
                    P.op("pool", lambda e: e.memset(V[:, :, :, 128:129], 1.0), writes=["Vones"])
                    P.op("dve", lambda e: e.tensor_copy(ang[:], posi[:]), reads=["posi"], writes=["ang"])
                    P.op("dve", lambda e: e.tensor_scalar(ang[:], ang[:], invf, None, op0=ALU.mult), reads=["ang", "cv"], writes=["ang"])
                    for which in range(2):
                        ra, ran = ras[which], "ra%d" % which
                        shift = 0.0 if which == 0 else 0.5 * math.pi
                        P.op("dve", lambda e: e.tensor_scalar(ra[:], ang[:], shift, None, op0=ALU.add), reads=["ang"], writes=[ran])
                        P.op("dve", lambda e: e.tensor_scalar(rk[:], ra[:], 1.0 / TWO_PI, None, op0=ALU.mult), reads=[ran], writes=["rk"])
                        P.op("dve", lambda e: e.tensor_copy(ki[:], rk[:]), reads=["rk"], writes=["ki"])
                        P.op("dve", lambda e: e.tensor_copy(rk[:], ki[:]), reads=["ki"], writes=["rk"])
                        P.op("dve", lambda e: e.scalar_tensor_tensor(ra[:], rk[:], -C1, ra[:], op0=ALU.mult, op1=ALU.add),
                             reads=["rk", ran], writes=[ran])
                        P.op("dve", lambda e: e.scalar_tensor_tensor(ra[:], rk[:], -C2, ra[:], op0=ALU.mult, op1=ALU.add),
                             reads=["rk", ran], writes=[ran])
                        P.op("dve", lambda e: e.tensor_scalar(rk[:], ra[:], math.pi, -TWO_PI, op0=ALU.is_gt, op1=ALU.mult),
                             reads=[ran], writes=["rk"])
                        P.op("dve", lambda e: e.tensor_tensor(ra[:], ra[:], rk[:], op=ALU.add), reads=[ran, "rk"], writes=[ran])
                        P.op("dve", lambda e: e.tensor_scalar(rk[:], ra[:], -math.pi, TWO_PI, op0=ALU.is_lt, op1=ALU.mult),
                             reads=[ran], writes=["rk"])
                        P.op("dve", lambda e: e.tensor_tensor(ra[:], ra[:], rk[:], op=ALU.add), reads=[ran, "rk"], writes=[ran])
                        P.op("dve", lambda e: e.tensor_scalar(ra[:], ra[:], 3.1415925, -3.1415925, op0=ALU.min, op1=ALU.max),
                             reads=[ran], writes=[ran])
                    for t in range(NT):
                        xb_ = xt[t % 2]
                        xn = "xt%d" % (t % 2)
                        P.dma("sp", lambda e: e.dma_start(out=xb_[:], in_=x[s, t * 128:(t + 1) * 128, :]), writes=[xn])
                        for hf in range(2):
                            bk = (t * 2 + hf) % 4
                            for j in range(4):
                                kc = hf * 4 + j
                                P.op("pe", lambda e: e.transpose(B[bk][:, j * 128:(j + 1) * 128], xb_[:, kc * 128:(kc + 1) * 128], ident_f),
                                     reads=[xn, "cm_f"], writes=[BN[bk]])
                            evac(xT[:, hf * 4:(hf + 1) * 4, t * 128:(t + 1) * 128],
                                 B[bk][:].rearrange("p (a b) -> p a b", a=4), reads=[BN[bk]], writes=[("xT", t)], eng="act")
                    P.op("act", lambda e: e.activation(out=sinT[:], in_=ras[0][:], func=AF.Sin), reads=["ra0"], writes=["sinT"])
                    P.op("act", lambda e: e.activation(out=cosT[:], in_=ras[1][:], func=AF.Sin), reads=["ra1"], writes=["cosT"])
                    if stop == "a1":
                        P.dma("sp", lambda e: e.dma_start(out=dbg["oT"][s], in_=xT[:]), reads=[("xT", t_) for t_ in range(NT)], writes=["dbg"])
                    for t in range(NT):
                        for hf in range(2):
                            bk = 4 + (t * 2 + hf) % 4
                            for kc in range(8):
                                P.op("pe", lambda e: e.matmul(B[bk][:], lhsT=xT[:, kc, t * 128:(t + 1) * 128],
                                                              rhs=wv[:, kc, hf * 512:(hf + 1) * 512],
                                                              start=(kc == 0), stop=(kc == 7)),
                                     reads=[("xT", t), ("wv", kc // 4)], writes=[BN[bk]])
                            evac(V[:, t, hf * 4:(hf + 1) * 4, 0:128], B[bk][:].rearrange("p (a b) -> p a b", a=4),
                                 reads=[BN[bk]], writes=[("V", t)])
                    P.barrier()
                if s == 0:
                    emit_xs_zero_fill()
                if stop in ("a1", "rope", "v"):
                    P.barrier()
                    continue
                with contextlib.ExitStack() as s2c:
                    wq = [sb(s2c, "wq%d" % i, [128, 8, 128], BF16) for i in range(2)]
                    wk = [sb(s2c, "wk%d" % i, [128, 8, 128], BF16) for i in range(2)]
                    qT = [sb(s2c, "qT%d" % i, [128, S], BF16) for i in range(2)]
                    kz = [[sb(s2c, "kz%d_%d" % (i, m), [128, S], BF16) for m in range(2)] for i in range(2)]
                    qb = [sb(s2c, "qb%d" % i, [128, 512], BF16) for i in range(2)]
                    t1 = [sb(s2c, "t1_%d" % i, [128, 512], F32) for i in range(2)]
                    t2 = [sb(s2c, "t2_%d" % i, [128, 512], F32) for i in range(2)]
                    PT = [sb(s2c, "PT%d" % i, [128, 512], BF16) for i in range(4)]
                    accs = [sb(s2c, "accs%d" % i, [128, 1032], F32) for i in range(2)]
                    rec3 = sb(s2c, "rec3", [128, 4, 2, 1], F32)
                    O4 = sb(s2c, "O4", [128, 4, 128], F32)
                    T4 = sb(s2c, "T4", [128, 4, 128], F32)
                    ss = sb(s2c, "ss", [128, 4], F32)
                    ssv = sb(s2c, "ssv", [128, 4], F32)
                    rstd3 = sb(s2c, "rstd3", [128, 4, 1], F32)
                    onb = [sb(s2c, "onb%d" % i, [128, 4, 128], BF16) for i in range(2)]
                    rope_i = [0]
                    NG = S // 512
                    for i_ in range(2):
                        P.op("pool", lambda e: e.memset(kz[i_][0][64:128, :], 0.0), writes=[("kzz", i_, 0)])
                        P.op("pool", lambda e: e.memset(kz[i_][1][0:64, :], 0.0), writes=[("kzz", i_, 1)])

                    def load_head_w(h):
                        b = h % 2
                        P.dma("pool", lambda e: e.dma_start(out=wq[b][:], in_=w_in_k[:, :, h * 128:(h + 1) * 128]),
                              writes=["wq%d" % b])
                        P.dma("pool", lambda e: e.dma_start(out=wk[b][:], in_=w_in_k[:, :, 1024 + h * 128:1024 + (h + 1) * 128]),
                              writes=["wk%d" % b])

                    def proj_units(h):
                        hb = h % 2
                        units = []
                        for which in range(2):
                            w_t = (wq if which == 0 else wk)[hb]
                            wname = ("wq%d" if which == 0 else "wk%d") % hb
                            for tg in range(NG):
                                st_ = {}

                                def part_a(w_t=w_t, wname=wname, tg=tg, st_=st_):
                                    i = rope_i[0]
                                    rope_i[0] += 1
                                    st_["i"] = i
                                    r = i % 2
                                    pb = 3 if i % 2 == 0 else 7
                                    tsl = slice(tg * 512, (tg + 1) * 512)
                                    for kc in range(8):
                                        P.op("pe", lambda e: e.matmul(B[pb][:], lhsT=w_t[:, kc, :], rhs=xT[:, kc, tsl],
                                                                      start=(kc == 0), stop=(kc == 7)),
                                             reads=[wname], writes=[BN[pb]])
                                    P.op("act", lambda e: e.activation(out=qb[r][:], in_=B[pb][:], func=AF.Copy),
                                         reads=[BN[pb]], writes=["qb%d" % r])
                                    P.op("dve", lambda e: e.tensor_tensor(t1[r][:], B[pb][:], cosT[:, tsl], op=ALU.mult),
                                         reads=[BN[pb], "cosT"], writes=["t1_%d" % r])

                                def part_b(which=which, tg=tg, st_=st_, hb=hb):
                                    i = st_["i"]
                                    r = i % 2
                                    pb = 3 if i % 2 == 0 else 7
                                    tsl = slice(tg * 512, (tg + 1) * 512)
                                    P.op("pe", lambda e: e.matmul(B[pb][:], lhsT=prot_b, rhs=qb[r][:], start=True, stop=True),
                                         reads=["qb%d" % r, "cm_b"], writes=[BN[pb]])
                                    P.op("dve", lambda e: e.tensor_tensor(t2[r][:], B[pb][:], sinT[:, tsl], op=ALU.mult),
                                         reads=[BN[pb], "sinT"], writes=["t2_%d" % r])
                                    if which == 0:
                                        P.op("dve", lambda e: e.tensor_tensor(qT[hb][:, tsl], t1[r][:], t2[r][:], op=ALU.add),
                                             reads=["t1_%d" % r, "t2_%d" % r], writes=[("qT%d" % hb, tg)])
                                    else:
                                        for m in range(2):
                                            ps_ = slice(m * 64, (m + 1) * 64)
                                            P.op("dve", lambda e: e.tensor_tensor(kz[hb][m][ps_, tsl], t1[r][ps_, :], t2[r][ps_, :], op=ALU.add),
                                                 reads=["t1_%d" % r, "t2_%d" % r, ("kzz", hb, m)], writes=[("kz%d_%d" % (hb, m), tg)])
                                units.append(part_a)
                                units.append(part_b)
                        return units

                    LOOK = 2
                    gstep = [0]
                    deferred = []

                    def run_due(force=False):
                        keep = []
                        for due, fn in deferred:
                            if force or due <= gstep[0]:
                                fn()
                            else:
                                keep.append((due, fn))
                        deferred[:] = keep

                    def defer(delay, fn):
                        deferred.append((gstep[0] + delay, fn))

                    grp_i = [0]
                    load_head_w(0)
                    for u in proj_units(0):
                        u()
                    for h in range(H):
                        hb = h % 2
                        if h + 1 < H:
                            load_head_w(h + 1)
                            nxt = proj_units(h + 1)
                        else:
                            nxt = []
                        qn = "qT%d" % hb
                        steps = []
                        for g in range(NG):
                            for j in range(4 * g + 4):
                                for m in range(2):
                                    steps.append((g, j, m))
                        nsteps = len(steps)
                        unit_at = {}
                        if nxt:
                            gap = max(1, (nsteps - 8) // len(nxt))
                            for ui, u in enumerate(nxt):
                                unit_at.setdefault(4 + ui * gap, []).append(u)
                        started = {}
                        pv_pending = []

                        def emit_pv(g, j, m, pt, ptn, q0):
                            for il in range(q0 - 4 * g, 4):
                                a = il * 2 + m
                                ab = 4 + a // 3
                                col = (a % 3) * 129
                                c0 = (il - (q0 - 4 * g)) * 128
                                first = (g, ab) not in started
                                started[(g, ab)] = True
                                P.op("pe", lambda e: e.matmul(B[ab][:, col:col + 129], lhsT=pt[:, c0:c0 + 128],
                                                              rhs=V[:, j, h, :], start=first, stop=(j == 4 * g + il),
                                                              skip_group_check=True),
                                     reads=[ptn, (ptn, "m"), ("V", j), "Vones"], writes=[BN[ab]])
                            if j == 4 * g + 3 and m == 1:
                                emit_group_end(g)

                        def emit_group_end(g, h=h):
                            gi = grp_i[0]
                            grp_i[0] += 1
                            ac = accs[gi % 2]
                            acn = "accs%d" % (gi % 2)
                            evac(ac[:, 0:387], B[4][:, 0:387], reads=[BN[4]], writes=[(acn, 0)], eng="dve")
                            evac(ac[:, 387:774], B[5][:, 0:387], reads=[BN[5]], writes=[(acn, 1)], eng="act")
                            evac(ac[:, 774:1032], B[6][:, 0:258], reads=[BN[6]], writes=[(acn, 2)], eng="dve")
                            acv = ac[:, :].rearrange("p (i m c) -> p i m c", i=4, m=2)
                            acr = [(acn, 0), (acn, 1), (acn, 2)]

                            def norm_1():
                                P.op("dve", lambda e: e.reciprocal(rec3[:, :, :, 0], acv[:, :, :, 128]), reads=acr, writes=["rec"])
                                P.op("dve", lambda e: e.tensor_scalar(rec3[:, :, 1, 0], rec3[:, :, 1, 0], lamneg, None, op0=ALU.mult),
                                     reads=["rec", "small"], writes=["rec"])
                                P.op("dve", lambda e: e.tensor_tensor(O4[:], acv[:, :, 0, 0:128], rec3[:, :, 0, :].to_broadcast([128, 4, 128]), op=ALU.mult),
                                     reads=acr + ["rec"], writes=["O4"])
                                P.op("dve", lambda e: e.tensor_tensor(T4[:], acv[:, :, 1, 0:128], rec3[:, :, 1, :].to_broadcast([128, 4, 128]), op=ALU.mult),
                                     reads=acr + ["rec"], writes=["T4"])

                            def norm_2():
                                P.op("dve", lambda e: e.tensor_tensor(O4[:], O4[:], T4[:], op=ALU.add), reads=["O4", "T4"], writes=["O4"])
                                P.op("dve", lambda e: e.tensor_tensor(T4[:], O4[:], O4[:], op=ALU.mult), reads=["O4"], writes=["T4"])
                                P.op("dve", lambda e: e.reduce_sum(ss[:], T4[:], axis=AX.X), reads=["T4"], writes=["ss"])
                                P.op("dve", lambda e: e.tensor_scalar(ssv[:], ss[:], 1.0 / 128.0, RMS_EPS, op0=ALU.mult, op1=ALU.add),
                                     reads=["ss"], writes=["ssv"])
                                P.op("pool", lambda e: e.tensor_tensor(rstd3[:, :, 0], ssv[:], negh[:], op=ALU.pow), reads=["ssv", "negh"], writes=["rstd"])

                            def norm_on():
                                ob = onb[gi % 2]
                                obn = "onb%d" % (gi % 2)
                                P.op("dve", lambda e: e.tensor_tensor(O4[:], O4[:], rstd3[:].to_broadcast([128, 4, 128]), op=ALU.mult),
                                     reads=["O4", "rstd"], writes=["O4"])
                                P.op("dve", lambda e: e.tensor_tensor(ob[:], O4[:], gainb[:, None, :].to_broadcast([128, 4, 128]), op=ALU.mult),
                                     reads=["O4", "gainb"], writes=[obn])

                            def norm_b():
                                ob = onb[gi % 2]
                                obn = "onb%d" % (gi % 2)
                                for il in range(4):
                                    P.op("pe", lambda e: e.transpose(bank_bf(7)[:, il * 128:(il + 1) * 128], ob[:, il, :], ident_b),
                                         reads=[obn, "cm_b"], writes=[BN[7]])
                                evac(oT[:, h, g * 512:(g + 1) * 512], bank_bf(7)[:, 0:512], reads=[BN[7]], writes=[("oT", g)])

                            defer(1, norm_1)
                            defer(3, norm_2)
                            defer(6, norm_on)
                            defer(11, norm_b)

                        for si, (g, j, m) in enumerate(steps):
                            gstep[0] += 1
                            run_due()
                            conv_tick[0] += 1
                            if conv_tick[0] % CONV_EVERY == 0:
                                conv_issue()
                            for u in unit_at.get(si, []):
                                u()
                            q0 = max(j, 4 * g)
                            N = (4 * g + 4 - q0) * 128
                            qsl = slice(q0 * 128, (4 * g + 4) * 128)
                            i = gstep[0]
                            bk = i % 3
                            pt = PT[i % 4]
                            ptn = "PT%d" % (i % 4)
                            P.op("pe", lambda e: e.matmul(B[bk][:, 0:N], lhsT=kz[hb][m][:, j * 128:(j + 1) * 128],
                                                          rhs=qT[hb][:, qsl], start=True, stop=True),
                                 reads=[("kz%d_%d" % (hb, m), j // 4), ("kzz", hb, m), (qn, g)], writes=[BN[bk]])
                            if j >= 4 * g:
                                P.op("act", lambda e: e.activation(out=pt[:, 0:64], in_=B[bk][:, 0:64], func=AF.Exp, scale=0.125, bias=cv[:, 112:113]),
                                     reads=[BN[bk], "cv"], writes=[(ptn, "m")])
                                P.op("act", lambda e: e.activation(out=pt[:, 64:N], in_=B[bk][:, 64:N], func=AF.Exp, scale=0.125),
                                     reads=[BN[bk]], writes=[ptn])
                            else:
                                P.op("act", lambda e: e.activation(out=pt[:, 0:N], in_=B[bk][:, 0:N], func=AF.Exp, scale=0.125),
                                     reads=[BN[bk]], writes=[ptn, (ptn, "m")])
                            pv_pending.append((g, j, m, pt, ptn, q0))
                            if len(pv_pending) > LOOK:
                                emit_pv(*pv_pending.pop(0))
                        while pv_pending:
                            emit_pv(*pv_pending.pop(0))
                        for si_ in sorted(unit_at):
                            if si_ >= nsteps:
                                for u in unit_at[si_]:
                                    u()
                    run_due(force=True)
                    if s == NSEQ - 1:
                        conv_issue(len(conv_jobs))
                    P.barrier()
            if stop in ("attn", "qk"):
                P.dma("sp", lambda e: e.dma_start(out=dbg["oT"][s], in_=oT[:]), reads=[], writes=["dbg"])
                P.barrier()
                continue
            with contextlib.ExitStack() as s3:
                mergedT = sb(s3, "mergedT", [128, 8, S], BF16)
                mixedT = sb(s3, "mixedT", [128, 4, S], BF16)
                NG = S // 512
                with contextlib.ExitStack() as s3a:
                    wu = sb(s3a, "wu", [128, 8, 512], BF16)
                    uT = sb(s3a, "uT", [128, 4, S], F32)
                    Tp = [sb(s3a, "Tp%d" % i, [128, S], F32) for i in range(2)]
                    pooledT = sb(s3a, "pooledT", [128, 4, S], BF16)
                    for kc2 in range(2):
                        P.dma("pool", lambda e: e.dma_start(out=wu[:, kc2 * 4:(kc2 + 1) * 4, :],
                                                            in_=w_in_k[:, kc2 * 4:(kc2 + 1) * 4, 3072:3584]),
                              writes=[("wu", kc2)])
                    bi = 0
                    for g in range(4):
                        for tg in range(NG):
                            bk = bi % 4
                            bi += 1
                            tsl = slice(tg * 512, (tg + 1) * 512)
                            for kc in range(8):
                                P.op("pe", lambda e: e.matmul(B[bk][:], lhsT=wu[:, kc, g * 128:(g + 1) * 128], rhs=xT[:, kc, tsl],
                                                              start=(kc == 0), stop=(kc == 7)),
                                     reads=[("wu", kc // 4)], writes=[BN[bk]])
                            evac(uT[:, g, tsl], B[bk][:], reads=[BN[bk]], writes=[("uT", g)])
                    for g, w in enumerate((2, 4, 8, 16)):
                        cur, cur_r = uT[:, g, :], [("uT", g)]
                        k = 0
                        sh = 1
                        while sh < w:
                            dst, dstn = Tp[k % 2], "Tp%d" % (k % 2)
                            eng = "dve"
                            P.op(eng, lambda e: e.tensor_tensor(dst[:, sh:S], cur[:, sh:S], cur[:, 0:S - sh], op=ALU.add),
                                 reads=cur_r, writes=[dstn])
                            P.op(eng, lambda e: e.tensor_copy(dst[:, 0:sh], cur[:, 0:sh]), reads=cur_r, writes=[dstn + "h"])
                            cur, cur_r = dst[:, :], [dstn, dstn + "h"]
                            k += 1
                            sh *= 2
                        P.op("dve", lambda e: e.tensor_tensor(cur[:, 0:16], cur[:, 0:16], cv[:, 16 + g * 16:32 + g * 16], op=ALU.mult),
                             reads=cur_r + ["cv"], writes=cur_r)
                        P.op("dve", lambda e: e.scalar_tensor_tensor(pooledT[:, g, :], cur, 1.0 / w, uT[:, g, :], op0=ALU.mult, op1=ALU.subtract),
                             reads=cur_r + [("uT", g)], writes=[("pooledT", g)])
                    for g in range(4):
                        for tg in range(NG):
                            bk = bi % 4
                            bi += 1
                            tsl = slice(tg * 512, (tg + 1) * 512)
                            P.op("pe", lambda e: e.matmul(B[bk][:], lhsT=wpool_b[:, g, :], rhs=pooledT[:, g, tsl], start=True, stop=True),
                                 reads=[("pooledT", g), "wpool_b"], writes=[BN[bk]])
                            P.op("dve", lambda e: e.tensor_scalar(mixedT[:, g, tsl], B[bk][:], pscale[:, g:g + 1], None, op0=ALU.mult),
                                 reads=[BN[bk], "pscale"], writes=[("mixedT", g)])
                    P.barrier()
                with contextlib.ExitStack() as s3b:
                    wa_c = [sb(s3b, "wa_c%d" % i, [128, 8, 128], BF16) for i in range(2)]
                    wb_c = [sb(s3b, "wb_c%d" % i, [128, 4, 128], BF16) for i in range(2)]
                    wga_c = [sb(s3b, "wga_c%d" % i, [128, 8, 128], BF16) for i in range(2)]
                    wgb_c = [sb(s3b, "wgb_c%d" % i, [128, 8, 128], BF16) for i in range(2)]
                    ga = [sb(s3b, "ga%d" % i, [128, 512], F32) for i in range(2)]
                    gb = [sb(s3b, "gb%d" % i, [128, 512], F32) for i in range(2)]
                    m1 = [sb(s3b, "m1_%d" % i, [128, 512], F32) for i in range(2)]
                    m2 = [sb(s3b, "m2_%d" % i, [128, 512], F32) for i in range(2)]

                    def load_chunk_w(c):
                        b = c % 2
                        cs = slice(c * 128, (c + 1) * 128)
                        P.dma("pool", lambda e: e.dma_start(out=wa_c[b][:], in_=w_a_k[:, :, cs]), writes=["wa_c%d" % b])
                        P.dma("pool", lambda e: e.dma_start(out=wb_c[b][:], in_=w_b_k[:, :, cs]), writes=["wb_c%d" % b])
                        P.dma("pool", lambda e: e.dma_start(out=wga_c[b][:], in_=w_in_k[:, :, 3584 + c * 128:3584 + (c + 1) * 128]),
                              writes=["wga_c%d" % b])
                        P.dma("pool", lambda e: e.dma_start(out=wgb_c[b][:], in_=w_in_k[:, :, 4608 + c * 128:4608 + (c + 1) * 128]),
                              writes=["wgb_c%d" % b])

                    load_chunk_w(0)
                    it = 0
                    for c in range(8):
                        cb = c % 2
                        if c + 1 < 8:
                            load_chunk_w(c + 1)
                        for tg in range(NG):
                            r = it % 2
                            it += 1
                            b0 = r * 4
                            tsl = slice(tg * 512, (tg + 1) * 512)
                            for hh_ in range(8):
                                P.op("pe", lambda e: e.matmul(B[b0][:], lhsT=wa_c[cb][:, hh_, :], rhs=oT[:, hh_, tsl],
                                                              start=(hh_ == 0), stop=(hh_ == 7)),
                                     reads=["wa_c%d" % cb], writes=[BN[b0]])
                            for g in range(4):
                                P.op("pe", lambda e: e.matmul(B[b0 + 1][:], lhsT=wb_c[cb][:, g, :], rhs=mixedT[:, g, tsl],
                                                              start=(g == 0), stop=(g == 3)),
                                     reads=["wb_c%d" % cb], writes=[BN[b0 + 1]])
                            for kc in range(8):
                                P.op("pe", lambda e: e.matmul(B[b0 + 2][:], lhsT=wga_c[cb][:, kc, :], rhs=xT[:, kc, tsl],
                                                              start=(kc == 0), stop=(kc == 7)),
                                     reads=["wga_c%d" % cb], writes=[BN[b0 + 2]])
                            for kc in range(8):
                                P.op("pe", lambda e: e.matmul(B[b0 + 3][:], lhsT=wgb_c[cb][:, kc, :], rhs=xT[:, kc, tsl],
                                                              start=(kc == 0), stop=(kc == 7)),
                                     reads=["wgb_c%d" % cb], writes=[BN[b0 + 3]])
                            P.op("act", lambda e: e.activation(out=ga[r][:], in_=B[b0 + 2][:], func=AF.Sigmoid, bias=gbias[:, c:c + 1]),
                                 reads=[BN[b0 + 2], "gbias"], writes=["ga%d" % r])
                            P.op("act", lambda e: e.activation(out=gb[r][:], in_=B[b0 + 3][:], func=AF.Sigmoid, bias=gbias[:, 8 + c:9 + c]),
                                 reads=[BN[b0 + 3], "gbias"], writes=["gb%d" % r])
                            P.op("dve", lambda e: e.tensor_tensor(m1[r][:], B[b0][:], ga[r][:], op=ALU.mult),
                                 reads=[BN[b0], "ga%d" % r], writes=["m1_%d" % r])
                            P.op("dve", lambda e: e.tensor_tensor(m2[r][:], B[b0 + 1][:], gb[r][:], op=ALU.mult),
                                 reads=[BN[b0 + 1], "gb%d" % r], writes=["m2_%d" % r])
                            P.op("pool", lambda e: e.tensor_tensor(mergedT[:, c, tsl], m1[r][:], m2[r][:], op=ALU.add),
                                 reads=["m1_%d" % r, "m2_%d" % r], writes=[("mergedT", c)])
                    P.barrier()
                if stop == "mix":
                    P.dma("sp", lambda e: e.dma_start(out=dbg["mergedT"][s], in_=mergedT[:]), reads=[], writes=["dbg"])
                    P.barrier()
                    continue
                with contextlib.ExitStack() as s3c:
                    wout = sb(s3c, "wout", [128, 8, D], BF16)
                    g1 = sb(s3c, "g1", [128, D], F32)
                    b1 = sb(s3c, "b1", [128, D], F32)
                    xt = [sb(s3c, "xt%d" % i, [128, D], F32) for i in range(2)]
                    zs = [sb(s3c, "z%d" % i, [128, D], F32) for i in range(3)]
                    hh = [sb(s3c, "hh%d" % i, [128, D], F32) for i in range(3)]
                    hbf = [sb(s3c, "hbf%d" % i, [128, D], BF16) for i in range(4)]
                    hTs = [sb(s3c, "hT%d" % i, [128, 8, 128], F32) for i in range(2)]
                    statss = [sb(s3c, "stats%d" % i, [128, 2, 6], F32) for i in range(3)]
                    mvs = [sb(s3c, "mv%d" % i, [128, 4], F32) for i in range(3)]
                    rrs = [sb(s3c, "rr%d" % i, [128, 16], F32) for i in range(2)]
                    nm = sb(s3c, "nm", [128, 3], F32)
                    lg = sb(s3c, "lg", [128, 36], F32)
                    ge = sb(s3c, "ge", [128, 4], F32)
                    gone = sb(s3c, "gone", [128, 4], F32)
                    mneg = sb(s3c, "mneg", [128, 4, 1], F32)
                    elms = [sb(s3c, "elm%d" % i, [128, 4, 8], F32) for i in range(2)]
                    elm2 = sb(s3c, "elm2", [128, 32], F32)
                    oh1s = [sb(s3c, "oh1_%d" % i, [128, 32], F32) for i in range(3)]
                    oh2s = [sb(s3c, "oh2_%d" % i, [128, 32], F32) for i in range(2)]
                    Mbs = [sb(s3c, "Mb%d" % i, [128, 32], BF16) for i in range(2)]
                    posin = sb(s3c, "posin", [128, 32], F32)
                    valid = sb(s3c, "valid", [128, 32], F32)
                    smv = sb(s3c, "smv", [128, 32], F32)
                    tmp32 = sb(s3c, "tmp32", [128, 32], F32)
                    for kc2 in range(2):
                        P.dma("pool", lambda e: e.dma_start(out=wout[:, kc2 * 4:(kc2 + 1) * 4, :], in_=w_out_k[:, kc2 * 4:(kc2 + 1) * 4, :]),
                              writes=[("wout", kc2)])
                    P.dma("sp", lambda e: e.dma_start(out=g1[:], in_=ln1_g[0:1, :].to_broadcast([128, D])), writes=["g1"])
                    P.dma("sp", lambda e: e.dma_start(out=b1[:], in_=ln1_b[0:1, :].to_broadcast([128, D])), writes=["b1"])

                    def L0a(t):
                        r = t % 2
                        xb_, xn = xt[r], "xt%d" % r
                        P.dma("sp", lambda e: e.dma_start(out=xb_[:], in_=x[s, t * 128:(t + 1) * 128, :]), writes=[xn])
                        for hf in range(2):
                            bk = (0, 1)[hf] if r == 0 else (6, 7)[hf]
                            for kc in range(8):
                                P.op("pe", lambda e: e.matmul(B[bk][:], lhsT=mergedT[:, kc, t * 128:(t + 1) * 128],
                                                              rhs=wout[:, kc, hf * 512:(hf + 1) * 512], start=(kc == 0), stop=(kc == 7)),
                                     reads=[("wout", kc // 4)], writes=[BN[bk]])

                    def L0b(t):
                        r = t % 2
                        q3 = t % 3
                        xb_, xn = xt[r], "xt%d" % r
                        z, stats, mv = zs[q3], statss[q3], mvs[q3]
                        zn = "z%d" % q3
                        for hf in range(2):
                            bk = (0, 1)[hf] if r == 0 else (6, 7)[hf]
                            P.op("dve", lambda e: e.scalar_tensor_tensor(z[:, hf * 512:(hf + 1) * 512], xb_[:, hf * 512:(hf + 1) * 512], ALPHA, B[bk][:],
                                                                         op0=ALU.mult, op1=ALU.add),
                                 reads=[xn, BN[bk]], writes=[(zn, hf)])
                            P.op("dve", lambda e: e.bn_stats(stats[:, hf, :], z[:, hf * 512:(hf + 1) * 512]), reads=[(zn, hf)], writes=[("stats", q3, hf)])
                        P.op("dve", lambda e: e.bn_aggr(mv[:, 0:2], stats[:].rearrange("p a b -> p (a b)")), reads=[("stats", q3, 0), ("stats", q3, 1)], writes=[("mv", q3)])
                        P.op("dve", lambda e: e.tensor_scalar(mv[:, 2:3], mv[:, 1:2], LN_EPS, None, op0=ALU.add), reads=[("mv", q3)], writes=[("mv2", q3)])

                    def L1a(t):
                        q3 = t % 3
                        z, mv = zs[q3], mvs[q3]
                        zn = "z%d" % q3
                        hb_, hn = hh[q3], "hh%d" % q3
                        P.op("pool", lambda e: e.tensor_tensor(mv[:, 3:4], mv[:, 2:3], negh[:, 0:1], op=ALU.pow), reads=[("mv2", q3), "negh"], writes=[("mv3", q3)])
                        P.op("dve", lambda e: e.scalar_tensor_tensor(nm[:, q3:q3 + 1], mv[:, 0:1], -1.0, mv[:, 3:4], op0=ALU.mult, op1=ALU.mult),
                             reads=[("mv", q3), ("mv3", q3)], writes=[("nm", q3)])
                        P.op("act", lambda e: e.activation(out=hb_[:], in_=z[:], func=AF.Identity, scale=mv[:, 3:4], bias=nm[:, q3:q3 + 1]),
                             reads=[(zn, 0), (zn, 1), ("mv3", q3), ("nm", q3)], writes=[hn])

                    def L1a2(t):
                        q3 = t % 3
                        hb_, hn = hh[q3], "hh%d" % q3
                        P.op("dve", lambda e: e.tensor_tensor(hb_[:], hb_[:], g1[:], op=ALU.mult), reads=[hn, "g1"], writes=[hn])
                        P.op("dve", lambda e: e.tensor_tensor(hb_[:], hb_[:], b1[:], op=ALU.add), reads=[hn, "b1"], writes=[hn])

                    def L1b(t):
                        gt = s * NT + t
                        r = t % 2
                        r3 = t % 3
                        hb_, hn = hh[r3], "hh%d" % r3
                        P.dma("sp", lambda e: e.dma_start(out=h_scr[gt * 128:(gt + 1) * 128, :], in_=hb_[:]), reads=[hn], writes=[("h_scr", gt)])
                        if "h" in dbg:
                            P.dma("sp", lambda e: e.dma_start(out=dbg["h"][gt * 128:(gt + 1) * 128, :], in_=hb_[:]), reads=[hn], writes=[("dbg_h", gt)])
                        P.op("act", lambda e: e.activation(out=hbf[t % 4][:], in_=hb_[:], func=AF.Copy), reads=[hn], writes=["hbf%d" % (t % 4)])
                        for hf in range(2):
                            bk = 2 + hf
                            for j in range(4):
                                kc = hf * 4 + j
                                P.op("pe", lambda e: e.transpose(B[bk][:, j * 128:(j + 1) * 128], hb_[:, kc * 128:(kc + 1) * 128], ident_f),
                                     reads=[hn, "cm_f"], writes=[BN[bk]])
                            evac(hTs[r][:, hf * 4:(hf + 1) * 4, :], B[bk][:].rearrange("p (a b) -> p a b", a=4), reads=[BN[bk]], writes=[("hT", r, hf)], eng="act")

                    def L2a1(t):
                        gt = s * NT + t
                        r = t % 2
                        hT = hTs[r]
                        rr = rrs[r]
                        elm = elms[r]
                        elm_f = elm[:].rearrange("p a b -> p (a b)")
                        oh1 = oh1s[t % 3]
                        o1n = "oh1_%d" % (t % 3)
                        for kc in range(8):
                            P.op("pe", lambda e: e.matmul(B[4][:, 0:36], lhsT=hT[:, kc, :], rhs=wrt[:, kc, :], start=(kc == 0), stop=(kc == 7)),
                                 reads=[("hT", r, kc // 4), "wrt"], writes=[BN[4]])
                        P.op("dve", lambda e: e.tensor_tensor(lg[:], B[4][:, 0:36], brt[:], op=ALU.add), reads=[BN[4], "brt"], writes=["lg"])
                        P.op("dve", lambda e: e.reduce_max(rr[:, 0:1], lg[:, 0:4], axis=AX.X), reads=["lg"], writes=[("rr0", r)])
                        P.op("dve", lambda e: e.tensor_scalar(rr[:, 1:2], rr[:, 0:1], -1.0, None, op0=ALU.mult), reads=[("rr0", r)], writes=[("rr1", r)])
                        P.op("act", lambda e: e.activation(out=ge[:], in_=lg[:, 0:4], func=AF.Exp, bias=rr[:, 1:2]), reads=["lg", ("rr1", r)], writes=["ge"])
                        P.op("dve", lambda e: e.reduce_sum(rr[:, 2:3], ge[:], axis=AX.X), reads=["ge"], writes=[("rr2", r)])
                        P.op("dve", lambda e: e.reciprocal(rr[:, 3:4], rr[:, 2:3]), reads=[("rr2", r)], writes=[("rr3", r)])
                        P.op("dve", lambda e: e.tensor_scalar(gone[:], lg[:, 0:4], rr[:, 0:1], None, op0=ALU.is_equal), reads=["lg", ("rr0", r)], writes=["gone"])
                        P.op("dve", lambda e: e.tensor_scalar(mneg[:, :, 0], gone[:], -1.0, 1e30, op0=ALU.add, op1=ALU.mult), reads=["gone"], writes=["mneg"])
                        P.op("dve", lambda e: e.tensor_tensor(elm[:], lg[:, 4:36].rearrange("p (a b) -> p a b", a=4), mneg[:].to_broadcast([128, 4, 8]), op=ALU.add),
                             reads=["lg", "mneg"], writes=[("elm", r)])
                        P.op("dve", lambda e: e.reduce_max(rr[:, 4:5], elm_f, axis=AX.X), reads=[("elm", r)], writes=[("rr4", r)])
                        P.op("dve", lambda e: e.tensor_scalar(oh1[:], elm_f, rr[:, 4:5], None, op0=ALU.is_equal), reads=[("elm", r), ("rr4", r)], writes=[o1n])

                    def L2a2(t):
                        gt = s * NT + t
                        r = t % 2
                        rr = rrs[r]
                        elm = elms[r]
                        elm_f = elm[:].rearrange("p a b -> p (a b)")
                        oh1, oh2, Mb = oh1s[t % 3], oh2s[r], Mbs[r]
                        o1n, o2n, mbn = "oh1_%d" % (t % 3), "oh2_%d" % r, "Mb%d" % r
                        P.op("dve", lambda e: e.scalar_tensor_tensor(elm2[:], oh1[:], -1e30, elm_f, op0=ALU.mult, op1=ALU.add), reads=[o1n, ("elm", r)], writes=["elm2"])
                        P.op("dve", lambda e: e.reduce_max(rr[:, 5:6], elm2[:], axis=AX.X), reads=["elm2"], writes=[("rr5", r)])
                        P.op("dve", lambda e: e.tensor_scalar(oh2[:], elm2[:], rr[:, 5:6], None, op0=ALU.is_equal), reads=["elm2", ("rr5", r)], writes=[o2n])
                        P.op("dve", lambda e: e.tensor_tensor(rr[:, 6:7], rr[:, 5:6], rr[:, 4:5], op=ALU.subtract), reads=[("rr4", r), ("rr5", r)], writes=[("rr6", r)])
                        P.op("act", lambda e: e.activation(out=rr[:, 7:8], in_=rr[:, 6:7], func=AF.Exp), reads=[("rr6", r)], writes=[("rr7", r)])
                        P.op("dve", lambda e: e.tensor_scalar(rr[:, 8:9], rr[:, 7:8], 1.0, None, op0=ALU.add), reads=[("rr7", r)], writes=[("rr8", r)])
                        P.op("dve", lambda e: e.reciprocal(rr[:, 9:10], rr[:, 8:9]), reads=[("rr8", r)], writes=[("rr9", r)])
                        P.op("dve", lambda e: e.tensor_tensor(wts[:, gt, 0:1], rr[:, 9:10], rr[:, 3:4], op=ALU.mult), reads=[("rr9", r), ("rr3", r)], writes=[("wts", gt, 0)])
                        P.op("dve", lambda e: e.tensor_tensor(rr[:, 10:11], rr[:, 7:8], rr[:, 9:10], op=ALU.mult), reads=[("rr7", r), ("rr9", r)], writes=[("rr10", r)])
                        P.op("dve", lambda e: e.tensor_tensor(wts[:, gt, 1:2], rr[:, 10:11], rr[:, 3:4], op=ALU.mult), reads=[("rr10", r), ("rr3", r)], writes=[("wts", gt, 1)])
                        P.op("dve", lambda e: e.tensor_tensor(Mb[:], oh1[:], oh2[:], op=ALU.add), reads=[o1n, o2n], writes=[mbn])

                    def L2b(t):
                        gt = s * NT + t
                        r = t % 2
                        r3 = t % 3
                        oh1, oh2, Mb = oh1s[t % 3], oh2s[r], Mbs[r]
                        o1n, o2n, mbn = "oh1_%d" % (t % 3), "oh2_%d" % r, "Mb%d" % r
                        r4 = t % 4
                        P.op("pe", lambda e: e.matmul(B[5][:, 0:32], lhsT=tri_b, rhs=Mb[:], start=True, stop=True, skip_group_check=True),
                             reads=[mbn, "cm_b"], writes=[BN[5]])
                        P.op("pe", lambda e: e.matmul(B[5][:, 32:64], lhsT=ones_b, rhs=Mb[:], start=False, stop=True, skip_group_check=True),
                             reads=[mbn, "cm_b"], writes=[BN[5]])
                        P.op("dve", lambda e: e.tensor_tensor(posin[:], B[5][:, 0:32], cnt[:], op=ALU.add), reads=[BN[5], "cnt"], writes=["posin"])
                        P.op("dve", lambda e: e.tensor_scalar(valid[:], posin[:], CAP - 0.5, None, op0=ALU.is_lt), reads=["posin"], writes=["valid"])
                        P.op("dve", lambda e: e.tensor_tensor(smv[:], posin[:], eoffmd, op=ALU.add), reads=["posin", "cv"], writes=["smv"])
                        P.op("dve", lambda e: e.tensor_tensor(smv[:], smv[:], valid[:], op=ALU.mult), reads=["smv", "valid"], writes=["smv"])
                        for k_, ohk, ohn in ((0, oh1, o1n), (1, oh2, o2n)):
                            P.op("dve", lambda e: e.tensor_tensor(tmp32[:], smv[:], ohk[:], op=ALU.mult), reads=["smv", ohn], writes=["tmp32"])
                            P.op("dve", lambda e: e.reduce_sum(slots_f[:, gt, k_:k_ + 1], tmp32[:], axis=AX.X), reads=["tmp32"], writes=[("slots_f", gt, k_)])
                        P.op("dve", lambda e: e.tensor_tensor(cnt[:], cnt[:], B[5][:, 32:64], op=ALU.add), reads=["cnt", BN[5]], writes=["cnt"])
                        P.op("dve", lambda e: e.tensor_scalar(slots_f[:, gt, :], slots_f[:, gt, :], float(DUMP), None, op0=ALU.add),
                             reads=[("slots_f", gt, 0), ("slots_f", gt, 1)], writes=[("slots_f2", gt)])
                        P.op("dve", lambda e: e.tensor_copy(slots_i[:, gt, :], slots_f[:, gt, :]), reads=[("slots_f2", gt)], writes=[("slots_i", gt)])
                        for k_ in range(2):
                            P.dma("pool", lambda e: e.indirect_dma_start(
                                out=xs[:, :], out_offset=bass.IndirectOffsetOnAxis(ap=slots_i[:, gt, k_:k_ + 1], axis=0),
                                in_=hbf[r4][:], in_offset=None), reads=["hbf%d" % r4, ("slots_i", gt)] + xs_init, writes=[("xs", gt, k_)])

                    pipeline(NT, [L0a, L0b, L1a, L1a2, L1b, L2a1, L2a2, L2b])
                    P.barrier()
        NTOT = NSEQ * NT
        if stop in ("ln1",):
            P.dma("sp", lambda e: e.dma_start(out=dbg["slots"][:, :, :], in_=slots_i[:]), writes=["dbg1"])
            P.dma("sp", lambda e: e.dma_start(out=dbg["wts"][:, :, :], in_=wts[:]), writes=["dbg2"])
            P.barrier()
            return nc
        if stop is not None and stop not in ("moe",):
            P.barrier()
            return nc

        xs_all = [("xs", gt, k_) for gt in range(NTOT) for k_ in range(2)]

        with contextlib.ExitStack() as s4:
            NWB = 3
            wg = [sb(s4, "wg%d" % i, [128, 8, D], BF16) for i in range(NWB)]
            wd = [sb(s4, "wd%d" % i, [128, 4, D], BF16) for i in range(NWB)]
            xsb3 = [sb(s4, "xsb3_%d" % i, [128, NBLK, D], BF16) for i in range(2)]
            xsT3 = [sb(s4, "xsT3_%d" % i, [128, 8, CAP], BF16) for i in range(2)]
            sg = [sb(s4, "sg%d" % i, [128, CAP], F32) for i in range(2)]
            actT3 = [sb(s4, "actT3_%d" % i, [128, 4, CAP], BF16) for i in range(2)]
            yb = [sb(s4, "yb%d" % i, [128, D], F32) for i in range(2)]

            def load_expert(e_):
                b = e_ % NWB
                for kc2 in range(2):
                    P.dma("sp", lambda e: e.dma_start(out=wg[b][:, kc2 * 4:(kc2 + 1) * 4, :],
                                                      in_=wgu_bf[e_].rearrange("(kc k) n -> k kc n", k=128)[:, kc2 * 4:(kc2 + 1) * 4, :]),
                          reads=[("wcv", e_, kc2)], writes=[("wg%d" % b, kc2)])
                P.dma("sp", lambda e: e.dma_start(out=wd[b][:], in_=wdn_bf[e_].rearrange("(kc k) n -> k kc n", k=128)),
                      reads=[("wcv", e_, 2)], writes=["wd%d" % b])

            for e0 in range(min(NWB, NE)):
                load_expert(e0)
            def E0(e_):
                r = e_ % 2
                for blk in range(NBLK):
                    row0 = e_ * CAP + blk * 128
                    P.dma("sp", lambda e: e.dma_start(out=xsb3[r][:, blk, :], in_=xs[row0:row0 + 128, :]), reads=xs_all, writes=[("xsb3", r, blk)])

            def E1(e_):
                r = e_ % 2
                for blk in range(NBLK):
                    bt = (e_ * NBLK + blk) % 2
                    for kc in range(8):
                        P.op("pe", lambda e: e.transpose(bank_bf(bt)[:, kc * 128:(kc + 1) * 128], xsb3[r][:, blk, kc * 128:(kc + 1) * 128], ident_b),
                             reads=[("xsb3", r, blk), "cm_b"], writes=[BN[bt]])
                    evac(xsT3[r][:, :, blk * 128:(blk + 1) * 128], bank_bf(bt)[:, :].rearrange("p (a b) -> p a b", a=8),
                         reads=[BN[bt]], writes=[("xsT3", r, blk)])

            def E2(e_):
                r = e_ % 2
                eb = e_ % NWB
                xr = [("xsT3", r, blk) for blk in range(NBLK)]
                for fc in range(4):
                    bg, bu = ((2, 3), (4, 5))[fc % 2]
                    for col0, bk in ((fc * 128, bg), (512 + fc * 128, bu)):
                        for kc in range(8):
                            P.op("pe", lambda e: e.matmul(B[bk][:, 0:CAP], lhsT=wg[eb][:, kc, col0:col0 + 128], rhs=xsT3[r][:, kc, :],
                                                          start=(kc == 0), stop=(kc == 7)),
                                 reads=xr + [("wg%d" % eb, kc // 4)], writes=[BN[bk]])
                    q_ = fc % 2
                    P.op("act", lambda e: e.activation(out=sg[q_][:], in_=B[bg][:, 0:CAP], func=AF.Silu), reads=[BN[bg]], writes=["sg%d" % q_])
                    P.op("dve", lambda e: e.tensor_tensor(actT3[r][:, fc, :], B[bu][:, 0:CAP], sg[q_][:], op=ALU.mult),
                         reads=[BN[bu], "sg%d" % q_], writes=[("actT3", r, fc)])

            def E3(e_):
                r = e_ % 2
                eb = e_ % NWB
                ar = [("actT3", r, fc) for fc in range(4)]
                for blk in range(NBLK):
                    q_ = (e_ * NBLK + blk) % 2
                    row0 = e_ * CAP + blk * 128
                    for half, bk in ((0, 6), (1, 7)):
                        for fc in range(4):
                            P.op("pe", lambda e: e.matmul(B[bk][:], lhsT=actT3[r][:, fc, blk * 128:(blk + 1) * 128], rhs=wd[eb][:, fc, half * 512:(half + 1) * 512],
                                                          start=(fc == 0), stop=(fc == 3)),
                                 reads=ar + ["wd%d" % eb], writes=[BN[bk]])
                        evac(yb[q_][:, half * 512:(half + 1) * 512], B[bk][:], reads=[BN[bk]], writes=[("yb%d" % q_, half)])
                    P.dma("pool", lambda e: e.dma_start(out=ys[row0:row0 + 128, :], in_=yb[q_][:]),
                          reads=[("yb%d" % q_, 0), ("yb%d" % q_, 1)], writes=[("ys", e_, blk)])
                if e_ + NWB < NE:
                    load_expert(e_ + NWB)

            pipeline(NE, [E0, E1, E2, E3])
            P.barrier()
        ys_all = [("ys", e_, blk) for e_ in range(NE) for blk in range(NBLK)] + ["ys_dump"]
        if stop == "moe":
            P.dma("sp", lambda e: e.dma_start(out=dbg["ys"][:, :], in_=ys[:, :]), writes=["dbg1"])
            P.dma("sp", lambda e: e.dma_start(out=dbg["slots"][:, :, :], in_=slots_i[:]), writes=["dbg2"])
            P.dma("sp", lambda e: e.dma_start(out=dbg["wts"][:, :, :], in_=wts[:]), writes=["dbg3"])
            P.barrier()
            return nc

        with contextlib.ExitStack() as s5:
            g2 = sb(s5, "g2", [128, D], F32)
            b2 = sb(s5, "b2", [128, D], F32)
            y1 = [sb(s5, "y1_%d" % i, [128, D], F32) for i in range(3)]
            y2 = [sb(s5, "y2_%d" % i, [128, D], F32) for i in range(3)]
            ht = [sb(s5, "ht%d" % i, [128, D], F32) for i in range(3)]
            za = [sb(s5, "za%d" % i, [128, D], F32) for i in range(2)]
            zc = [sb(s5, "zc%d" % i, [128, D], F32) for i in range(2)]
            oo = [sb(s5, "oo%d" % i, [128, D], F32) for i in range(2)]
            stats2 = [sb(s5, "stats2_%d" % i, [128, 2, 6], F32) for i in range(2)]
            mv2 = [sb(s5, "mv2_%d" % i, [128, 4], F32) for i in range(2)]
            nm2 = sb(s5, "nm2", [128, 2], F32)
            P.dma("sp", lambda e: e.dma_start(out=g2[:], in_=ln2_g[0:1, :].to_broadcast([128, D])), writes=["g2"])
            P.dma("sp", lambda e: e.dma_start(out=b2[:], in_=ln2_b[0:1, :].to_broadcast([128, D])), writes=["b2"])

            def c0(gt):
                r = gt % 3
                P.dma("pool", lambda e: e.indirect_dma_start(
                    out=y1[r][:], out_offset=None, in_=ys[:, :],
                    in_offset=bass.IndirectOffsetOnAxis(ap=slots_i[:, gt, 0:1], axis=0)), reads=ys_all, writes=["y1_%d" % r])
                P.dma("pool", lambda e: e.indirect_dma_start(
                    out=y2[r][:], out_offset=None, in_=ys[:, :],
                    in_offset=bass.IndirectOffsetOnAxis(ap=slots_i[:, gt, 1:2], axis=0)), reads=ys_all, writes=["y2_%d" % r])
                P.dma("sp", lambda e: e.dma_start(out=ht[r][:], in_=h_scr[gt * 128:(gt + 1) * 128, :]), reads=[("h_scr", gt)], writes=["ht%d" % r])

            def c1(gt):
                r = gt % 3
                q_ = gt % 2
                P.op("act", lambda e: e.activation(out=za[q_][:], in_=ht[r][:], func=AF.Copy, scale=ALPHA), reads=["ht%d" % r], writes=["za%d" % q_])
                P.op("dve", lambda e: e.scalar_tensor_tensor(za[q_][:], y1[r][:], wts[:, gt, 0:1], za[q_][:], op0=ALU.mult, op1=ALU.add),
                     reads=["y1_%d" % r, "za%d" % q_], writes=["za%d" % q_])
                P.op("dve", lambda e: e.scalar_tensor_tensor(zc[q_][:], y2[r][:], wts[:, gt, 1:2], za[q_][:], op0=ALU.mult, op1=ALU.add),
                     reads=["y2_%d" % r, "za%d" % q_], writes=["zc%d" % q_])
                for hf in range(2):
                    P.op("dve", lambda e: e.bn_stats(stats2[q_][:, hf, :], zc[q_][:, hf * 512:(hf + 1) * 512]), reads=["zc%d" % q_], writes=[("stats2", q_, hf)])
                P.op("dve", lambda e: e.bn_aggr(mv2[q_][:, 0:2], stats2[q_][:].rearrange("p a b -> p (a b)")),
                     reads=[("stats2", q_, 0), ("stats2", q_, 1)], writes=[("mv2a", q_)])
                P.op("dve", lambda e: e.tensor_scalar(mv2[q_][:, 2:3], mv2[q_][:, 1:2], LN_EPS, None, op0=ALU.add), reads=[("mv2a", q_)], writes=[("mv2b", q_)])

            def c2(gt):
                q_ = gt % 2
                s_, t_ = gt // NT, gt % NT
                P.op("pool", lambda e: e.tensor_tensor(mv2[q_][:, 3:4], mv2[q_][:, 2:3], negh[:, 0:1], op=ALU.pow), reads=[("mv2b", q_), "negh"], writes=[("mv2c", q_)])
                P.op("dve", lambda e: e.scalar_tensor_tensor(nm2[:, q_:q_ + 1], mv2[q_][:, 0:1], -1.0, mv2[q_][:, 3:4], op0=ALU.mult, op1=ALU.mult),
                     reads=[("mv2a", q_), ("mv2c", q_)], writes=[("nm2", q_)])
                P.op("act", lambda e: e.activation(out=oo[q_][:], in_=zc[q_][:], func=AF.Identity, scale=mv2[q_][:, 3:4], bias=nm2[:, q_:q_ + 1]),
                     reads=["zc%d" % q_, ("mv2c", q_), ("nm2", q_)], writes=["oo%d" % q_])
                P.op("dve", lambda e: e.tensor_tensor(oo[q_][:], oo[q_][:], g2[:], op=ALU.mult), reads=["oo%d" % q_, "g2"], writes=["oo%d" % q_])
                P.op("dve", lambda e: e.tensor_tensor(oo[q_][:], oo[q_][:], b2[:], op=ALU.add), reads=["oo%d" % q_, "b2"], writes=["oo%d" % q_])
                P.dma("sp", lambda e: e.dma_start(out=out[s_, t_ * 128:(t_ + 1) * 128, :], in_=oo[q_][:]), reads=["oo%d" % q_], writes=[("out", gt)])

            pipeline(NTOT, [c0, c1, c2])
            P.barrier()
    return nc


def _in_maps(inputs):
    f = lambda a: np.ascontiguousarray(np.asarray(a))
    cmat, cvec = _consts()
    common = {
        "w_in": f(inputs["w_in"][0]),
        "gate_bias": f(inputs["gate_bias"][0].reshape(16, 128).T),
        "lam_vecs": f(inputs["lam_vecs"][0].reshape(1, 256)),
        "subln_gain": f(inputs["subln_gain"][0].reshape(1, 128)),
        "w_pool": f(inputs["w_pool"][0]),
        "pool_scale": f(inputs["pool_scale"][0].reshape(4, 128).T),
        "w_a": f(inputs["w_branch_a"][0]),
        "w_b": f(inputs["w_branch_b"][0]),
        "w_out": f(inputs["w_out"][0]),
        "ln1_g": f(inputs["ln1_gain"][0].reshape(1, D)),
        "ln1_b": f(inputs["ln1_bias"][0].reshape(1, D)),
        "w_rt": f(np.concatenate([inputs["w_router_group"][0], inputs["w_router_expert"][0]], axis=1)),
        "b_rt": f(np.concatenate([inputs["b_router_group"][0], inputs["b_router_expert"][0]]).reshape(1, 36)),
        "w_gu": f(inputs["w_gate_up"][0]),
        "w_dn": f(inputs["w_down"][0]),
        "ln2_g": f(inputs["ln2_gain"][0].reshape(1, D)),
        "ln2_b": f(inputs["ln2_bias"][0].reshape(1, D)),
        "cmat": cmat,
        "cvec": cvec,
    }
    maps = []
    xx = np.asarray(inputs["x"])
    pp = np.asarray(inputs["positions"]).astype(np.int32)
    for c in range(NCORES):
        m = dict(common)
        m["x"] = f(xx[c * NSEQ:(c + 1) * NSEQ])
        m["pos"] = f(pp[c * NSEQ:(c + 1) * NSEQ])
        maps.append(m)
    return maps


def kernel(**inputs):
    nc = build_nc()
    res = run_bass_kernel_spmd(nc, _in_maps(inputs), core_ids=list(range(NCORES)))
    return np.concatenate([np.asarray(r["out"]) for r in res.results], axis=0).astype(np.float32)
```

```python
import contextlib
import math
import numpy as np
import concourse.bass as bass
import concourse.mybir as mybir
from concourse.bass_utils import run_bass_kernel_spmd

F32 = mybir.dt.float32
BF16 = mybir.dt.bfloat16
I32 = mybir.dt.int32
AF = mybir.ActivationFunctionType
ALU = mybir.AluOpType
AX = mybir.AxisListType

NCORES = 8
NSEQ = 2
S = 2048
D = 1024
H = 8
NT = S // 128
NE = 32
CAP = 384
NBLK = CAP // 128
NSLOT = NE * CAP + 128
DUMP = NE * CAP
ALPHA = 2.0 ** 0.25
LAMBDA_INIT = 0.2
LN_EPS = 1e-5
RMS_EPS = 1e-5
IN_W = 5632
TWO_PI = 2.0 * math.pi
C1 = 6.28125
C2 = TWO_PI - C1

SEM_LIMIT = 30000
DMA_RING = 6
DMA_RING_Q = {"poolc": 2}
CONV_EVERY = 13


class Prog:
    def __init__(self, nc, stack):
        self.nc = nc
        self.stack = stack
        self.eng = {"pe": nc.tensor, "dve": nc.vector, "act": nc.scalar,
                    "pool": nc.gpsimd, "sp": nc.sync, "poolc": nc.gpsimd}
        self.cnt = {}
        self.sems = {}
        self.seen = {}
        self.res = {}
        self.dma_n = {}
        self.dma_ring = {}
        self.dma_ring_cnt = {}
        self.ninst = 0
        self.recording = None
        for s in ["pe", "dve", "act", "pool"]:
            self.cnt[s] = 0
            self.sems[s] = []
            self._new_sem(s)

    def _alloc_sem(self, name):
        return self.stack.enter_context(self.nc.semaphore(name))

    def _new_sem(self, s):
        sem = self._alloc_sem("s_%s_%d" % (s, len(self.sems[s])))
        self.sems[s].append((sem, self.cnt[s]))

    def _wait(self, engname, tok):
        stream, idx = tok
        key = (engname, stream)
        if self.seen.get(key, 0) >= idx:
            return
        if stream.startswith("dma"):
            self.seen[key] = idx
            q, r = stream.split(":")[1:]
            self.eng[engname].wait_ge(self.dma_ring[q][int(r)], 16 * idx)
            self.ninst += 1
            return
        if stream == engname and engname == "pe":
            return
        self.seen[key] = idx
        for sem, base in reversed(self.sems[stream]):
            if idx > base:
                self.eng[engname].wait_ge(sem, idx - base)
                self.ninst += 1
                return
        raise RuntimeError("bad token")

    def _deps(self, reads, writes):
        deps = []
        for r in reads:
            st = self.res.get(r)
            if st and st[0] is not None:
                deps.append(st[0])
        for w in writes:
            st = self.res.get(w)
            if st:
                if st[0] is not None:
                    deps.append(st[0])
                deps.extend(st[1])
        return deps

    def _commit(self, tok, reads, writes):
        for r in reads:
            st = self.res.setdefault(r, [None, []])
            st[1].append(tok)
        for w in writes:
            self.res[w] = [tok, []]

    def _excl(self, reads, writes):
        r2 = [r for r in reads if not (isinstance(r, str) and r.startswith("B") and r[1:].isdigit())]
        w2 = list(writes) + [r for r in reads if (isinstance(r, str) and r.startswith("B") and r[1:].isdigit())]
        return r2, w2

    def op(self, engname, fn, reads=(), writes=()):
        if self.recording is not None:
            rec = _Rec()
            fn(rec)
            self.recording.append(("op", engname, rec.call, tuple(reads), tuple(writes)))
            return None
        reads, writes = self._excl(reads, writes)
        for d in self._deps(reads, writes):
            self._wait(engname, d)
        inst = fn(self.eng[engname])
        if self.cnt[engname] - self.sems[engname][-1][1] >= SEM_LIMIT:
            self._new_sem(engname)
        sem, base = self.sems[engname][-1]
        inst.then_inc(sem, 1)
        self.cnt[engname] += 1
        self.ninst += 1
        tok = (engname, self.cnt[engname])
        self._commit(tok, reads, writes)
        return tok

    def dma(self, q, fn, reads=(), writes=()):
        if self.recording is not None:
            rec = _Rec()
            fn(rec)
            self.recording.append(("dma", q, rec.call, tuple(reads), tuple(writes)))
            return None
        nring = DMA_RING_Q.get(q, DMA_RING)
        if q not in self.dma_ring:
            self.dma_ring[q] = [self._alloc_sem("d_%s_%d" % (q, i)) for i in range(nring)]
            self.dma_ring_cnt[q] = [0] * nring
            self.dma_n[q] = 0
        r = self.dma_n[q] % nring
        self.dma_n[q] += 1
        stream = "dma:%s:%d" % (q, r)
        if self.dma_ring_cnt[q][r] > 0:
            self._wait(q, (stream, self.dma_ring_cnt[q][r]))
        for d in self._deps(reads, writes):
            self._wait(q, d)
        inst = fn(self.eng[q])
        inst.then_inc(self.dma_ring[q][r], 16)
        self.dma_ring_cnt[q][r] += 1
        self.ninst += 1
        tok = (stream, self.dma_ring_cnt[q][r])
        self._commit(tok, reads, writes)
        return tok

    def record(self, fn, *args):
        assert self.recording is None
        self.recording = []
        try:
            fn(*args)
            return self.recording
        finally:
            self.recording = None

    def replay(self, item):
        kind, eng, call, reads, writes = item
        name, a, kw = call
        f = lambda e: getattr(e, name)(*a, **kw)
        if kind == "op":
            return self.op(eng, f, reads, writes)
        return self.dma(eng, f, reads, writes)

    def barrier(self):
        toks = []
        for s in ["pe", "dve", "act", "pool"]:
            if self.cnt[s] > 0:
                toks.append((s, self.cnt[s]))
        for q in self.dma_ring:
            for r in range(len(self.dma_ring[q])):
                if self.dma_ring_cnt[q][r] > 0:
                    toks.append(("dma:%s:%d" % (q, r), self.dma_ring_cnt[q][r]))
        for e in ["pe", "dve", "act", "pool", "sp"]:
            for t in toks:
                if e == "pe" and t[0] == "pe":
                    continue
                self._wait(e, t)
        self.res = {}


class _Rec:
    def __init__(self):
        self.call = None

    def __getattr__(self, name):
        def f(*a, **kw):
            self.call = (name, a, kw)
            return self
        return f


def _consts():
    ident = np.eye(128, dtype=np.float32)
    prot = np.zeros((128, 128), np.float32)
    for pp in range(128):
        if (pp % 64) < 32:
            prot[pp + 32, pp] = -1.0
        else:
            prot[pp - 32, pp] = 1.0
    tri = np.triu(np.ones((128, 128), np.float32), 1)
    ones = np.ones((128, 128), np.float32)
    cmat = np.stack([ident, prot, tri, ones], axis=1)
    half = 32
    inv_freq = (10000.0 ** (-np.arange(half, dtype=np.float32) * np.float32(2.0 / 64))).astype(np.float32)
    cvec = np.zeros((128, 128), np.float32)
    cvec[:, 0] = inv_freq[np.arange(128) % 32]
    for g, w in enumerate((2, 4, 8, 16)):
        for t in range(16):
            cvec[:, 16 + g * 16 + t] = (w / (t + 1.0)) if t < w - 1 else 1.0
    cvec[:, 80:112] = (np.arange(NE, dtype=np.float32) * CAP - DUMP)[None, :]
    cvec[64:, 112] = -30000.0
    return np.ascontiguousarray(cmat), cvec


def build_nc(stop=None):
    nc = bass.Bass("TRN2", target_bir_lowering=False)

    def dram(name, shape, dt, kind="ExternalInput"):
        return nc.dram_tensor(name, shape, dt, kind=kind).ap()

    x = dram("x", [NSEQ, S, D], F32)
    pos = dram("pos", [NSEQ, S], I32)
    w_in = dram("w_in", [D, IN_W], F32)
    gate_bias = dram("gate_bias", [128, 16], F32)
    lam_vecs = dram("lam_vecs", [1, 256], F32)
    subln_gain = dram("subln_gain", [1, 128], F32)
    w_pool = dram("w_pool", [4, 128, 128], F32)
    pool_scale = dram("pool_scale", [128, 4], F32)
    w_a = dram("w_a", [D, D], F32)
    w_b = dram("w_b", [512, D], F32)
    w_out = dram("w_out", [D, D], F32)
    ln1_g = dram("ln1_g", [1, D], F32)
    ln1_b = dram("ln1_b", [1, D], F32)
    w_rt = dram("w_rt", [D, 36], F32)
    b_rt = dram("b_rt", [1, 36], F32)
    w_gu = dram("w_gu", [NE, D, D], F32)
    w_dn = dram("w_dn", [NE, 512, D], F32)
    ln2_g = dram("ln2_g", [1, D], F32)
    ln2_b = dram("ln2_b", [1, D], F32)
    cmat = dram("cmat", [128, 4, 128], F32)
    cvec = dram("cvec", [128, 128], F32)
    out = dram("out", [NSEQ, S, D], F32, kind="ExternalOutput")
    h_scr = dram("h_scr", [NSEQ * S, D], F32, kind="Internal")
    xs = dram("xs", [NSLOT, D], BF16, kind="Internal")
    ys = dram("ys", [NSLOT, D], F32, kind="Internal")
    wgu_bf = dram("wgu_bf", [NE, D, D], BF16, kind="Internal")
    wdn_bf = dram("wdn_bf", [NE, 512, D], BF16, kind="Internal")
    dbg = {}
    if stop in ("attn", "a1", "rope", "v", "qk"):
        dbg["oT"] = dram("dbg_oT", [NSEQ, 128, H, S], BF16, kind="ExternalOutput")
    if stop == "mix":
        dbg["mergedT"] = dram("dbg_mergedT", [NSEQ, 128, 8, S], BF16, kind="ExternalOutput")
    if stop in ("ln1",):
        dbg["h"] = dram("dbg_h", [NSEQ * S, D], F32, kind="ExternalOutput")
    if stop in ("ln1", "moe"):
        dbg["slots"] = dram("dbg_slots", [128, 32, 2], I32, kind="ExternalOutput")
        dbg["wts"] = dram("dbg_wts", [128, 32, 2], F32, kind="ExternalOutput")
    if stop == "moe":
        dbg["ys"] = dram("dbg_ys", [NSLOT, D], F32, kind="ExternalOutput")

    w_in_k = w_in.rearrange("(kc k) n -> k kc n", k=128)
    w_a_k = w_a.rearrange("(kc k) n -> k kc n", k=128)
    w_b_k = w_b.rearrange("(kc k) n -> k kc n", k=128)
    w_out_k = w_out.rearrange("(kc k) n -> k kc n", k=128)
    w_rt_k = w_rt.rearrange("(kc k) n -> k kc n", k=128)

    with contextlib.ExitStack() as st:
        P = Prog(nc, st)

        uniq = [0]

        def sb(stack, name, shape, dt):
            uniq[0] += 1
            return stack.enter_context(nc.sbuf_tensor("%s_u%d" % (name, uniq[0]), shape, dt))

        B = [st.enter_context(nc.psum_tensor("bank%d" % i, [128, 512], F32)) for i in range(8)]
        BN = ["B%d" % i for i in range(8)]

        def bank_bf(i):
            return B[i][:].bitcast(BF16)

        cm_f = sb(st, "cm_f", [128, 4, 128], F32)
        cm_b = sb(st, "cm_b", [128, 4, 128], BF16)
        cv = sb(st, "cv", [128, 128], F32)
        lvb = sb(st, "lvb", [128, 256], F32)
        gainb = sb(st, "gainb", [128, 128], F32)
        gbias = sb(st, "gbias", [128, 16], F32)
        pscale = sb(st, "pscale", [128, 4], F32)
        wpool_b = sb(st, "wpool_b", [128, 4, 128], BF16)
        wrt = sb(st, "wrt", [128, 8, 36], F32)
        brt = sb(st, "brt", [128, 36], F32)
        small = sb(st, "small", [128, 16], F32)
        negh = sb(st, "negh", [128, 4], F32)
        cnt = sb(st, "cnt", [128, 32], F32)
        slots_f = sb(st, "slots_f", [128, 32, 2], F32)
        slots_i = sb(st, "slots_i", [128, 32, 2], I32)
        wts = sb(st, "wts", [128, 32, 2], F32)
        zrow = sb(st, "zrow", [128, 1024], F32)
        zb3 = sb(st, "zb3", [128, 1, 1024], BF16)
        xT = sb(st, "xT", [128, 8, S], BF16)
        oT = sb(st, "oT", [128, 8, S], BF16)

        ident_f = cm_f[:, 0, :]
        ident_b = cm_b[:, 0, :]
        prot_b = cm_b[:, 1, :]
        tri_b = cm_b[:, 2, :]
        ones_b = cm_b[:, 3, :]
        invf = cv[:, 0:1]
        eoffmd = cv[:, 80:112]
        lamneg = small[:, 0:1]

        P.dma("sp", lambda e: e.dma_start(out=cm_f[:], in_=cmat[:, :, :]), writes=["cm_f"])
        P.dma("pool", lambda e: e.dma_start(out=cm_b[:], in_=cmat[:, :, :]), writes=["cm_b"])
        P.dma("sp", lambda e: e.dma_start(out=cv[:], in_=cvec[:, :]), writes=["cv"])
        P.dma("sp", lambda e: e.dma_start(out=lvb[:], in_=lam_vecs[0:1, :].to_broadcast([128, 256])), writes=["lvb"])
        P.dma("sp", lambda e: e.dma_start(out=gainb[:], in_=subln_gain[0:1, :].to_broadcast([128, 128])), writes=["gainb"])
        P.dma("sp", lambda e: e.dma_start(out=brt[:], in_=b_rt[0:1, :].to_broadcast([128, 36])), writes=["brt"])
        P.dma("sp", lambda e: e.dma_start(out=gbias[:], in_=gate_bias[:, :]), writes=["gbias"])
        P.dma("sp", lambda e: e.dma_start(out=pscale[:], in_=pool_scale[:, :]), writes=["pscale"])
        P.dma("pool", lambda e: e.dma_start(out=wpool_b[:], in_=w_pool.rearrange("g c d -> c g d")), writes=["wpool_b"])
        P.dma("sp", lambda e: e.dma_start(out=wrt[:], in_=w_rt_k), writes=["wrt"])
        P.op("dve", lambda e: e.memset(cnt[:], 0.0), writes=["cnt"])
        P.op("dve", lambda e: e.memset(slots_i[:], 0), writes=["slots_i_init"])
        P.op("dve", lambda e: e.memset(wts[:], 0.0), writes=["wts_init"])
        P.op("dve", lambda e: e.memset(zrow[:], 0.0), writes=["zrow"])
        P.op("dve", lambda e: e.memset(negh[:], -0.5), writes=["negh"])
        P.op("dve", lambda e: e.tensor_scalar(gainb[:], gainb[:], 1.0 - LAMBDA_INIT, None, op0=ALU.mult),
             reads=["gainb"], writes=["gainb"])
        P.op("dve", lambda e: e.memset(zb3[:], 0.0), writes=["zb3"])
        xs_v = xs.rearrange("(n p) d -> p n d", p=128)
        n_tot = NSLOT // 128
        xs_init = [("xs_init", n0) for n0 in range(0, n_tot, 25)]

        def emit_xs_zero_fill():
            for n0 in range(0, n_tot, 25):
                n1 = min(n_tot, n0 + 25)
                P.dma("sp", lambda e: e.dma_start(out=xs_v[:, n0:n1, :], in_=zb3[:].to_broadcast([128, n1 - n0, 1024])),
                      reads=["zb3"], writes=[("xs_init", n0)])

        P.dma("sp", lambda e: e.dma_start(out=ys[DUMP:DUMP + 128, :], in_=zrow[:]), reads=["zrow"], writes=["ys_dump"])
        P.op("dve", lambda e: e.tensor_tensor(lvb[:, 0:64], lvb[:, 0:64], lvb[:, 64:128], op=ALU.mult),
             reads=["lvb"], writes=["lvb"])
        P.op("dve", lambda e: e.tensor_tensor(lvb[:, 128:192], lvb[:, 128:192], lvb[:, 192:256], op=ALU.mult),
             reads=["lvb"], writes=["lvb"])
        P.op("dve", lambda e: e.reduce_sum(small[:, 1:2], lvb[:, 0:64], axis=AX.X), reads=["lvb"], writes=["small"])
        P.op("dve", lambda e: e.reduce_sum(small[:, 2:3], lvb[:, 128:192], axis=AX.X), reads=["lvb", "small"], writes=["small"])
        P.op("act", lambda e: e.activation(out=small[:, 3:5], in_=small[:, 1:3], func=AF.Exp), reads=["small"], writes=["small"])
        P.op("dve", lambda e: e.tensor_tensor(small[:, 5:6], small[:, 4:5], small[:, 3:4], op=ALU.subtract),
             reads=["small"], writes=["small"])
        P.op("dve", lambda e: e.tensor_scalar(small[:, 0:1], small[:, 5:6], -LAMBDA_INIT, None, op0=ALU.add),
             reads=["small"], writes=["small"])

        ev_flip = [0]

        def evac(out_ap, in_ap, reads, writes, eng=None):
            if eng is None:
                eng = "act" if ev_flip[0] % 2 == 0 else "dve"
                ev_flip[0] += 1
            if eng == "act":
                return P.op("act", lambda e: e.activation(out=out_ap, in_=in_ap, func=AF.Copy), reads=reads, writes=writes)
            return P.op("dve", lambda e: e.tensor_copy(out_ap, in_ap), reads=reads, writes=writes)

        def pipeline(n_items, stages):
            for step in range(n_items + len(stages) - 1):
                lists = []
                for si, f in enumerate(stages):
                    k = step - si
                    if 0 <= k < n_items:
                        lists.append(P.record(f, k))
                lists.reverse()
                pos = [0] * len(lists)
                left = sum(len(l) for l in lists)
                while left:
                    for li, l in enumerate(lists):
                        if pos[li] < len(l):
                            P.replay(l[pos[li]])
                            pos[li] += 1
                            left -= 1

        conv_jobs = []
        for e_ in range(NE):
            conv_jobs.append((wgu_bf[e_, 0:512, :], w_gu[e_, 0:512, :], ("wcv", e_, 0)))
            conv_jobs.append((wgu_bf[e_, 512:1024, :], w_gu[e_, 512:1024, :], ("wcv", e_, 1)))
            conv_jobs.append((wdn_bf[e_, :, :], w_dn[e_, :, :], ("wcv", e_, 2)))
        conv_next = [0]
        conv_tick = [0]

        def conv_issue(n=1):
            for _ in range(n):
                if conv_next[0] < len(conv_jobs):
                    o_, i_, nm_ = conv_jobs[conv_next[0]]
                    conv_next[0] += 1
                    P.dma("poolc", lambda e: e.dma_start(out=o_, in_=i_), writes=[nm_])

        for s in range(NSEQ):
            s2 = contextlib.ExitStack()
            with s2:
                V = sb(s2, "V", [128, NT, H, 129], BF16)
                cosT = sb(s2, "cosT", [128, S], F32)
                sinT = sb(s2, "sinT", [128, S], F32)
                with contextlib.ExitStack() as s2a:
                    xt = [sb(s2a, "xt%d" % i, [128, D], F32) for i in range(2)]
                    posi = sb(s2a, "posi", [128, S], I32)
                    ang = sb(s2a, "ang", [128, S], F32)
                    ras = [sb(s2a, "ra%d" % i, [128, S], F32) for i in range(2)]
                    rk = sb(s2a, "rk", [128, S], F32)
                    ki = sb(s2a, "ki", [128, S], I32)
                    wv = sb(s2a, "wv", [128, 8, D], BF16)
                    for kc2 in range(2):
                        P.dma("pool", lambda e: e.dma_start(out=wv[:, kc2 * 4:(kc2 + 1) * 4, :],
                                                            in_=w_in_k[:, kc2 * 4:(kc2 + 1) * 4, 2048:3072]),
                              writes=[("wv", kc2)])
                    P.dma("sp", lambda e: e.dma_start(out=posi[:], in_=pos[s:s + 1, :].to_broadcast([128, S])), writes=["posi"])
                    P.op("pool", lambda e: e.memset(V[:, :, :, 128:129], 1.0), writes=["Vones"])
                    P.op("dve", lambda e: e.tensor_copy(ang[:], posi[:]), reads=["posi"], writes=["ang"])
                    P.op("dve", lambda e: e.tensor_scalar(ang[:], ang[:], invf, None, op0=ALU.mult), reads=["ang", "cv"], writes=["ang"])
                    for which in range(2):
                        ra, ran = ras[which], "ra%d" % which
                        shift = 0.0 if which == 0 else 0.5 * math.pi
                        P.op("dve", lambda e: e.tensor_scalar(ra[:], ang[:], shift, None, op0=ALU.add), reads=["ang"], writes=[ran])
                        P.op("dve", lambda e: e.tensor_scalar(rk[:], ra[:], 1.0 / TWO_PI, None, op0=ALU.mult), reads=[ran], writes=["rk"])
                        P.op("dve", lambda e: e.tensor_copy(ki[:], rk[:]), reads=["rk"], writes=["ki"])
                        P.op("dve", lambda e: e.tensor_copy(rk[:], ki[:]), reads=["ki"], writes=["rk"])
                        P.op("dve", lambda e: e.scalar_tensor_tensor(ra[:], rk[:], -C1, ra[:], op0=ALU.mult, op1=ALU.add),
                             reads=["rk", ran], writes=[ran])
                        P.op("dve", lambda e: e.scalar_tensor_tensor(ra[:], rk[:], -C2, ra[:], op0=ALU.mult, op1=ALU.add),
                             reads=["rk", ran], writes=[ran])
                        P.op("dve", lambda e: e.tensor_scalar(rk[:], ra[:], math.pi, -TWO_PI, op0=ALU.is_gt, op1=ALU.mult),
                             reads=[ran], writes=["rk"])
                        P.op("dve", lambda e: e.tensor_tensor(ra[:], ra[:], rk[:], op=ALU.add), reads=[ran, "rk"], writes=[ran])
                        P.op("dve", lambda e: e.tensor_scalar(rk[:], ra[:], -math.pi, TWO_PI, op0=ALU.is_lt, op1=ALU.mult),
                             reads=[ran], writes=["rk"])
                        P.op("dve", lambda e: e.tensor_tensor(ra[:], ra[:], rk[:], op=ALU.add), reads=[ran, "rk"], writes=[ran])
                        P.op("dve", lambda e: e.tensor_scalar(ra[:], ra[:], 3.1415925, -3.1415925, op0=ALU.min, op1=ALU.max),
                             reads=[ran], writes=[ran])
                    for t in range(NT):
                        xb_ = xt[t % 2]
                        xn = "xt%d" % (t % 2)
                        P.dma("sp", lambda e: e.dma_start(out=xb_[:], in_=x[s, t * 128:(t + 1) * 128, :]), writes=[xn])
                        for hf in range(2):
                            bk = (t * 2 + hf) % 4
                            for j in range(4):
                                kc = hf * 4 + j
                                P.op("pe", lambda e: e.transpose(B[bk][:, j * 128:(j + 1) * 128], xb_[:, kc * 128:(kc + 1) * 128], ident_f),
                                     reads=[xn, "cm_f"], writes=[BN[bk]])
                            evac(xT[:, hf * 4:(hf + 1) * 4, t * 128:(t + 1) * 128],
                                 B[bk][:].rearrange("p (a b) -> p a b", a=4), reads=[BN[bk]], writes=[("xT", t)], eng="act")
                    P.op("act", lambda e: e.activation(out=sinT[:], in_=ras[0][:], func=AF.Sin), reads=["ra0"], writes=["sinT"])
                    P.op("act", lambda e: e.activation(out=cosT[:], in_=ras[1][:], func=AF.Sin), reads=["ra1"], writes=["cosT"])
                    if stop == "a1":
                        P.dma("sp", lambda e: e.dma_start(out=dbg["oT"][s], in_=xT[:]), reads=[("xT", t_) for t_ in range(NT)], writes=["dbg"])
                    for t in range(NT):
                        for hf in range(2):
                            bk = 4 + (t * 2 + hf) % 4
                            for kc in range(8):
                                P.op("pe", lambda e: e.matmul(B[bk][:], lhsT=xT[:, kc, t * 128:(t + 1) * 128],
                                                              rhs=wv[:, kc, hf * 512:(hf + 1) * 512],
                                                              start=(kc == 0), stop=(kc == 7)),
                                     reads=[("xT", t), ("wv", kc // 4)], writes=[BN[bk]])
                            evac(V[:, t, hf * 4:(hf + 1) * 4, 0:128], B[bk][:].rearrange("p (a b) -> p a b", a=4),
                                 reads=[BN[bk]], writes=[("V", t)])
                    P.barrier()
                if s == 0:
                    emit_xs_zero_fill()
                if stop in ("a1", "rope", "v"):
                    P.barrier()
                    continue
                with contextlib.ExitStack() as s2c:
                    wq = [sb(s2c, "wq%d" % i, [128, 8, 128], BF16) for i in range(2)]
                    wk = [sb(s2c, "wk%d" % i, [128, 8, 128], BF16) for i in range(2)]
                    qT = [sb(s2c, "qT%d" % i, [128, S], BF16) for i in range(2)]
                    kz = [[sb(s2c, "kz%d_%d" % (i, m), [128, S], BF16) for m in range(2)] for i in range(2)]
                    qb = [sb(s2c, "qb%d" % i, [128, 512], BF16) for i in range(2)]
                    t1 = [sb(s2c, "t1_%d" % i, [128, 512], F32) for i in range(2)]
                    t2 = [sb(s2c, "t2_%d" % i, [128, 512], F32) for i in range(2)]
                    PT = [sb(s2c, "PT%d" % i, [128, 512], BF16) for i in range(4)]
                    accs = [sb(s2c, "accs%d" % i, [128, 1032], F32) for i in range(2)]
                    rec3 = sb(s2c, "rec3", [128, 4, 2, 1], F32)
                    O4 = sb(s2c, "O4", [128, 4, 128], F32)
                    T4 = sb(s2c, "T4", [128, 4, 128], F32)
                    ss = sb(s2c, "ss", [128, 4], F32)
                    ssv = sb(s2c, "ssv", [128, 4], F32)
                    rstd3 = sb(s2c, "rstd3", [128, 4, 1], F32)
                    onb = [sb(s2c, "onb%d" % i, [128, 4, 128], BF16) for i in range(2)]
                    rope_i = [0]
                    NG = S // 512
                    for i_ in range(2):
                        P.op("pool", lambda e: e.memset(kz[i_][0][64:128, :], 0.0), writes=[("kzz", i_, 0)])
                        P.op("pool", lambda e: e.memset(kz[i_][1][0:64, :], 0.0), writes=[("kzz", i_, 1)])

                    def load_head_w(h):
                        b = h % 2
                        P.dma("pool", lambda e: e.dma_start(out=wq[b][:], in_=w_in_k[:, :, h * 128:(h + 1) * 128]),
                              writes=["wq%d" % b])
                        P.dma("pool", lambda e: e.dma_start(out=wk[b][:], in_=w_in_k[:, :, 1024 + h * 128:1024 + (h + 1) * 128]),
                              writes=["wk%d" % b])

                    def proj_units(h):
                        hb = h % 2
                        units = []
                        for which in range(2):
                            w_t = (wq if which == 0 else wk)[hb]
                            wname = ("wq%d" if which == 0 else "wk%d") % hb
                            for tg in range(NG):
                                st_ = {}

                                def part_a(w_t=w_t, wname=wname, tg=tg, st_=st_):
                                    i = rope_i[0]
                                    rope_i[0] += 1
                                    st_["i"] = i
                                    r = i % 2
                                    pb = 3 if i % 2 == 0 else 7
                                    tsl = slice(tg * 512, (tg + 1) * 512)
                                    for kc in range(8):
                                        P.op("pe", lambda e: e.matmul(B[pb][:], lhsT=w_t[:, kc, :], rhs=xT[:, kc, tsl],
                                                                      start=(kc == 0), stop=(kc == 7)),
                                             reads=[wname], writes=[BN[pb]])
                                    P.op("act", lambda e: e.activation(out=qb[r][:], in_=B[pb][:], func=AF.Copy),
                                         reads=[BN[pb]], writes=["qb%d" % r])
                                    P.op("dve", lambda e: e.tensor_tensor(t1[r][:], B[pb][:], cosT[:, tsl], op=ALU.mult),
                                         reads=[BN[pb], "cosT"], writes=["t1_%d" % r])

                                def part_b(which=which, tg=tg, st_=st_, hb=hb):
                                    i = st_["i"]
                                    r = i % 2
                                    pb = 3 if i % 2 == 0 else 7
                                    tsl = slice(tg * 512, (tg + 1) * 512)
                                    P.op("pe", lambda e: e.matmul(B[pb][:], lhsT=prot_b, rhs=qb[r][:], start=True, stop=True),
                                         reads=["qb%d" % r, "cm_b"], writes=[BN[pb]])
                                    P.op("dve", lambda e: e.tensor_tensor(t2[r][:], B[pb][:], sinT[:, tsl], op=ALU.mult),
                                         reads=[BN[pb], "sinT"], writes=["t2_%d" % r])
                                    if which == 0:
                                        P.op("dve", lambda e: e.tensor_tensor(qT[hb][:, tsl], t1[r][:], t2[r][:], op=ALU.add),
                                             reads=["t1_%d" % r, "t2_%d" % r], writes=[("qT%d" % hb, tg)])
                                    else:
                                        for m in range(2):
                                            ps_ = slice(m * 64, (m + 1) * 64)
                                            P.op("dve", lambda e: e.tensor_tensor(kz[hb][m][ps_, tsl], t1[r][ps_, :], t2[r][ps_, :], op=ALU.add),
                                                 reads=["t1_%d" % r, "t2_%d" % r, ("kzz", hb, m)], writes=[("kz%d_%d" % (hb, m), tg)])
                                units.append(part_a)
                                units.append(part_b)
                        return units

                    LOOK = 2
                    gstep = [0]
                    deferred = []

                    def run_due(force=False):
                        keep = []
                        for due, fn in deferred:
                            if force or due <= gstep[0]:
                                fn()
                            else:
                                keep.append((due, fn))
                        deferred[:] = keep

                    def defer(delay, fn):
                        deferred.append((gstep[0] + delay, fn))

                    grp_i = [0]
                    load_head_w(0)
                    for u in proj_units(0):
                        u()
                    for h in range(H):
                        hb = h % 2
                        if h + 1 < H:
                            load_head_w(h + 1)
                            nxt = proj_units(h + 1)
                        else:
                            nxt = []
                        qn = "qT%d" % hb
                        steps = []
                        for g in range(NG):
                            for j in range(4 * g + 4):
                                for m in range(2):
                                    steps.append((g, j, m))
                        nsteps = len(steps)
                        unit_at = {}
                        if nxt:
                            gap = max(1, (nsteps - 8) // len(nxt))
                            for ui, u in enumerate(nxt):
                                unit_at.setdefault(4 + ui * gap, []).append(u)
                        started = {}
                        pv_pending = []

                        def emit_pv(g, j, m, pt, ptn, q0):
                            for il in range(q0 - 4 * g, 4):
                                a = il * 2 + m
                                ab = 4 + a // 3
                                col = (a % 3) * 129
                                c0 = (il - (q0 - 4 * g)) * 128
                                first = (g, ab) not in started
                                started[(g, ab)] = True
                                P.op("pe", lambda e: e.matmul(B[ab][:, col:col + 129], lhsT=pt[:, c0:c0 + 128],
                                                              rhs=V[:, j, h, :], start=first, stop=(j == 4 * g + il),
                                                              skip_group_check=True),
                                     reads=[ptn, (ptn, "m"), ("V", j), "Vones"], writes=[BN[ab]])
                            if j == 4 * g + 3 and m == 1:
                                emit_group_end(g)

                        def emit_group_end(g, h=h):
                            gi = grp_i[0]
                            grp_i[0] += 1
                            ac = accs[gi % 2]
                            acn = "accs%d" % (gi % 2)
                            evac(ac[:, 0:387], B[4][:, 0:387], reads=[BN[4]], writes=[(acn, 0)], eng="dve")
                            evac(ac[:, 387:774], B[5][:, 0:387], reads=[BN[5]], writes=[(acn, 1)], eng="act")
                            evac(ac[:, 774:1032], B[6][:, 0:258], reads=[BN[6]], writes=[(acn, 2)], eng="dve")
                            acv = ac[:, :].rearrange("p (i m c) -> p i m c", i=4, m=2)
                            acr = [(acn, 0), (acn, 1), (acn, 2)]

                            def norm_1():
                                P.op("dve", lambda e: e.reciprocal(rec3[:, :, :, 0], acv[:, :, :, 128]), reads=acr, writes=["rec"])
                                P.op("dve", lambda e: e.tensor_scalar(rec3[:, :, 1, 0], rec3[:, :, 1, 0], lamneg, None, op0=ALU.mult),
                                     reads=["rec", "small"], writes=["rec"])
                                P.op("dve", lambda e: e.tensor_tensor(O4[:], acv[:, :, 0, 0:128], rec3[:, :, 0, :].to_broadcast([128, 4, 128]), op=ALU.mult),
                                     reads=acr + ["rec"], writes=["O4"])
                                P.op("dve", lambda e: e.tensor_tensor(T4[:], acv[:, :, 1, 0:128], rec3[:, :, 1, :].to_broadcast([128, 4, 128]), op=ALU.mult),
                                     reads=acr + ["rec"], writes=["T4"])

                            def norm_2():
                                P.op("dve", lambda e: e.tensor_tensor(O4[:], O4[:], T4[:], op=ALU.add), reads=["O4", "T4"], writes=["O4"])
                                P.op("dve", lambda e: e.tensor_tensor(T4[:], O4[:], O4[:], op=ALU.mult), reads=["O4"], writes=["T4"])
                                P.op("dve", lambda e: e.reduce_sum(ss[:], T4[:], axis=AX.X), reads=["T4"], writes=["ss"])
                                P.op("dve", lambda e: e.tensor_scalar(ssv[:], ss[:], 1.0 / 128.0, RMS_EPS, op0=ALU.mult, op1=ALU.add),
                                     reads=["ss"], writes=["ssv"])
                                P.op("pool", lambda e: e.tensor_tensor(rstd3[:, :, 0], ssv[:], negh[:], op=ALU.pow), reads=["ssv", "negh"], writes=["rstd"])

                            def norm_on():
                                ob = onb[gi % 2]
                                obn = "onb%d" % (gi % 2)
                                P.op("dve", lambda e: e.tensor_tensor(O4[:], O4[:], rstd3[:].to_broadcast([128, 4, 128]), op=ALU.mult),
                                     reads=["O4", "rstd"], writes=["O4"])
                                P.op("dve", lambda e: e.tensor_tensor(ob[:], O4[:], gainb[:, None, :].to_broadcast([128, 4, 128]), op=ALU.mult),
                                     reads=["O4", "gainb"], writes=[obn])

                            def norm_b():
                                ob = onb[gi % 2]
                                obn = "onb%d" % (gi % 2)
                                for il in range(4):
                                    P.op("pe", lambda e: e.transpose(bank_bf(7)[:, il * 128:(il + 1) * 128], ob[:, il, :], ident_b),
                                         reads=[obn, "cm_b"], writes=[BN[7]])
                                evac(oT[:, h, g * 512:(g + 1) * 512], bank_bf(7)[:, 0:512], reads=[BN[7]], writes=[("oT", g)])

                            defer(1, norm_1)
                            defer(3, norm_2)
                            defer(6, norm_on)
                            defer(11, norm_b)

                        for si, (g, j, m) in enumerate(steps):
                            gstep[0] += 1
                            run_due()
                            conv_tick[0] += 1
                            if conv_tick[0] % CONV_EVERY == 0:
                                conv_issue()
                            for u in unit_at.get(si, []):
                                u()
                            q0 = max(j, 4 * g)
                            N = (4 * g + 4 - q0) * 128
                            qsl = slice(q0 * 128, (4 * g + 4) * 128)
                            i = gstep[0]
                            bk = i % 3
                            pt = PT[i % 4]
                            ptn = "PT%d" % (i % 4)
                            P.op("pe", lambda e: e.matmul(B[bk][:, 0:N], lhsT=kz[hb][m][:, j * 128:(j + 1) * 128],
                                                          rhs=qT[hb][:, qsl], start=True, stop=True),
                                 reads=[("kz%d_%d" % (hb, m), j // 4), ("kzz", hb, m), (qn, g)], writes=[BN[bk]])
                            if j >= 4 * g:
                                P.op("act", lambda e: e.activation(out=pt[:, 0:64], in_=B[bk][:, 0:64], func=AF.Exp, scale=0.125, bias=cv[:, 112:113]),
                                     reads=[BN[bk], "cv"], writes=[(ptn, "m")])
                                P.op("act", lambda e: e.activation(out=pt[:, 64:N], in_=B[bk][:, 64:N], func=AF.Exp, scale=0.125),
                                     reads=[BN[bk]], writes=[ptn])
                            else:
                                P.op("act", lambda e: e.activation(out=pt[:, 0:N], in_=B[bk][:, 0:N], func=AF.Exp, scale=0.125),
                                     reads=[BN[bk]], writes=[ptn, (ptn, "m")])
                            pv_pending.append((g, j, m, pt, ptn, q0))
                            if len(pv_pending) > LOOK:
                                emit_pv(*pv_pending.pop(0))
                        while pv_pending:
                            emit_pv(*pv_pending.pop(0))
                        for si_ in sorted(unit_at):
                            if si_ >= nsteps:
                                for u in unit_at[si_]:
                                    u()
                    run_due(force=True)
                    if s == NSEQ - 1:
                        conv_issue(len(conv_jobs))
                    P.barrier()
            if stop in ("attn", "qk"):
                P.dma("sp", lambda e: e.dma_start(out=dbg["oT"][s], in_=oT[:]), reads=[], writes=["dbg"])
                P.barrier()
                continue
            with contextlib.ExitStack() as s3:
                mergedT = sb(s3, "mergedT", [128, 8, S], BF16)
                mixedT = sb(s3, "mixedT", [128, 4, S], BF16)
                NG = S // 512
                with contextlib.ExitStack() as s3a:
                    wu = sb(s3a, "wu", [128, 8, 512], BF16)
                    uT = sb(s3a, "uT", [128, 4, S], F32)
                    Tp = [sb(s3a, "Tp%d" % i, [128, S], F32) for i in range(2)]
                    pooledT = sb(s3a, "pooledT", [128, 4, S], BF16)
                    for kc2 in range(2):
                        P.dma("pool", lambda e: e.dma_start(out=wu[:, kc2 * 4:(kc2 + 1) * 4, :],
                                                            in_=w_in_k[:, kc2 * 4:(kc2 + 1) * 4, 3072:3584]),
                              writes=[("wu", kc2)])
                    bi = 0
                    for g in range(4):
                        for tg in range(NG):
                            bk = bi % 4
                            bi += 1
                            tsl = slice(tg * 512, (tg + 1) * 512)
                            for kc in range(8):
                                P.op("pe", lambda e: e.matmul(B[bk][:], lhsT=wu[:, kc, g * 128:(g + 1) * 128], rhs=xT[:, kc, tsl],
                                                              start=(kc == 0), stop=(kc == 7)),
                                     reads=[("wu", kc // 4)], writes=[BN[bk]])
                            evac(uT[:, g, tsl], B[bk][:], reads=[BN[bk]], writes=[("uT", g)])
                    for g, w in enumerate((2, 4, 8, 16)):
                        cur, cur_r = uT[:, g, :], [("uT", g)]
                        k = 0
                        sh = 1
                        while sh < w:
                            dst, dstn = Tp[k % 2], "Tp%d" % (k % 2)
                            eng = "dve"
                            P.op(eng, lambda e: e.tensor_tensor(dst[:, sh:S], cur[:, sh:S], cur[:, 0:S - sh], op=ALU.add),
                                 reads=cur_r, writes=[dstn])
                            P.op(eng, lambda e: e.tensor_copy(dst[:, 0:sh], cur[:, 0:sh]), reads=cur_r, writes=[dstn + "h"])
                            cur, cur_r = dst[:, :], [dstn, dstn + "h"]
                            k += 1
                            sh *= 2
                        P.op("dve", lambda e: e.tensor_tensor(cur[:, 0:16], cur[:, 0:16], cv[:, 16 + g * 16:32 + g * 16], op=ALU.mult),
                             reads=cur_r + ["cv"], writes=cur_r)
                        P.op("dve", lambda e: e.scalar_tensor_tensor(pooledT[:, g, :], cur, 1.0 / w, uT[:, g, :], op0=ALU.mult, op1=ALU.subtract),
                             reads=cur_r + [("uT", g)], writes=[("pooledT", g)])
                    for g in range(4):
                        for tg in range(NG):
                            bk = bi % 4
                            bi += 1
                            tsl = slice(tg * 512, (tg + 1) * 512)
                            P.op("pe", lambda e: e.matmul(B[bk][:], lhsT=wpool_b[:, g, :], rhs=pooledT[:, g, tsl], start=True, stop=True),
                                 reads=[("pooledT", g), "wpool_b"], writes=[BN[bk]])
                            P.op("dve", lambda e: e.tensor_scalar(mixedT[:, g, tsl], B[bk][:], pscale[:, g:g + 1], None, op0=ALU.mult),
                                 reads=[BN[bk], "pscale"], writes=[("mixedT", g)])
                    P.barrier()
                with contextlib.ExitStack() as s3b:
                    wa_c = [sb(s3b, "wa_c%d" % i, [128, 8, 128], BF16) for i in range(2)]
                    wb_c = [sb(s3b, "wb_c%d" % i, [128, 4, 128], BF16) for i in range(2)]
                    wga_c = [sb(s3b, "wga_c%d" % i, [128, 8, 128], BF16) for i in range(2)]
                    wgb_c = [sb(s3b, "wgb_c%d" % i, [128, 8, 128], BF16) for i in range(2)]
                    ga = [sb(s3b, "ga%d" % i, [128, 512], F32) for i in range(2)]
                    gb = [sb(s3b, "gb%d" % i, [128, 512], F32) for i in range(2)]
                    m1 = [sb(s3b, "m1_%d" % i, [128, 512], F32) for i in range(2)]
                    m2 = [sb(s3b, "m2_%d" % i, [128, 512], F32) for i in range(2)]

                    def load_chunk_w(c):
                        b = c % 2
                        cs = slice(c * 128, (c + 1) * 128)
                        P.dma("pool", lambda e: e.dma_start(out=wa_c[b][:], in_=w_a_k[:, :, cs]), writes=["wa_c%d" % b])
                        P.dma("pool", lambda e: e.dma_start(out=wb_c[b][:], in_=w_b_k[:, :, cs]), writes=["wb_c%d" % b])
                        P.dma("pool", lambda e: e.dma_start(out=wga_c[b][:], in_=w_in_k[:, :, 3584 + c * 128:3584 + (c + 1) * 128]),
                              writes=["wga_c%d" % b])
                        P.dma("pool", lambda e: e.dma_start(out=wgb_c[b][:], in_=w_in_k[:, :, 4608 + c * 128:4608 + (c + 1) * 128]),
                              writes=["wgb_c%d" % b])

                    load_chunk_w(0)
                    it = 0
                    for c in range(8):
                        cb = c % 2
                        if c + 1 < 8:
                            load_chunk_w(c + 1)
                        for tg in range(NG):
                            r = it % 2
                            it += 1
                            b0 = r * 4
                            tsl = slice(tg * 512, (tg + 1) * 512)
                            for hh_ in range(8):
                                P.op("pe", lambda e: e.matmul(B[b0][:], lhsT=wa_c[cb][:, hh_, :], rhs=oT[:, hh_, tsl],
                                                              start=(hh_ == 0), stop=(hh_ == 7)),
                                     reads=["wa_c%d" % cb], writes=[BN[b0]])
                            for g in range(4):
                                P.op("pe", lambda e: e.matmul(B[b0 + 1][:], lhsT=wb_c[cb][:, g, :], rhs=mixedT[:, g, tsl],
                                                              start=(g == 0), stop=(g == 3)),
                                     reads=["wb_c%d" % cb], writes=[BN[b0 + 1]])
                            for kc in range(8):
                                P.op("pe", lambda e: e.matmul(B[b0 + 2][:], lhsT=wga_c[cb][:, kc, :], rhs=xT[:, kc, tsl],
                                                              start=(kc == 0), stop=(kc == 7)),
                                     reads=["wga_c%d" % cb], writes=[BN[b0 + 2]])
                            for kc in range(8):
                                P.op("pe", lambda e: e.matmul(B[b0 + 3][:], lhsT=wgb_c[cb][:, kc, :], rhs=xT[:, kc, tsl],
                                                              start=(kc == 0), stop=(kc == 7)),
                                     reads=["wgb_c%d" % cb], writes=[BN[b0 + 3]])
                            P.op("act", lambda e: e.activation(out=ga[r][:], in_=B[b0 + 2][:], func=AF.Sigmoid, bias=gbias[:, c:c + 1]),
                                 reads=[BN[b0 + 2], "gbias"], writes=["ga%d" % r])
                            P.op("act", lambda e: e.activation(out=gb[r][:], in_=B[b0 + 3][:], func=AF.Sigmoid, bias=gbias[:, 8 + c:9 + c]),
                                 reads=[BN[b0 + 3], "gbias"], writes=["gb%d" % r])
                            P.op("dve", lambda e: e.tensor_tensor(m1[r][:], B[b0][:], ga[r][:], op=ALU.mult),
                                 reads=[BN[b0], "ga%d" % r], writes=["m1_%d" % r])
                            P.op("dve", lambda e: e.tensor_tensor(m2[r][:], B[b0 + 1][:], gb[r][:], op=ALU.mult),
                                 reads=[BN[b0 + 1], "gb%d" % r], writes=["m2_%d" % r])
                            P.op("pool", lambda e: e.tensor_tensor(mergedT[:, c, tsl], m1[r][:], m2[r][:], op=ALU.add),
                                 reads=["m1_%d" % r, "m2_%d" % r], writes=[("mergedT", c)])
                    P.barrier()
                if stop == "mix":
                    P.dma("sp", lambda e: e.dma_start(out=dbg["mergedT"][s], in_=mergedT[:]), reads=[], writes=["dbg"])
                    P.barrier()
                    continue
                with contextlib.ExitStack() as s3c:
                    wout = sb(s3c, "wout", [128, 8, D], BF16)
                    g1 = sb(s3c, "g1", [128, D], F32)
                    b1 = sb(s3c, "b1", [128, D], F32)
                    xt = [sb(s3c, "xt%d" % i, [128, D], F32) for i in range(2)]
                    zs = [sb(s3c, "z%d" % i, [128, D], F32) for i in range(3)]
                    hh = [sb(s3c, "hh%d" % i, [128, D], F32) for i in range(3)]
                    hbf = [sb(s3c, "hbf%d" % i, [128, D], BF16) for i in range(4)]
                    hTs = [sb(s3c, "hT%d" % i, [128, 8, 128], F32) for i in range(2)]
                    statss = [sb(s3c, "stats%d" % i, [128, 2, 6], F32) for i in range(3)]
                    mvs = [sb(s3c, "mv%d" % i, [128, 4], F32) for i in range(3)]
                    rrs = [sb(s3c, "rr%d" % i, [128, 16], F32) for i in range(2)]
                    nm = sb(s3c, "nm", [128, 3], F32)
                    lg = sb(s3c, "lg", [128, 36], F32)
                    ge = sb(s3c, "ge", [128, 4], F32)
                    gone = sb(s3c, "gone", [128, 4], F32)
                    mneg = sb(s3c, "mneg", [128, 4, 1], F32)
                    elms = [sb(s3c, "elm%d" % i, [128, 4, 8], F32) for i in range(2)]
                    elm2 = sb(s3c, "elm2", [128, 32], F32)
                    oh1s = [sb(s3c, "oh1_%d" % i, [128, 32], F32) for i in range(3)]
                    oh2s = [sb(s3c, "oh2_%d" % i, [128, 32], F32) for i in range(2)]
                    Mbs = [sb(s3c, "Mb%d" % i, [128, 32], BF16) for i in range(2)]
                    posin = sb(s3c, "posin", [128, 32], F32)
                    valid = sb(s3c, "valid", [128, 32], F32)
                    smv = sb(s3c, "smv", [128, 32], F32)
                    tmp32 = sb(s3c, "tmp32", [128, 32], F32)
                    for kc2 in range(2):
                        P.dma("pool", lambda e: e.dma_start(out=wout[:, kc2 * 4:(kc2 + 1) * 4, :], in_=w_out_k[:, kc2 * 4:(kc2 + 1) * 4, :]),
                              writes=[("wout", kc2)])
                    P.dma("sp", lambda e: e.dma_start(out=g1[:], in_=ln1_g[0:1, :].to_broadcast([128, D])), writes=["g1"])
                    P.dma("sp", lambda e: e.dma_start(out=b1[:], in_=ln1_b[0:1, :].to_broadcast([128, D])), writes=["b1"])

                    def L0a(t):
                        r = t % 2
                        xb_, xn = xt[r], "xt%d" % r
                        P.dma("sp", lambda e: e.dma_start(out=xb_[:], in_=x[s, t * 128:(t + 1) * 128, :]), writes=[xn])
                        for hf in range(2):
                            bk = (0, 1)[hf] if r == 0 else (6, 7)[hf]
                            for kc in range(8):
                                P.op("pe", lambda e: e.matmul(B[bk][:], lhsT=mergedT[:, kc, t * 128:(t + 1) * 128],
                                                              rhs=wout[:, kc, hf * 512:(hf + 1) * 512], start=(kc == 0), stop=(kc == 7)),
                                     reads=[("wout", kc // 4)], writes=[BN[bk]])

                    def L0b(t):
                        r = t % 2
                        q3 = t % 3
                        xb_, xn = xt[r], "xt%d" % r
                        z, stats, mv = zs[q3], statss[q3], mvs[q3]
                        zn = "z%d" % q3
                        for hf in range(2):
                            bk = (0, 1)[hf] if r == 0 else (6, 7)[hf]
                            P.op("dve", lambda e: e.scalar_tensor_tensor(z[:, hf * 512:(hf + 1) * 512], xb_[:, hf * 512:(hf + 1) * 512], ALPHA, B[bk][:],
                                                                         op0=ALU.mult, op1=ALU.add),
                                 reads=[xn, BN[bk]], writes=[(zn, hf)])
                            P.op("dve", lambda e: e.bn_stats(stats[:, hf, :], z[:, hf * 512:(hf + 1) * 512]), reads=[(zn, hf)], writes=[("stats", q3, hf)])
                        P.op("dve", lambda e: e.bn_aggr(mv[:, 0:2], stats[:].rearrange("p a b -> p (a b)")), reads=[("stats", q3, 0), ("stats", q3, 1)], writes=[("mv", q3)])
                        P.op("dve", lambda e: e.tensor_scalar(mv[:, 2:3], mv[:, 1:2], LN_EPS, None, op0=ALU.add), reads=[("mv", q3)], writes=[("mv2", q3)])

                    def L1a(t):
                        q3 = t % 3
                        z, mv = zs[q3], mvs[q3]
                        zn = "z%d" % q3
                        hb_, hn = hh[q3], "hh%d" % q3
                        P.op("pool", lambda e: e.tensor_tensor(mv[:, 3:4], mv[:, 2:3], negh[:, 0:1], op=ALU.pow), reads=[("mv2", q3), "negh"], writes=[("mv3", q3)])
                        P.op("dve", lambda e: e.scalar_tensor_tensor(nm[:, q3:q3 + 1], mv[:, 0:1], -1.0, mv[:, 3:4], op0=ALU.mult, op1=ALU.mult),
                             reads=[("mv", q3), ("mv3", q3)], writes=[("nm", q3)])
                        P.op("act", lambda e: e.activation(out=hb_[:], in_=z[:], func=AF.Identity, scale=mv[:, 3:4], bias=nm[:, q3:q3 + 1]),
                             reads=[(zn, 0), (zn, 1), ("mv3", q3), ("nm", q3)], writes=[hn])

                    def L1a2(t):
                        q3 = t % 3
                        hb_, hn = hh[q3], "hh%d" % q3
                        P.op("dve", lambda e: e.tensor_tensor(hb_[:], hb_[:], g1[:], op=ALU.mult), reads=[hn, "g1"], writes=[hn])
                        P.op("dve", lambda e: e.tensor_tensor(hb_[:], hb_[:], b1[:], op=ALU.add), reads=[hn, "b1"], writes=[hn])

                    def L1b(t):
                        gt = s * NT + t
                        r = t % 2
                        r3 = t % 3
                        hb_, hn = hh[r3], "hh%d" % r3
                        P.dma("sp", lambda e: e.dma_start(out=h_scr[gt * 128:(gt + 1) * 128, :], in_=hb_[:]), reads=[hn], writes=[("h_scr", gt)])
                        if "h" in dbg:
                            P.dma("sp", lambda e: e.dma_start(out=dbg["h"][gt * 128:(gt + 1) * 128, :], in_=hb_[:]), reads=[hn], writes=[("dbg_h", gt)])
                        P.op("act", lambda e: e.activation(out=hbf[t % 4][:], in_=hb_[:], func=AF.Copy), reads=[hn], writes=["hbf%d" % (t % 4)])
                        for hf in range(2):
                            bk = 2 + hf
                            for j in range(4):
                                kc = hf * 4 + j
                                P.op("pe", lambda e: e.transpose(B[bk][:, j * 128:(j + 1) * 128], hb_[:, kc * 128:(kc + 1) * 128], ident_f),
                                     reads=[hn, "cm_f"], writes=[BN[bk]])
                            evac(hTs[r][:, hf * 4:(hf + 1) * 4, :], B[bk][:].rearrange("p (a b) -> p a b", a=4), reads=[BN[bk]], writes=[("hT", r, hf)], eng="act")

                    def L2a1(t):
                        gt = s * NT + t
                        r = t % 2
                        hT = hTs[r]
                        rr = rrs[r]
                        elm = elms[r]
                        elm_f = elm[:].rearrange("p a b -> p (a b)")
                        oh1 = oh1s[t % 3]
                        o1n = "oh1_%d" % (t % 3)
                        for kc in range(8):
                            P.op("pe", lambda e: e.matmul(B[4][:, 0:36], lhsT=hT[:, kc, :], rhs=wrt[:, kc, :], start=(kc == 0), stop=(kc == 7)),
                                 reads=[("hT", r, kc // 4), "wrt"], writes=[BN[4]])
                        P.op("dve", lambda e: e.tensor_tensor(lg[:], B[4][:, 0:36], brt[:], op=ALU.add), reads=[BN[4], "brt"], writes=["lg"])
                        P.op("dve", lambda e: e.reduce_max(rr[:, 0:1], lg[:, 0:4], axis=AX.X), reads=["lg"], writes=[("rr0", r)])
                        P.op("dve", lambda e: e.tensor_scalar(rr[:, 1:2], rr[:, 0:1], -1.0, None, op0=ALU.mult), reads=[("rr0", r)], writes=[("rr1", r)])
                        P.op("act", lambda e: e.activation(out=ge[:], in_=lg[:, 0:4], func=AF.Exp, bias=rr[:, 1:2]), reads=["lg", ("rr1", r)], writes=["ge"])
                        P.op("dve", lambda e: e.reduce_sum(rr[:, 2:3], ge[:], axis=AX.X), reads=["ge"], writes=[("rr2", r)])
                        P.op("dve", lambda e: e.reciprocal(rr[:, 3:4], rr[:, 2:3]), reads=[("rr2", r)], writes=[("rr3", r)])
                        P.op("dve", lambda e: e.tensor_scalar(gone[:], lg[:, 0:4], rr[:, 0:1], None, op0=ALU.is_equal), reads=["lg", ("rr0", r)], writes=["gone"])
                        P.op("dve", lambda e: e.tensor_scalar(mneg[:, :, 0], gone[:], -1.0, 1e30, op0=ALU.add, op1=ALU.mult), reads=["gone"], writes=["mneg"])
                        P.op("dve", lambda e: e.tensor_tensor(elm[:], lg[:, 4:36].rearrange("p (a b) -> p a b", a=4), mneg[:].to_broadcast([128, 4, 8]), op=ALU.add),
                             reads=["lg", "mneg"], writes=[("elm", r)])
                        P.op("dve", lambda e: e.reduce_max(rr[:, 4:5], elm_f, axis=AX.X), reads=[("elm", r)], writes=[("rr4", r)])
                        P.op("dve", lambda e: e.tensor_scalar(oh1[:], elm_f, rr[:, 4:5], None, op0=ALU.is_equal), reads=[("elm", r), ("rr4", r)], writes=[o1n])

                    def L2a2(t):
                        gt = s * NT + t
                        r = t % 2
                        rr = rrs[r]
                        elm = elms[r]
                        elm_f = elm[:].rearrange("p a b -> p (a b)")
                        oh1, oh2, Mb = oh1s[t % 3], oh2s[r], Mbs[r]
                        o1n, o2n, mbn = "oh1_%d" % (t % 3), "oh2_%d" % r, "Mb%d" % r
                        P.op("dve", lambda e: e.scalar_tensor_tensor(elm2[:], oh1[:], -1e30, elm_f, op0=ALU.mult, op1=ALU.add), reads=[o1n, ("elm", r)], writes=["elm2"])
                        P.op("dve", lambda e: e.reduce_max(rr[:, 5:6], elm2[:], axis=AX.X), reads=["elm2"], writes=[("rr5", r)])
                        P.op("dve", lambda e: e.tensor_scalar(oh2[:], elm2[:], rr[:, 5:6], None, op0=ALU.is_equal), reads=["elm2", ("rr5", r)], writes=[o2n])
                        P.op("dve", lambda e: e.tensor_tensor(rr[:, 6:7], rr[:, 5:6], rr[:, 4:5], op=ALU.subtract), reads=[("rr4", r), ("rr5", r)], writes=[("rr6", r)])
                        P.op("act", lambda e: e.activation(out=rr[:, 7:8], in_=rr[:, 6:7], func=AF.Exp), reads=[("rr6", r)], writes=[("rr7", r)])
                        P.op("dve", lambda e: e.tensor_scalar(rr[:, 8:9], rr[:, 7:8], 1.0, None, op0=ALU.add), reads=[("rr7", r)], writes=[("rr8", r)])
                        P.op("dve", lambda e: e.reciprocal(rr[:, 9:10], rr[:, 8:9]), reads=[("rr8", r)], writes=[("rr9", r)])
                        P.op("dve", lambda e: e.tensor_tensor(wts[:, gt, 0:1], rr[:, 9:10], rr[:, 3:4], op=ALU.mult), reads=[("rr9", r), ("rr3", r)], writes=[("wts", gt, 0)])
                        P.op("dve", lambda e: e.tensor_tensor(rr[:, 10:11], rr[:, 7:8], rr[:, 9:10], op=ALU.mult), reads=[("rr7", r), ("rr9", r)], writes=[("rr10", r)])
                        P.op("dve", lambda e: e.tensor_tensor(wts[:, gt, 1:2], rr[:, 10:11], rr[:, 3:4], op=ALU.mult), reads=[("rr10", r), ("rr3", r)], writes=[("wts", gt, 1)])
                        P.op("dve", lambda e: e.tensor_tensor(Mb[:], oh1[:], oh2[:], op=ALU.add), reads=[o1n, o2n], writes=[mbn])

                    def L2b(t):
                        gt = s * NT + t
                        r = t % 2
                        r3 = t % 3
                        oh1, oh2, Mb = oh1s[t % 3], oh2s[r], Mbs[r]
                        o1n, o2n, mbn = "oh1_%d" % (t % 3), "oh2_%d" % r, "Mb%d" % r
                        r4 = t % 4
                        P.op("pe", lambda e: e.matmul(B[5][:, 0:32], lhsT=tri_b, rhs=Mb[:], start=True, stop=True, skip_group_check=True),
                             reads=[mbn, "cm_b"], writes=[BN[5]])
                        P.op("pe", lambda e: e.matmul(B[5][:, 32:64], lhsT=ones_b, rhs=Mb[:], start=False, stop=True, skip_group_check=True),
                             reads=[mbn, "cm_b"], writes=[BN[5]])
                        P.op("dve", lambda e: e.tensor_tensor(posin[:], B[5][:, 0:32], cnt[:], op=ALU.add), reads=[BN[5], "cnt"], writes=["posin"])
                        P.op("dve", lambda e: e.tensor_scalar(valid[:], posin[:], CAP - 0.5, None, op0=ALU.is_lt), reads=["posin"], writes=["valid"])
                        P.op("dve", lambda e: e.tensor_tensor(smv[:], posin[:], eoffmd, op=ALU.add), reads=["posin", "cv"], writes=["smv"])
                        P.op("dve", lambda e: e.tensor_tensor(smv[:], smv[:], valid[:], op=ALU.mult), reads=["smv", "valid"], writes=["smv"])
                        for k_, ohk, ohn in ((0, oh1, o1n), (1, oh2, o2n)):
                            P.op("dve", lambda e: e.tensor_tensor(tmp32[:], smv[:], ohk[:], op=ALU.mult), reads=["smv", ohn], writes=["tmp32"])
                            P.op("dve", lambda e: e.reduce_sum(slots_f[:, gt, k_:k_ + 1], tmp32[:], axis=AX.X), reads=["tmp32"], writes=[("slots_f", gt, k_)])
                        P.op("dve", lambda e: e.tensor_tensor(cnt[:], cnt[:], B[5][:, 32:64], op=ALU.add), reads=["cnt", BN[5]], writes=["cnt"])
                        P.op("dve", lambda e: e.tensor_scalar(slots_f[:, gt, :], slots_f[:, gt, :], float(DUMP), None, op0=ALU.add),
                             reads=[("slots_f", gt, 0), ("slots_f", gt, 1)], writes=[("slots_f2", gt)])
                        P.op("dve", lambda e: e.tensor_copy(slots_i[:, gt, :], slots_f[:, gt, :]), reads=[("slots_f2", gt)], writes=[("slots_i", gt)])
                        for k_ in range(2):
                            P.dma("pool", lambda e: e.indirect_dma_start(
                                out=xs[:, :], out_offset=bass.IndirectOffsetOnAxis(ap=slots_i[:, gt, k_:k_ + 1], axis=0),
                                in_=hbf[r4][:], in_offset=None), reads=["hbf%d" % r4, ("slots_i", gt)] + xs_init, writes=[("xs", gt, k_)])

                    pipeline(NT, [L0a, L0b, L1a, L1a2, L1b, L2a1, L2a2, L2b])
                    P.barrier()
        NTOT = NSEQ * NT
        if stop in ("ln1",):
            P.dma("sp", lambda e: e.dma_start(out=dbg["slots"][:, :, :], in_=slots_i[:]), writes=["dbg1"])
            P.dma("sp", lambda e: e.dma_start(out=dbg["wts"][:, :, :], in_=wts[:]), writes=["dbg2"])
            P.barrier()
            return nc
        if stop is not None and stop not in ("moe",):
            P.barrier()
            return nc

        xs_all = [("xs", gt, k_) for gt in range(NTOT) for k_ in range(2)]

        with contextlib.ExitStack() as s4:
            NWB = 3
            wg = [sb(s4, "wg%d" % i, [128, 8, D], BF16) for i in range(NWB)]
            wd = [sb(s4, "wd%d" % i, [128, 4, D], BF16) for i in range(NWB)]
            xsb3 = [sb(s4, "xsb3_%d" % i, [128, NBLK, D], BF16) for i in range(2)]
            xsT3 = [sb(s4, "xsT3_%d" % i, [128, 8, CAP], BF16) for i in range(2)]
            sg = [sb(s4, "sg%d" % i, [128, CAP], F32) for i in range(2)]
            actT3 = [sb(s4, "actT3_%d" % i, [128, 4, CAP], BF16) for i in range(2)]
            yb = [sb(s4, "yb%d" % i, [128, D], F32) for i in range(2)]

            def load_expert(e_):
                b = e_ % NWB
                for kc2 in range(2):
                    P.dma("sp", lambda e: e.dma_start(out=wg[b][:, kc2 * 4:(kc2 + 1) * 4, :],
                                                      in_=wgu_bf[e_].rearrange("(kc k) n -> k kc n", k=128)[:, kc2 * 4:(kc2 + 1) * 4, :]),
                          reads=[("wcv", e_, kc2)], writes=[("wg%d" % b, kc2)])
                P.dma("sp", lambda e: e.dma_start(out=wd[b][:], in_=wdn_bf[e_].rearrange("(kc k) n -> k kc n", k=128)),
                      reads=[("wcv", e_, 2)], writes=["wd%d" % b])

            for e0 in range(min(NWB, NE)):
                load_expert(e0)
            def E0(e_):
                r = e_ % 2
                for blk in range(NBLK):
                    row0 = e_ * CAP + blk * 128
                    P.dma("sp", lambda e: e.dma_start(out=xsb3[r][:, blk, :], in_=xs[row0:row0 + 128, :]), reads=xs_all, writes=[("xsb3", r, blk)])

            def E1(e_):
                r = e_ % 2
                for blk in range(NBLK):
                    bt = (e_ * NBLK + blk) % 2
                    for kc in range(8):
                        P.op("pe", lambda e: e.transpose(bank_bf(bt)[:, kc * 128:(kc + 1) * 128], xsb3[r][:, blk, kc * 128:(kc + 1) * 128], ident_b),
                             reads=[("xsb3", r, blk), "cm_b"], writes=[BN[bt]])
                    evac(xsT3[r][:, :, blk * 128:(blk + 1) * 128], bank_bf(bt)[:, :].rearrange("p (a b) -> p a b", a=8),
                         reads=[BN[bt]], writes=[("xsT3", r, blk)])

            def E2(e_):
                r = e_ % 2
                eb = e_ % NWB
                xr = [("xsT3", r, blk) for blk in range(NBLK)]
                for fc in range(4):
                    bg, bu = ((2, 3), (4, 5))[fc % 2]
                    for col0, bk in ((fc * 128, bg), (512 + fc * 128, bu)):
                        for kc in range(8):
                            P.op("pe", lambda e: e.matmul(B[bk][:, 0:CAP], lhsT=wg[eb][:, kc, col0:col0 + 128], rhs=xsT3[r][:, kc, :],
                                                          start=(kc == 0), stop=(kc == 7)),
                                 reads=xr + [("wg%d" % eb, kc // 4)], writes=[BN[bk]])
                    q_ = fc % 2
                    P.op("act", lambda e: e.activation(out=sg[q_][:], in_=B[bg][:, 0:CAP], func=AF.Silu), reads=[BN[bg]], writes=["sg%d" % q_])
                    P.op("dve", lambda e: e.tensor_tensor(actT3[r][:, fc, :], B[bu][:, 0:CAP], sg[q_][:], op=ALU.mult),
                         reads=[BN[bu], "sg%d" % q_], writes=[("actT3", r, fc)])

            def E3(e_):
                r = e_ % 2
                eb = e_ % NWB
                ar = [("actT3", r, fc) for fc in range(4)]
                for blk in range(NBLK):
                    q_ = (e_ * NBLK + blk) % 2
                    row0 = e_ * CAP + blk * 128
                    for half, bk in ((0, 6), (1, 7)):
                        for fc in range(4):
                            P.op("pe", lambda e: e.matmul(B[bk][:], lhsT=actT3[r][:, fc, blk * 128:(blk + 1) * 128], rhs=wd[eb][:, fc, half * 512:(half + 1) * 512],
                                                          start=(fc == 0), stop=(fc == 3)),
                                 reads=ar + ["wd%d" % eb], writes=[BN[bk]])
                        evac(yb[q_][:, half * 512:(half + 1) * 512], B[bk][:], reads=[BN[bk]], writes=[("yb%d" % q_, half)])
                    P.dma("pool", lambda e: e.dma_start(out=ys[row0:row0 + 128, :], in_=yb[q_][:]),
                          reads=[("yb%d" % q_, 0), ("yb%d" % q_, 1)], writes=[("ys", e_, blk)])
                if e_ + NWB < NE:
                    load_expert(e_ + NWB)

            pipeline(NE, [E0, E1, E2, E3])
            P.barrier()
        ys_all = [("ys", e_, blk) for e_ in range(NE) for blk in range(NBLK)] + ["ys_dump"]
        if stop == "moe":
            P.dma("sp", lambda e: e.dma_start(out=dbg["ys"][:, :], in_=ys[:, :]), writes=["dbg1"])
            P.dma("sp", lambda e: e.dma_start(out=dbg["slots"][:, :, :], in_=slots_i[:]), writes=["dbg2"])
            P.dma("sp", lambda e: e.dma_start(out=dbg["wts"][:, :, :], in_=wts[:]), writes=["dbg3"])
            P.barrier()
            return nc

        with contextlib.ExitStack() as s5:
            g2 = sb(s5, "g2", [128, D], F32)
            b2 = sb(s5, "b2", [128, D], F32)
            y1 = [sb(s5, "y1_%d" % i, [128, D], F32) for i in range(3)]
            y2 = [sb(s5, "y2_%d" % i, [128, D], F32) for i in range(3)]
            ht = [sb(s5, "ht%d" % i, [128, D], F32) for i in range(3)]
            za = [sb(s5, "za%d" % i, [128, D], F32) for i in range(2)]
            zc = [sb(s5, "zc%d" % i, [128, D], F32) for i in range(2)]
            oo = [sb(s5, "oo%d" % i, [128, D], F32) for i in range(2)]
            stats2 = [sb(s5, "stats2_%d" % i, [128, 2, 6], F32) for i in range(2)]
            mv2 = [sb(s5, "mv2_%d" % i, [128, 4], F32) for i in range(2)]
            nm2 = sb(s5, "nm2", [128, 2], F32)
            P.dma("sp", lambda e: e.dma_start(out=g2[:], in_=ln2_g[0:1, :].to_broadcast([128, D])), writes=["g2"])
            P.dma("sp", lambda e: e.dma_start(out=b2[:], in_=ln2_b[0:1, :].to_broadcast([128, D])), writes=["b2"])

            def c0(gt):
                r = gt % 3
                P.dma("pool", lambda e: e.indirect_dma_start(
                    out=y1[r][:], out_offset=None, in_=ys[:, :],
                    in_offset=bass.IndirectOffsetOnAxis(ap=slots_i[:, gt, 0:1], axis=0)), reads=ys_all, writes=["y1_%d" % r])
                P.dma("pool", lambda e: e.indirect_dma_start(
                    out=y2[r][:], out_offset=None, in_=ys[:, :],
                    in_offset=bass.IndirectOffsetOnAxis(ap=slots_i[:, gt, 1:2], axis=0)), reads=ys_all, writes=["y2_%d" % r])
                P.dma("sp", lambda e: e.dma_start(out=ht[r][:], in_=h_scr[gt * 128:(gt + 1) * 128, :]), reads=[("h_scr", gt)], writes=["ht%d" % r])

            def c1(gt):
                r = gt % 3
                q_ = gt % 2
                P.op("act", lambda e: e.activation(out=za[q_][:], in_=ht[r][:], func=AF.Copy, scale=ALPHA), reads=["ht%d" % r], writes=["za%d" % q_])
                P.op("dve", lambda e: e.scalar_tensor_tensor(za[q_][:], y1[r][:], wts[:, gt, 0:1], za[q_][:], op0=ALU.mult, op1=ALU.add),
                     reads=["y1_%d" % r, "za%d" % q_], writes=["za%d" % q_])
                P.op("dve", lambda e: e.scalar_tensor_tensor(zc[q_][:], y2[r][:], wts[:, gt, 1:2], za[q_][:], op0=ALU.mult, op1=ALU.add),
                     reads=["y2_%d" % r, "za%d" % q_], writes=["zc%d" % q_])
                for hf in range(2):
                    P.op("dve", lambda e: e.bn_stats(stats2[q_][:, hf, :], zc[q_][:, hf * 512:(hf + 1) * 512]), reads=["zc%d" % q_], writes=[("stats2", q_, hf)])
                P.op("dve", lambda e: e.bn_aggr(mv2[q_][:, 0:2], stats2[q_][:].rearrange("p a b -> p (a b)")),
                     reads=[("stats2", q_, 0), ("stats2", q_, 1)], writes=[("mv2a", q_)])
                P.op("dve", lambda e: e.tensor_scalar(mv2[q_][:, 2:3], mv2[q_][:, 1:2], LN_EPS, None, op0=ALU.add), reads=[("mv2a", q_)], writes=[("mv2b", q_)])

            def c2(gt):
                q_ = gt % 2
                s_, t_ = gt // NT, gt % NT
                P.op("pool", lambda e: e.tensor_tensor(mv2[q_][:, 3:4], mv2[q_][:, 2:3], negh[:, 0:1], op=ALU.pow), reads=[("mv2b", q_), "negh"], writes=[("mv2c", q_)])
                P.op("dve", lambda e: e.scalar_tensor_tensor(nm2[:, q_:q_ + 1], mv2[q_][:, 0:1], -1.0, mv2[q_][:, 3:4], op0=ALU.mult, op1=ALU.mult),
                     reads=[("mv2a", q_), ("mv2c", q_)], writes=[("nm2", q_)])
                P.op("act", lambda e: e.activation(out=oo[q_][:], in_=zc[q_][:], func=AF.Identity, scale=mv2[q_][:, 3:4], bias=nm2[:, q_:q_ + 1]),
                     reads=["zc%d" % q_, ("mv2c", q_), ("nm2", q_)], writes=["oo%d" % q_])
                P.op("dve", lambda e: e.tensor_tensor(oo[q_][:], oo[q_][:], g2[:], op=ALU.mult), reads=["oo%d" % q_, "g2"], writes=["oo%d" % q_])
                P.op("dve", lambda e: e.tensor_tensor(oo[q_][:], oo[q_][:], b2[:], op=ALU.add), reads=["oo%d" % q_, "b2"], writes=["oo%d" % q_])
                P.dma("sp", lambda e: e.dma_start(out=out[s_, t_ * 128:(t_ + 1) * 128, :], in_=oo[q_][:]), reads=["oo%d" % q_], writes=[("out", gt)])

            pipeline(NTOT, [c0, c1, c2])
            P.barrier()
    return nc


def _in_maps(inputs):
    f = lambda a: np.ascontiguousarray(np.asarray(a))
    cmat, cvec = _consts()
    common = {
        "w_in": f(inputs["w_in"][0]),
        "gate_bias": f(inputs["gate_bias"][0].reshape(16, 128).T),
        "lam_vecs": f(inputs["lam_vecs"][0].reshape(1, 256)),
        "subln_gain": f(inputs["subln_gain"][0].reshape(1, 128)),
        "w_pool": f(inputs["w_pool"][0]),
        "pool_scale": f(inputs["pool_scale"][0].reshape(4, 128).T),
        "w_a": f(inputs["w_branch_a"][0]),
        "w_b": f(inputs["w_branch_b"][0]),
        "w_out": f(inputs["w_out"][0]),
        "ln1_g": f(inputs["ln1_gain"][0].reshape(1, D)),
        "ln1_b": f(inputs["ln1_bias"][0].reshape(1, D)),
        "w_rt": f(np.concatenate([inputs["w_router_group"][0], inputs["w_router_expert"][0]], axis=1)),
        "b_rt": f(np.concatenate([inputs["b_router_group"][0], inputs["b_router_expert"][0]]).reshape(1, 36)),
        "w_gu": f(inputs["w_gate_up"][0]),
        "w_dn": f(inputs["w_down"][0]),
        "ln2_g": f(inputs["ln2_gain"][0].reshape(1, D)),
        "ln2_b": f(inputs["ln2_bias"][0].reshape(1, D)),
        "cmat": cmat,
        "cvec": cvec,
    }
    maps = []
    xx = np.asarray(inputs["x"])
    pp = np.asarray(inputs["positions"]).astype(np.int32)
    for c in range(NCORES):
        m = dict(common)
        m["x"] = f(xx[c * NSEQ:(c + 1) * NSEQ])
        m["pos"] = f(pp[c * NSEQ:(c + 1) * NSEQ])
        maps.append(m)
    return maps


def kernel(**inputs):
    nc = build_nc()
    res = run_bass_kernel_spmd(nc, _in_maps(inputs), core_ids=list(range(NCORES)))
    return np.concatenate([np.asarray(r["out"]) for r in res.results], axis=0).astype(np.float32)
```

```python
import contextlib
import math
import numpy as np
import concourse.bass as bass
import concourse.mybir as mybir
from concourse.bass_utils import run_bass_kernel_spmd

F32 = mybir.dt.float32
BF16 = mybir.dt.bfloat16
I32 = mybir.dt.int32
AF = mybir.ActivationFunctionType
ALU = mybir.AluOpType
AX = mybir.AxisListType

NCORES = 8
NSEQ = 2
S = 2048
D = 1024
H = 8
NT = S // 128
NE = 32
CAP = 384
NBLK = CAP // 128
NSLOT = NE * CAP + 128
DUMP = NE * CAP
ALPHA = 2.0 ** 0.25
LAMBDA_INIT = 0.2
LN_EPS = 1e-5
RMS_EPS = 1e-5
IN_W = 5632
TWO_PI = 2.0 * math.pi
C1 = 6.28125
C2 = TWO_PI - C1

SEM_LIMIT = 30000
DMA_RING = 6
DMA_RING_Q = {"poolc": 2}
CONV_EVERY = 13


class Prog:
    def __init__(self, nc, stack):
        self.nc = nc
        self.stack = stack
        self.eng = {"pe": nc.tensor, "dve": nc.vector, "act": nc.scalar,
                    "pool": nc.gpsimd, "sp": nc.sync, "poolc": nc.gpsimd}
        self.cnt = {}
        self.sems = {}
        self.seen = {}
        self.res = {}
        self.dma_n = {}
        self.dma_ring = {}
        self.dma_ring_cnt = {}
        self.ninst = 0
        self.recording = None
        for s in ["pe", "dve", "act", "pool"]:
            self.cnt[s] = 0
            self.sems[s] = []
            self._new_sem(s)

    def _alloc_sem(self, name):
        return self.stack.enter_context(self.nc.semaphore(name))

    def _new_sem(self, s):
        sem = self._alloc_sem("s_%s_%d" % (s, len(self.sems[s])))
        self.sems[s].append((sem, self.cnt[s]))

    def _wait(self, engname, tok):
        stream, idx = tok
        key = (engname, stream)
        if self.seen.get(key, 0) >= idx:
            return
        if stream.startswith("dma"):
            self.seen[key] = idx
            q, r = stream.split(":")[1:]
            self.eng[engname].wait_ge(self.dma_ring[q][int(r)], 16 * idx)
            self.ninst += 1
            return
        if stream == engname and engname == "pe":
            return
        self.seen[key] = idx
        for sem, base in reversed(self.sems[stream]):
            if idx > base:
                self.eng[engname].wait_ge(sem, idx - base)
                self.ninst += 1
                return
        raise RuntimeError("bad token")

    def _deps(self, reads, writes):
        deps = []
        for r in reads:
            st = self.res.get(r)
            if st and st[0] is not None:
                deps.append(st[0])
        for w in writes:
            st = self.res.get(w)
            if st:
                if st[0] is not None:
                    deps.append(st[0])
                deps.extend(st[1])
        return deps

    def _commit(self, tok, reads, writes):
        for r in reads:
            st = self.res.setdefault(r, [None, []])
            st[1].append(tok)
        for w in writes:
            self.res[w] = [tok, []]

    def _excl(self, reads, writes):
        r2 = [r for r in reads if not (isinstance(r, str) and r.startswith("B") and r[1:].isdigit())]
        w2 = list(writes) + [r for r in reads if (isinstance(r, str) and r.startswith("B") and r[1:].isdigit())]
        return r2, w2

    def op(self, engname, fn, reads=(), writes=()):
        if self.recording is not None:
            rec = _Rec()
            fn(rec)
            self.recording.append(("op", engname, rec.call, tuple(reads), tuple(writes)))
            return None
        reads, writes = self._excl(reads, writes)
        for d in self._deps(reads, writes):
            self._wait(engname, d)
        inst = fn(self.eng[engname])
        if self.cnt[engname] - self.sems[engname][-1][1] >= SEM_LIMIT:
            self._new_sem(engname)
        sem, base = self.sems[engname][-1]
        inst.then_inc(sem, 1)
        self.cnt[engname] += 1
        self.ninst += 1
        tok = (engname, self.cnt[engname])
        self._commit(tok, reads, writes)
        return tok

    def dma(self, q, fn, reads=(), writes=()):
        if self.recording is not None:
            rec = _Rec()
            fn(rec)
            self.recording.append(("dma", q, rec.call, tuple(reads), tuple(writes)))
            return None
        nring = DMA_RING_Q.get(q, DMA_RING)
        if q not in self.dma_ring:
            self.dma_ring[q] = [self._alloc_sem("d_%s_%d" % (q, i)) for i in range(nring)]
            self.dma_ring_cnt[q] = [0] * nring
            self.dma_n[q] = 0
        r = self.dma_n[q] % nring
        self.dma_n[q] += 1
        stream = "dma:%s:%d" % (q, r)
        if self.dma_ring_cnt[q][r] > 0:
            self._wait(q, (stream, self.dma_ring_cnt[q][r]))
        for d in self._deps(reads, writes):
            self._wait(q, d)
        inst = fn(self.eng[q])
        inst.then_inc(self.dma_ring[q][r], 16)
        self.dma_ring_cnt[q][r] += 1
        self.ninst += 1
        tok = (stream, self.dma_ring_cnt[q][r])
        self._commit(tok, reads, writes)
        return tok

    def record(self, fn, *args):
        assert self.recording is None
        self.recording = []
        try:
            fn(*args)
            return self.recording
        finally:
            self.recording = None

    def replay(self, item):
        kind, eng, call, reads, writes = item
        name, a, kw = call
        f = lambda e: getattr(e, name)(*a, **kw)
        if kind == "op":
            return self.op(eng, f, reads, writes)
        return self.dma(eng, f, reads, writes)

    def barrier(self):
        toks = []
        for s in ["pe", "dve", "act", "pool"]:
            if self.cnt[s] > 0:
                toks.append((s, self.cnt[s]))
        for q in self.dma_ring:
            for r in range(len(self.dma_ring[q])):
                if self.dma_ring_cnt[q][r] > 0:
                    toks.append(("dma:%s:%d" % (q, r), self.dma_ring_cnt[q][r]))
        for e in ["pe", "dve", "act", "pool", "sp"]:
            for t in toks:
                if e == "pe" and t[0] == "pe":
                    continue
                self._wait(e, t)
        self.res = {}


class _Rec:
    def __init__(self):
        self.call = None

    def __getattr__(self, name):
        def f(*a, **kw):
            self.call = (name, a, kw)
            return self
        return f


def _consts():
    ident = np.eye(128, dtype=np.float32)
    prot = np.zeros((128, 128), np.float32)
    for pp in range(128):
        if (pp % 64) < 32:
            prot[pp + 32, pp] = -1.0
        else:
            prot[pp - 32, pp] = 1.0
    tri = np.triu(np.ones((128, 128), np.float32), 1)
    ones = np.ones((128, 128), np.float32)
    cmat = np.stack([ident, prot, tri, ones], axis=1)
    half = 32
    inv_freq = (10000.0 ** (-np.arange(half, dtype=np.float32) * np.float32(2.0 / 64))).astype(np.float32)
    cvec = np.zeros((128, 128), np.float32)
    cvec[:, 0] = inv_freq[np.arange(128) % 32]
    for g, w in enumerate((2, 4, 8, 16)):
        for t in range(16):
            cvec[:, 16 + g * 16 + t] = (w / (t + 1.0)) if t < w - 1 else 1.0
    cvec[:, 80:112] = (np.arange(NE, dtype=np.float32) * CAP - DUMP)[None, :]
    cvec[64:, 112] = -30000.0
    return np.ascontiguousarray(cmat), cvec


def build_nc(stop=None):
    nc = bass.Bass("TRN2", target_bir_lowering=False)

    def dram(name, shape, dt, kind="ExternalInput"):
        return nc.dram_tensor(name, shape, dt, kind=kind).ap()

    x = dram("x", [NSEQ, S, D], F32)
    pos = dram("pos", [NSEQ, S], I32)
    w_in = dram("w_in", [D, IN_W], F32)
    gate_bias = dram("gate_bias", [128, 16], F32)
    lam_vecs = dram("lam_vecs", [1, 256], F32)
    subln_gain = dram("subln_gain", [1, 128], F32)
    w_pool = dram("w_pool", [4, 128, 128], F32)
    pool_scale = dram("pool_scale", [128, 4], F32)
    w_a = dram("w_a", [D, D], F32)
    w_b = dram("w_b", [512, D], F32)
    w_out = dram("w_out", [D, D], F32)
    ln1_g = dram("ln1_g", [1, D], F32)
    ln1_b = dram("ln1_b", [1, D], F32)
    w_rt = dram("w_rt", [D, 36], F32)
    b_rt = dram("b_rt", [1, 36], F32)
    w_gu = dram("w_gu", [NE, D, D], F32)
    w_dn = dram("w_dn", [NE, 512, D], F32)
    ln2_g = dram("ln2_g", [1, D], F32)
    ln2_b = dram("ln2_b", [1, D], F32)
    cmat = dram("cmat", [128, 4, 128], F32)
    cvec = dram("cvec", [128, 128], F32)
    out = dram("out", [NSEQ, S, D], F32, kind="ExternalOutput")
    h_scr = dram("h_scr", [NSEQ * S, D], F32, kind="Internal")
    xs = dram("xs", [NSLOT, D], BF16, kind="Internal")
    ys = dram("ys", [NSLOT, D], F32, kind="Internal")
    wgu_bf = dram("wgu_bf", [NE, D, D], BF16, kind="Internal")
    wdn_bf = dram("wdn_bf", [NE, 512, D], BF16, kind="Internal")
    dbg = {}
    if stop in ("attn", "a1", "rope", "v", "qk"):
        dbg["oT"] = dram("dbg_oT", [NSEQ, 128, H, S], BF16, kind="ExternalOutput")
    if stop == "mix":
        dbg["mergedT"] = dram("dbg_mergedT", [NSEQ, 128, 8, S], BF16, kind="ExternalOutput")
    if stop in ("ln1",):
        dbg["h"] = dram("dbg_h", [NSEQ * S, D], F32, kind="ExternalOutput")
    if stop in ("ln1", "moe"):
        dbg["slots"] = dram("dbg_slots", [128, 32, 2], I32, kind="ExternalOutput")
        dbg["wts"] = dram("dbg_wts", [128, 32, 2], F32, kind="ExternalOutput")
    if stop == "moe":
        dbg["ys"] = dram("dbg_ys", [NSLOT, D], F32, kind="ExternalOutput")

    w_in_k = w_in.rearrange("(kc k) n -> k kc n", k=128)
    w_a_k = w_a.rearrange("(kc k) n -> k kc n", k=128)
    w_b_k = w_b.rearrange("(kc k) n -> k kc n", k=128)
    w_out_k = w_out.rearrange("(kc k) n -> k kc n", k=128)
    w_rt_k = w_rt.rearrange("(kc k) n -> k kc n", k=128)

    with contextlib.ExitStack() as st:
        P = Prog(nc, st)

        uniq = [0]

        def sb(stack, name, shape, dt):
            uniq[0] += 1
            return stack.enter_context(nc.sbuf_tensor("%s_u%d" % (name, uniq[0]), shape, dt))

        B = [st.enter_context(nc.psum_tensor("bank%d" % i, [128, 512], F32)) for i in range(8)]
        BN = ["B%d" % i for i in range(8)]

        def bank_bf(i):
            return B[i][:].bitcast(BF16)

        cm_f = sb(st, "cm_f", [128, 4, 128], F32)
        cm_b = sb(st, "cm_b", [128, 4, 128], BF16)
        cv = sb(st, "cv", [128, 128], F32)
        lvb = sb(st, "lvb", [128, 256], F32)
        gainb = sb(st, "gainb", [128, 128], F32)
        gbias = sb(st, "gbias", [128, 16], F32)
        pscale = sb(st, "pscale", [128, 4], F32)
        wpool_b = sb(st, "wpool_b", [128, 4, 128], BF16)
        wrt = sb(st, "wrt", [128, 8, 36], F32)
        brt = sb(st, "brt", [128, 36], F32)
        small = sb(st, "small", [128, 16], F32)
        negh = sb(st, "negh", [128, 4], F32)
        cnt = sb(st, "cnt", [128, 32], F32)
        slots_f = sb(st, "slots_f", [128, 32, 2], F32)
        slots_i = sb(st, "slots_i", [128, 32, 2], I32)
        wts = sb(st, "wts", [128, 32, 2], F32)
        zrow = sb(st, "zrow", [128, 1024], F32)
        zb3 = sb(st, "zb3", [128, 1, 1024], BF16)
        xT = sb(st, "xT", [128, 8, S], BF16)
        oT = sb(st, "oT", [128, 8, S], BF16)

        ident_f = cm_f[:, 0, :]
        ident_b = cm_b[:, 0, :]
        prot_b = cm_b[:, 1, :]
        tri_b = cm_b[:, 2, :]
        ones_b = cm_b[:, 3, :]
        invf = cv[:, 0:1]
        eoffmd = cv[:, 80:112]
        lamneg = small[:, 0:1]

        P.dma("sp", lambda e: e.dma_start(out=cm_f[:], in_=cmat[:, :, :]), writes=["cm_f"])
        P.dma("pool", lambda e: e.dma_start(out=cm_b[:], in_=cmat[:, :, :]), writes=["cm_b"])
        P.dma("sp", lambda e: e.dma_start(out=cv[:], in_=cvec[:, :]), writes=["cv"])
        P.dma("sp", lambda e: e.dma_start(out=lvb[:], in_=lam_vecs[0:1, :].to_broadcast([128, 256])), writes=["lvb"])
        P.dma("sp", lambda e: e.dma_start(out=gainb[:], in_=subln_gain[0:1, :].to_broadcast([128, 128])), writes=["gainb"])
        P.dma("sp", lambda e: e.dma_start(out=brt[:], in_=b_rt[0:1, :].to_broadcast([128, 36])), writes=["brt"])
        P.dma("sp", lambda e: e.dma_start(out=gbias[:], in_=gate_bias[:, :]), writes=["gbias"])
        P.dma("sp", lambda e: e.dma_start(out=pscale[:], in_=pool_scale[:, :]), writes=["pscale"])
        P.dma("pool", lambda e: e.dma_start(out=wpool_b[:], in_=w_pool.rearrange("g c d -> c g d")), writes=["wpool_b"])
        P.dma("sp", lambda e: e.dma_start(out=wrt[:], in_=w_rt_k), writes=["wrt"])
        P.op("dve", lambda e: e.memset(cnt[:], 0.0), writes=["cnt"])
        P.op("dve", lambda e: e.memset(slots_i[:], 0), writes=["slots_i_init"])
        P.op("dve", lambda e: e.memset(wts[:], 0.0), writes=["wts_init"])
        P.op("dve", lambda e: e.memset(zrow[:], 0.0), writes=["zrow"])
        P.op("dve", lambda e: e.memset(negh[:], -0.5), writes=["negh"])
        P.op("dve", lambda e: e.tensor_scalar(gainb[:], gainb[:], 1.0 - LAMBDA_INIT, None, op0=ALU.mult),
             reads=["gainb"], writes=["gainb"])
        P.op("dve", lambda e: e.memset(zb3[:], 0.0), writes=["zb3"])
        xs_v = xs.rearrange("(n p) d -> p n d", p=128)
        n_tot = NSLOT // 128
        xs_init = [("xs_init", n0) for n0 in range(0, n_tot, 25)]

        def emit_xs_zero_fill():
            for n0 in range(0, n_tot, 25):
                n1 = min(n_tot, n0 + 25)
                P.dma("sp", lambda e: e.dma_start(out=xs_v[:, n0:n1, :], in_=zb3[:].to_broadcast([128, n1 - n0, 1024])),
                      reads=["zb3"], writes=[("xs_init", n0)])

        P.dma("sp", lambda e: e.dma_start(out=ys[DUMP:DUMP + 128, :], in_=zrow[:]), reads=["zrow"], writes=["ys_dump"])
        P.op("dve", lambda e: e.tensor_tensor(lvb[:, 0:64], lvb[:, 0:64], lvb[:, 64:128], op=ALU.mult),
             reads=["lvb"], writes=["lvb"])
        P.op("dve", lambda e: e.tensor_tensor(lvb[:, 128:192], lvb[:, 128:192], lvb[:, 192:256], op=ALU.mult),
             reads=["lvb"], writes=["lvb"])
        P.op("dve", lambda e: e.reduce_sum(small[:, 1:2], lvb[:, 0:64], axis=AX.X), reads=["lvb"], writes=["small"])
        P.op("dve", lambda e: e.reduce_sum(small[:, 2:3], lvb[:, 128:192], axis=AX.X), reads=["lvb", "small"], writes=["small"])
        P.op("act", lambda e: e.activation(out=small[:, 3:5], in_=small[:, 1:3], func=AF.Exp), reads=["small"], writes=["small"])
        P.op("dve", lambda e: e.tensor_tensor(small[:, 5:6], small[:, 4:5], small[:, 3:4], op=ALU.subtract),
             reads=["small"], writes=["small"])
        P.op("dve", lambda e: e.tensor_scalar(small[:, 0:1], small[:, 5:6], -LAMBDA_INIT, None, op0=ALU.add),
             reads=["small"], writes=["small"])

        ev_flip = [0]

        def evac(out_ap, in_ap, reads, writes, eng=None):
            if eng is None:
                eng = "act" if ev_flip[0] % 2 == 0 else "dve"
                ev_flip[0] += 1
            if eng == "act":
                return P.op("act", lambda e: e.activation(out=out_ap, in_=in_ap, func=AF.Copy), reads=reads, writes=writes)
            return P.op("dve", lambda e: e.tensor_copy(out_ap, in_ap), reads=reads, writes=writes)

        def pipeline(n_items, stages):
            for step in range(n_items + len(stages) - 1):
                lists = []
                for si, f in enumerate(stages):
                    k = step - si
                    if 0 <= k < n_items:
                        lists.append(P.record(f, k))
                pos = [0] * len(lists)
                left = sum(len(l) for l in lists)
                while left:
                    for li, l in enumerate(lists):
                        if pos[li] < len(l):
                            P.replay(l[pos[li]])
                            pos[li] += 1
                            left -= 1

        conv_jobs = []
        for e_ in range(NE):
            conv_jobs.append((wgu_bf[e_, 0:512, :], w_gu[e_, 0:512, :], ("wcv", e_, 0)))
            conv_jobs.append((wgu_bf[e_, 512:1024, :], w_gu[e_, 512:1024, :], ("wcv", e_, 1)))
            conv_jobs.append((wdn_bf[e_, :, :], w_dn[e_, :, :], ("wcv", e_, 2)))
        conv_next = [0]
        conv_tick = [0]

        def conv_issue(n=1):
            for _ in range(n):
                if conv_next[0] < len(conv_jobs):
                    o_, i_, nm_ = conv_jobs[conv_next[0]]
                    conv_next[0] += 1
                    P.dma("poolc", lambda e: e.dma_start(out=o_, in_=i_), writes=[nm_])

        for s in range(NSEQ):
            s2 = contextlib.ExitStack()
            with s2:
                V = sb(s2, "V", [128, NT, H, 129], BF16)
                cosT = sb(s2, "cosT", [128, S], F32)
                sinT = sb(s2, "sinT", [128, S], F32)
                with contextlib.ExitStack() as s2a:
                    xt = [sb(s2a, "xt%d" % i, [128, D], F32) for i in range(2)]
                    posi = sb(s2a, "posi", [128, S], I32)
                    ang = sb(s2a, "ang", [128, S], F32)
                    ras = [sb(s2a, "ra%d" % i, [128, S], F32) for i in range(2)]
                    rk = sb(s2a, "rk", [128, S], F32)
                    ki = sb(s2a, "ki", [128, S], I32)
                    wv = sb(s2a, "wv", [128, 8, D], BF16)
                    for kc2 in range(2):
                        P.dma("pool", lambda e: e.dma_start(out=wv[:, kc2 * 4:(kc2 + 1) * 4, :],
                                                            in_=w_in_k[:, kc2 * 4:(kc2 + 1) * 4, 2048:3072]),
                              writes=[("wv", kc2)])
                    P.dma("sp", lambda e: e.dma_start(out=posi[:], in_=pos[s:s + 1, :].to_broadcast([128, S])), writes=["posi"])
                    P.op("pool", lambda e: e.memset(V[:, :, :, 128:129], 1.0), writes=["Vones"])
                    P.op("dve", lambda e: e.tensor_copy(ang[:], posi[:]), reads=["posi"], writes=["ang"])
                    P.op("dve", lambda e: e.tensor_scalar(ang[:], ang[:], invf, None, op0=ALU.mult), reads=["ang", "cv"], writes=["ang"])
                    for which in range(2):
                        ra, ran = ras[which], "ra%d" % which
                        shift = 0.0 if which == 0 else 0.5 * math.pi
                        P.op("dve", lambda e: e.tensor_scalar(ra[:], ang[:], shift, None, op0=ALU.add), reads=["ang"], writes=[ran])
                        P.op("dve", lambda e: e.tensor_scalar(rk[:], ra[:], 1.0 / TWO_PI, None, op0=ALU.mult), reads=[ran], writes=["rk"])
                        P.op("dve", lambda e: e.tensor_copy(ki[:], rk[:]), reads=["rk"], writes=["ki"])
                        P.op("dve", lambda e: e.tensor_copy(rk[:], ki[:]), reads=["ki"], writes=["rk"])
                        P.op("dve", lambda e: e.scalar_tensor_tensor(ra[:], rk[:], -C1, ra[:], op0=ALU.mult, op1=ALU.add),
                             reads=["rk", ran], writes=[ran])
                        P.op("dve", lambda e: e.scalar_tensor_tensor(ra[:], rk[:], -C2, ra[:], op0=ALU.mult, op1=ALU.add),
                             reads=["rk", ran], writes=[ran])
                        P.op("dve", lambda e: e.tensor_scalar(rk[:], ra[:], math.pi, -TWO_PI, op0=ALU.is_gt, op1=ALU.mult),
                             reads=[ran], writes=["rk"])
                        P.op("dve", lambda e: e.tensor_tensor(ra[:], ra[:], rk[:], op=ALU.add), reads=[ran, "rk"], writes=[ran])
                        P.op("dve", lambda e: e.tensor_scalar(rk[:], ra[:], -math.pi, TWO_PI, op0=ALU.is_lt, op1=ALU.mult),
                             reads=[ran], writes=["rk"])
                        P.op("dve", lambda e: e.tensor_tensor(ra[:], ra[:], rk[:], op=ALU.add), reads=[ran, "rk"], writes=[ran])
                        P.op("dve", lambda e: e.tensor_scalar(ra[:], ra[:], 3.1415925, -3.1415925, op0=ALU.min, op1=ALU.max),
                             reads=[ran], writes=[ran])
                    for t in range(NT):
                        xb_ = xt[t % 2]
                        xn = "xt%d" % (t % 2)
                        P.dma("sp", lambda e: e.dma_start(out=xb_[:], in_=x[s, t * 128:(t + 1) * 128, :]), writes=[xn])
                        for hf in range(2):
                            bk = (t * 2 + hf) % 4
                            for j in range(4):
                                kc = hf * 4 + j
                                P.op("pe", lambda e: e.transpose(B[bk][:, j * 128:(j + 1) * 128], xb_[:, kc * 128:(kc + 1) * 128], ident_f),
                                     reads=[xn, "cm_f"], writes=[BN[bk]])
                            evac(xT[:, hf * 4:(hf + 1) * 4, t * 128:(t + 1) * 128],
                                 B[bk][:].rearrange("p (a b) -> p a b", a=4), reads=[BN[bk]], writes=[("xT", t)], eng="act")
                    P.op("act", lambda e: e.activation(out=sinT[:], in_=ras[0][:], func=AF.Sin), reads=["ra0"], writes=["sinT"])
                    P.op("act", lambda e: e.activation(out=cosT[:], in_=ras[1][:], func=AF.Sin), reads=["ra1"], writes=["cosT"])
                    if stop == "a1":
                        P.dma("sp", lambda e: e.dma_start(out=dbg["oT"][s], in_=xT[:]), reads=[("xT", t_) for t_ in range(NT)], writes=["dbg"])
                    for t in range(NT):
                        for hf in range(2):
                            bk = 4 + (t * 2 + hf) % 4
                            for kc in range(8):
                                P.op("pe", lambda e: e.matmul(B[bk][:], lhsT=xT[:, kc, t * 128:(t + 1) * 128],
                                                              rhs=wv[:, kc, hf * 512:(hf + 1) * 512],
                                                              start=(kc == 0), stop=(kc == 7)),
                                     reads=[("xT", t), ("wv", kc // 4)], writes=[BN[bk]])
                            evac(V[:, t, hf * 4:(hf + 1) * 4, 0:128], B[bk][:].rearrange("p (a b) -> p a b", a=4),
                                 reads=[BN[bk]], writes=[("V", t)])
                    P.barrier()
                if s == 0:
                    emit_xs_zero_fill()
                if stop in ("a1", "rope", "v"):
                    P.barrier()
                    continue
                with contextlib.ExitStack() as s2c:
                    wq = [sb(s2c, "wq%d" % i, [128, 8, 128], BF16) for i in range(2)]
                    wk = [sb(s2c, "wk%d" % i, [128, 8, 128], BF16) for i in range(2)]
                    qT = [sb(s2c, "qT%d" % i, [128, S], BF16) for i in range(2)]
                    kz = [[sb(s2c, "kz%d_%d" % (i, m), [128, S], BF16) for m in range(2)] for i in range(2)]
                    qb = [sb(s2c, "qb%d" % i, [128, 512], BF16) for i in range(2)]
                    t1 = [sb(s2c, "t1_%d" % i, [128, 512], F32) for i in range(2)]
                    t2 = [sb(s2c, "t2_%d" % i, [128, 512], F32) for i in range(2)]
                    PT = [sb(s2c, "PT%d" % i, [128, 512], BF16) for i in range(4)]
                    accs = [sb(s2c, "accs%d" % i, [128, 1032], F32) for i in range(2)]
                    rec3 = sb(s2c, "rec3", [128, 4, 2, 1], F32)
                    O4 = sb(s2c, "O4", [128, 4, 128], F32)
                    T4 = sb(s2c, "T4", [128, 4, 128], F32)
                    ss = sb(s2c, "ss", [128, 4], F32)
                    ssv = sb(s2c, "ssv", [128, 4], F32)
                    rstd3 = sb(s2c, "rstd3", [128, 4, 1], F32)
                    onb = [sb(s2c, "onb%d" % i, [128, 4, 128], BF16) for i in range(2)]
                    rope_i = [0]
                    NG = S // 512
                    for i_ in range(2):
                        P.op("pool", lambda e: e.memset(kz[i_][0][64:128, :], 0.0), writes=[("kzz", i_, 0)])
                        P.op("pool", lambda e: e.memset(kz[i_][1][0:64, :], 0.0), writes=[("kzz", i_, 1)])

                    def load_head_w(h):
                        b = h % 2
                        P.dma("pool", lambda e: e.dma_start(out=wq[b][:], in_=w_in_k[:, :, h * 128:(h + 1) * 128]),
                              writes=["wq%d" % b])
                        P.dma("pool", lambda e: e.dma_start(out=wk[b][:], in_=w_in_k[:, :, 1024 + h * 128:1024 + (h + 1) * 128]),
                              writes=["wk%d" % b])

                    def proj_units(h):
                        hb = h % 2
                        units = []
                        for which in range(2):
                            w_t = (wq if which == 0 else wk)[hb]
                            wname = ("wq%d" if which == 0 else "wk%d") % hb
                            for tg in range(NG):
                                st_ = {}

                                def part_a(w_t=w_t, wname=wname, tg=tg, st_=st_):
                                    i = rope_i[0]
                                    rope_i[0] += 1
                                    st_["i"] = i
                                    r = i % 2
                                    pb = 3 if i % 2 == 0 else 7
                                    tsl = slice(tg * 512, (tg + 1) * 512)
                                    for kc in range(8):
                                        P.op("pe", lambda e: e.matmul(B[pb][:], lhsT=w_t[:, kc, :], rhs=xT[:, kc, tsl],
                                                                      start=(kc == 0), stop=(kc == 7)),
                                             reads=[wname], writes=[BN[pb]])
                                    P.op("act", lambda e: e.activation(out=qb[r][:], in_=B[pb][:], func=AF.Copy),
                                         reads=[BN[pb]], writes=["qb%d" % r])
                                    P.op("dve", lambda e: e.tensor_tensor(t1[r][:], B[pb][:], cosT[:, tsl], op=ALU.mult),
                                         reads=[BN[pb], "cosT"], writes=["t1_%d" % r])

                                def part_b(which=which, tg=tg, st_=st_, hb=hb):
                                    i = st_["i"]
                                    r = i % 2
                                    pb = 3 if i % 2 == 0 else 7
                                    tsl = slice(tg * 512, (tg + 1) * 512)
                                    P.op("pe", lambda e: e.matmul(B[pb][:], lhsT=prot_b, rhs=qb[r][:], start=True, stop=True),
                                         reads=["qb%d" % r, "cm_b"], writes=[BN[pb]])
                                    P.op("dve", lambda e: e.tensor_tensor(t2[r][:], B[pb][:], sinT[:, tsl], op=ALU.mult),
                                         reads=[BN[pb], "sinT"], writes=["t2_%d" % r])
                                    if which == 0:
                                        P.op("dve", lambda e: e.tensor_tensor(qT[hb][:, tsl], t1[r][:], t2[r][:], op=ALU.add),
                                             reads=["t1_%d" % r, "t2_%d" % r], writes=[("qT%d" % hb, tg)])
                                    else:
                                        for m in range(2):
                                            ps_ = slice(m * 64, (m + 1) * 64)
                                            P.op("dve", lambda e: e.tensor_tensor(kz[hb][m][ps_, tsl], t1[r][ps_, :], t2[r][ps_, :], op=ALU.add),
                                                 reads=["t1_%d" % r, "t2_%d" % r, ("kzz", hb, m)], writes=[("kz%d_%d" % (hb, m), tg)])
                                units.append(part_a)
                                units.append(part_b)
                        return units

                    LOOK = 2
                    gstep = [0]
                    deferred = []

                    def run_due(force=False):
                        keep = []
                        for due, fn in deferred:
                            if force or due <= gstep[0]:
                                fn()
                            else:
                                keep.append((due, fn))
                        deferred[:] = keep

                    def defer(delay, fn):
                        deferred.append((gstep[0] + delay, fn))

                    grp_i = [0]
                    load_head_w(0)
                    for u in proj_units(0):
                        u()
                    for h in range(H):
                        hb = h % 2
                        if h + 1 < H:
                            load_head_w(h + 1)
                            nxt = proj_units(h + 1)
                        else:
                            nxt = []
                        qn = "qT%d" % hb
                        steps = []
                        for g in range(NG):
                            for j in range(4 * g + 4):
                                for m in range(2):
                                    steps.append((g, j, m))
                        nsteps = len(steps)
                        unit_at = {}
                        if nxt:
                            gap = max(1, (nsteps - 8) // len(nxt))
                            for ui, u in enumerate(nxt):
                                unit_at.setdefault(4 + ui * gap, []).append(u)
                        started = {}
                        pv_pending = []

                        def emit_pv(g, j, m, pt, ptn, q0):
                            for il in range(q0 - 4 * g, 4):
                                a = il * 2 + m
                                ab = 4 + a // 3
                                col = (a % 3) * 129
                                c0 = (il - (q0 - 4 * g)) * 128
                                first = (g, ab) not in started
                                started[(g, ab)] = True
                                P.op("pe", lambda e: e.matmul(B[ab][:, col:col + 129], lhsT=pt[:, c0:c0 + 128],
                                                              rhs=V[:, j, h, :], start=first, stop=(j == 4 * g + il),
                                                              skip_group_check=True),
                                     reads=[ptn, (ptn, "m"), ("V", j), "Vones"], writes=[BN[ab]])
                            if j == 4 * g + 3 and m == 1:
                                emit_group_end(g)

                        def emit_group_end(g, h=h):
                            gi = grp_i[0]
                            grp_i[0] += 1
                            ac = accs[gi % 2]
                            acn = "accs%d" % (gi % 2)
                            evac(ac[:, 0:387], B[4][:, 0:387], reads=[BN[4]], writes=[(acn, 0)], eng="dve")
                            evac(ac[:, 387:774], B[5][:, 0:387], reads=[BN[5]], writes=[(acn, 1)], eng="act")
                            evac(ac[:, 774:1032], B[6][:, 0:258], reads=[BN[6]], writes=[(acn, 2)], eng="dve")
                            acv = ac[:, :].rearrange("p (i m c) -> p i m c", i=4, m=2)
                            acr = [(acn, 0), (acn, 1), (acn, 2)]

                            def norm_1():
                                P.op("dve", lambda e: e.reciprocal(rec3[:, :, :, 0], acv[:, :, :, 128]), reads=acr, writes=["rec"])
                                P.op("dve", lambda e: e.tensor_scalar(rec3[:, :, 1, 0], rec3[:, :, 1, 0], lamneg, None, op0=ALU.mult),
                                     reads=["rec", "small"], writes=["rec"])
                                P.op("dve", lambda e: e.tensor_tensor(O4[:], acv[:, :, 0, 0:128], rec3[:, :, 0, :].to_broadcast([128, 4, 128]), op=ALU.mult),
                                     reads=acr + ["rec"], writes=["O4"])
                                P.op("dve", lambda e: e.tensor_tensor(T4[:], acv[:, :, 1, 0:128], rec3[:, :, 1, :].to_broadcast([128, 4, 128]), op=ALU.mult),
                                     reads=acr + ["rec"], writes=["T4"])

                            def norm_2():
                                P.op("dve", lambda e: e.tensor_tensor(O4[:], O4[:], T4[:], op=ALU.add), reads=["O4", "T4"], writes=["O4"])
                                P.op("dve", lambda e: e.tensor_tensor(T4[:], O4[:], O4[:], op=ALU.mult), reads=["O4"], writes=["T4"])
                                P.op("dve", lambda e: e.reduce_sum(ss[:], T4[:], axis=AX.X), reads=["T4"], writes=["ss"])
                                P.op("dve", lambda e: e.tensor_scalar(ssv[:], ss[:], 1.0 / 128.0, RMS_EPS, op0=ALU.mult, op1=ALU.add),
                                     reads=["ss"], writes=["ssv"])
                                P.op("pool", lambda e: e.tensor_tensor(rstd3[:, :, 0], ssv[:], negh[:], op=ALU.pow), reads=["ssv", "negh"], writes=["rstd"])

                            def norm_on():
                                ob = onb[gi % 2]
                                obn = "onb%d" % (gi % 2)
                                P.op("dve", lambda e: e.tensor_tensor(O4[:], O4[:], rstd3[:].to_broadcast([128, 4, 128]), op=ALU.mult),
                                     reads=["O4", "rstd"], writes=["O4"])
                                P.op("dve", lambda e: e.tensor_tensor(ob[:], O4[:], gainb[:, None, :].to_broadcast([128, 4, 128]), op=ALU.mult),
                                     reads=["O4", "gainb"], writes=[obn])

                            def norm_b():
                                ob = onb[gi % 2]
                                obn = "onb%d" % (gi % 2)
                                for il in range(4):
                                    P.op("pe", lambda e: e.transpose(bank_bf(7)[:, il * 128:(il + 1) * 128], ob[:, il, :], ident_b),
                                         reads=[obn, "cm_b"], writes=[BN[7]])
                                evac(oT[:, h, g * 512:(g + 1) * 512], bank_bf(7)[:, 0:512], reads=[BN[7]], writes=[("oT", g)])

                            defer(1, norm_1)
                            defer(3, norm_2)
                            defer(6, norm_on)
                            defer(11, norm_b)

                        for si, (g, j, m) in enumerate(steps):
                            gstep[0] += 1
                            run_due()
                            conv_tick[0] += 1
                            if conv_tick[0] % CONV_EVERY == 0:
                                conv_issue()
                            for u in unit_at.get(si, []):
                                u()
                            q0 = max(j, 4 * g)
                            N = (4 * g + 4 - q0) * 128
                            qsl = slice(q0 * 128, (4 * g + 4) * 128)
                            i = gstep[0]
                            bk = i % 3
                            pt = PT[i % 4]
                            ptn = "PT%d" % (i % 4)
                            P.op("pe", lambda e: e.matmul(B[bk][:, 0:N], lhsT=kz[hb][m][:, j * 128:(j + 1) * 128],
                                                          rhs=qT[hb][:, qsl], start=True, stop=True),
                                 reads=[("kz%d_%d" % (hb, m), j // 4), ("kzz", hb, m), (qn, g)], writes=[BN[bk]])
                            if j >= 4 * g:
                                P.op("act", lambda e: e.activation(out=pt[:, 0:64], in_=B[bk][:, 0:64], func=AF.Exp, scale=0.125, bias=cv[:, 112:113]),
                                     reads=[BN[bk], "cv"], writes=[(ptn, "m")])
                                P.op("act", lambda e: e.activation(out=pt[:, 64:N], in_=B[bk][:, 64:N], func=AF.Exp, scale=0.125),
                                     reads=[BN[bk]], writes=[ptn])
                            else:
                                P.op("act", lambda e: e.activation(out=pt[:, 0:N], in_=B[bk][:, 0:N], func=AF.Exp, scale=0.125),
                                     reads=[BN[bk]], writes=[ptn, (ptn, "m")])
                            pv_pending.append((g, j, m, pt, ptn, q0))
                            if len(pv_pending) > LOOK:
                                emit_pv(*pv_pending.pop(0))
                        while pv_pending:
                            emit_pv(*pv_pending.pop(0))
                        for si_ in sorted(unit_at):
                            if si_ >= nsteps:
                                for u in unit_at[si_]:
                                    u()
                    run_due(force=True)
                    if s == NSEQ - 1:
                        conv_issue(len(conv_jobs))
                    P.barrier()
            if stop in ("attn", "qk"):
                P.dma("sp", lambda e: e.dma_start(out=dbg["oT"][s], in_=oT[:]), reads=[], writes=["dbg"])
                P.barrier()
                continue
            with contextlib.ExitStack() as s3:
                mergedT = sb(s3, "mergedT", [128, 8, S], BF16)
                mixedT = sb(s3, "mixedT", [128, 4, S], BF16)
                NG = S // 512
                with contextlib.ExitStack() as s3a:
                    wu = sb(s3a, "wu", [128, 8, 512], BF16)
                    uT = sb(s3a, "uT", [128, 4, S], F32)
                    Tp = [sb(s3a, "Tp%d" % i, [128, S], F32) for i in range(2)]
                    pooledT = sb(s3a, "pooledT", [128, 4, S], BF16)
                    for kc2 in range(2):
                        P.dma("pool", lambda e: e.dma_start(out=wu[:, kc2 * 4:(kc2 + 1) * 4, :],
                                                            in_=w_in_k[:, kc2 * 4:(kc2 + 1) * 4, 3072:3584]),
                              writes=[("wu", kc2)])
                    bi = 0
                    for g in range(4):
                        for tg in range(NG):
                            bk = bi % 4
                            bi += 1
                            tsl = slice(tg * 512, (tg + 1) * 512)
                            for kc in range(8):
                                P.op("pe", lambda e: e.matmul(B[bk][:], lhsT=wu[:, kc, g * 128:(g + 1) * 128], rhs=xT[:, kc, tsl],
                                                              start=(kc == 0), stop=(kc == 7)),
                                     reads=[("wu", kc // 4)], writes=[BN[bk]])
                            evac(uT[:, g, tsl], B[bk][:], reads=[BN[bk]], writes=[("uT", g)])
                    for g, w in enumerate((2, 4, 8, 16)):
                        cur, cur_r = uT[:, g, :], [("uT", g)]
                        k = 0
                        sh = 1
                        while sh < w:
                            dst, dstn = Tp[k % 2], "Tp%d" % (k % 2)
                            eng = "dve"
                            P.op(eng, lambda e: e.tensor_tensor(dst[:, sh:S], cur[:, sh:S], cur[:, 0:S - sh], op=ALU.add),
                                 reads=cur_r, writes=[dstn])
                            P.op(eng, lambda e: e.tensor_copy(dst[:, 0:sh], cur[:, 0:sh]), reads=cur_r, writes=[dstn + "h"])
                            cur, cur_r = dst[:, :], [dstn, dstn + "h"]
                            k += 1
                            sh *= 2
                        P.op("dve", lambda e: e.tensor_tensor(cur[:, 0:16], cur[:, 0:16], cv[:, 16 + g * 16:32 + g * 16], op=ALU.mult),
                             reads=cur_r + ["cv"], writes=cur_r)
                        P.op("dve", lambda e: e.scalar_tensor_tensor(pooledT[:, g, :], cur, 1.0 / w, uT[:, g, :], op0=ALU.mult, op1=ALU.subtract),
                             reads=cur_r + [("uT", g)], writes=[("pooledT", g)])
                    for g in range(4):
                        for tg in range(NG):
                            bk = bi % 4
                            bi += 1
                            tsl = slice(tg * 512, (tg + 1) * 512)
                            P.op("pe", lambda e: e.matmul(B[bk][:], lhsT=wpool_b[:, g, :], rhs=pooledT[:, g, tsl], start=True, stop=True),
                                 reads=[("pooledT", g), "wpool_b"], writes=[BN[bk]])
                            P.op("dve", lambda e: e.tensor_scalar(mixedT[:, g, tsl], B[bk][:], pscale[:, g:g + 1], None, op0=ALU.mult),
                                 reads=[BN[bk], "pscale"], writes=[("mixedT", g)])
                    P.barrier()
                with contextlib.ExitStack() as s3b:
                    wa_c = [sb(s3b, "wa_c%d" % i, [128, 8, 128], BF16) for i in range(2)]
                    wb_c = [sb(s3b, "wb_c%d" % i, [128, 4, 128], BF16) for i in range(2)]
                    wga_c = [sb(s3b, "wga_c%d" % i, [128, 8, 128], BF16) for i in range(2)]
                    wgb_c = [sb(s3b, "wgb_c%d" % i, [128, 8, 128], BF16) for i in range(2)]
                    ga = [sb(s3b, "ga%d" % i, [128, 512], F32) for i in range(2)]
                    gb = [sb(s3b, "gb%d" % i, [128, 512], F32) for i in range(2)]
                    m1 = [sb(s3b, "m1_%d" % i, [128, 512], F32) for i in range(2)]
                    m2 = [sb(s3b, "m2_%d" % i, [128, 512], F32) for i in range(2)]

                    def load_chunk_w(c):
                        b = c % 2
                        cs = slice(c * 128, (c + 1) * 128)
                        P.dma("pool", lambda e: e.dma_start(out=wa_c[b][:], in_=w_a_k[:, :, cs]), writes=["wa_c%d" % b])
                        P.dma("pool", lambda e: e.dma_start(out=wb_c[b][:], in_=w_b_k[:, :, cs]), writes=["wb_c%d" % b])
                        P.dma("pool", lambda e: e.dma_start(out=wga_c[b][:], in_=w_in_k[:, :, 3584 + c * 128:3584 + (c + 1) * 128]),
                              writes=["wga_c%d" % b])
                        P.dma("pool", lambda e: e.dma_start(out=wgb_c[b][:], in_=w_in_k[:, :, 4608 + c * 128:4608 + (c + 1) * 128]),
                              writes=["wgb_c%d" % b])

                    load_chunk_w(0)
                    it = 0
                    for c in range(8):
                        cb = c % 2
                        if c + 1 < 8:
                            load_chunk_w(c + 1)
                        for tg in range(NG):
                            r = it % 2
                            it += 1
                            b0 = r * 4
                            tsl = slice(tg * 512, (tg + 1) * 512)
                            for hh_ in range(8):
                                P.op("pe", lambda e: e.matmul(B[b0][:], lhsT=wa_c[cb][:, hh_, :], rhs=oT[:, hh_, tsl],
                                                              start=(hh_ == 0), stop=(hh_ == 7)),
                                     reads=["wa_c%d" % cb], writes=[BN[b0]])
                            for g in range(4):
                                P.op("pe", lambda e: e.matmul(B[b0 + 1][:], lhsT=wb_c[cb][:, g, :], rhs=mixedT[:, g, tsl],
                                                              start=(g == 0), stop=(g == 3)),
                                     reads=["wb_c%d" % cb], writes=[BN[b0 + 1]])
                            for kc in range(8):
                                P.op("pe", lambda e: e.matmul(B[b0 + 2][:], lhsT=wga_c[cb][:, kc, :], rhs=xT[:, kc, tsl],
                                                              start=(kc == 0), stop=(kc == 7)),
                                     reads=["wga_c%d" % cb], writes=[BN[b0 + 2]])
                            for kc in range(8):
                                P.op("pe", lambda e: e.matmul(B[b0 + 3][:], lhsT=wgb_c[cb][:, kc, :], rhs=xT[:, kc, tsl],
                                                              start=(kc == 0), stop=(kc == 7)),
                                     reads=["wgb_c%d" % cb], writes=[BN[b0 + 3]])
                            P.op("act", lambda e: e.activation(out=ga[r][:], in_=B[b0 + 2][:], func=AF.Sigmoid, bias=gbias[:, c:c + 1]),
                                 reads=[BN[b0 + 2], "gbias"], writes=["ga%d" % r])
                            P.op("act", lambda e: e.activation(out=gb[r][:], in_=B[b0 + 3][:], func=AF.Sigmoid, bias=gbias[:, 8 + c:9 + c]),
                                 reads=[BN[b0 + 3], "gbias"], writes=["gb%d" % r])
                            P.op("dve", lambda e: e.tensor_tensor(m1[r][:], B[b0][:], ga[r][:], op=ALU.mult),
                                 reads=[BN[b0], "ga%d" % r], writes=["m1_%d" % r])
                            P.op("dve", lambda e: e.tensor_tensor(m2[r][:], B[b0 + 1][:], gb[r][:], op=ALU.mult),
                                 reads=[BN[b0 + 1], "gb%d" % r], writes=["m2_%d" % r])
                            P.op("pool", lambda e: e.tensor_tensor(mergedT[:, c, tsl], m1[r][:], m2[r][:], op=ALU.add),
                                 reads=["m1_%d" % r, "m2_%d" % r], writes=[("mergedT", c)])
                    P.barrier()
                if stop == "mix":
                    P.dma("sp", lambda e: e.dma_start(out=dbg["mergedT"][s], in_=mergedT[:]), reads=[], writes=["dbg"])
                    P.barrier()
                    continue
                with contextlib.ExitStack() as s3c:
                    wout = sb(s3c, "wout", [128, 8, D], BF16)
                    g1 = sb(s3c, "g1", [128, D], F32)
                    b1 = sb(s3c, "b1", [128, D], F32)
                    xt = [sb(s3c, "xt%d" % i, [128, D], F32) for i in range(2)]
                    zs = [sb(s3c, "z%d" % i, [128, D], F32) for i in range(3)]
                    hh = [sb(s3c, "hh%d" % i, [128, D], F32) for i in range(3)]
                    hbf = [sb(s3c, "hbf%d" % i, [128, D], BF16) for i in range(4)]
                    hTs = [sb(s3c, "hT%d" % i, [128, 8, 128], F32) for i in range(2)]
                    statss = [sb(s3c, "stats%d" % i, [128, 2, 6], F32) for i in range(3)]
                    mvs = [sb(s3c, "mv%d" % i, [128, 4], F32) for i in range(3)]
                    rrs = [sb(s3c, "rr%d" % i, [128, 16], F32) for i in range(2)]
                    nm = sb(s3c, "nm", [128, 3], F32)
                    lg = sb(s3c, "lg", [128, 36], F32)
                    ge = sb(s3c, "ge", [128, 4], F32)
                    gone = sb(s3c, "gone", [128, 4], F32)
                    mneg = sb(s3c, "mneg", [128, 4, 1], F32)
                    elms = [sb(s3c, "elm%d" % i, [128, 4, 8], F32) for i in range(2)]
                    elm2 = sb(s3c, "elm2", [128, 32], F32)
                    oh1s = [sb(s3c, "oh1_%d" % i, [128, 32], F32) for i in range(3)]
                    oh2s = [sb(s3c, "oh2_%d" % i, [128, 32], F32) for i in range(2)]
                    Mbs = [sb(s3c, "Mb%d" % i, [128, 32], BF16) for i in range(2)]
                    posin = sb(s3c, "posin", [128, 32], F32)
                    valid = sb(s3c, "valid", [128, 32], F32)
                    smv = sb(s3c, "smv", [128, 32], F32)
                    tmp32 = sb(s3c, "tmp32", [128, 32], F32)
                    for kc2 in range(2):
                        P.dma("pool", lambda e: e.dma_start(out=wout[:, kc2 * 4:(kc2 + 1) * 4, :], in_=w_out_k[:, kc2 * 4:(kc2 + 1) * 4, :]),
                              writes=[("wout", kc2)])
                    P.dma("sp", lambda e: e.dma_start(out=g1[:], in_=ln1_g[0:1, :].to_broadcast([128, D])), writes=["g1"])
                    P.dma("sp", lambda e: e.dma_start(out=b1[:], in_=ln1_b[0:1, :].to_broadcast([128, D])), writes=["b1"])

                    def L0a(t):
                        r = t % 2
                        xb_, xn = xt[r], "xt%d" % r
                        P.dma("sp", lambda e: e.dma_start(out=xb_[:], in_=x[s, t * 128:(t + 1) * 128, :]), writes=[xn])
                        for hf in range(2):
                            bk = (0, 1)[hf] if r == 0 else (6, 7)[hf]
                            for kc in range(8):
                                P.op("pe", lambda e: e.matmul(B[bk][:], lhsT=mergedT[:, kc, t * 128:(t + 1) * 128],
                                                              rhs=wout[:, kc, hf * 512:(hf + 1) * 512], start=(kc == 0), stop=(kc == 7)),
                                     reads=[("wout", kc // 4)], writes=[BN[bk]])

                    def L0b(t):
                        r = t % 2
                        q3 = t % 3
                        xb_, xn = xt[r], "xt%d" % r
                        z, stats, mv = zs[q3], statss[q3], mvs[q3]
                        zn = "z%d" % q3
                        for hf in range(2):
                            bk = (0, 1)[hf] if r == 0 else (6, 7)[hf]
                            P.op("dve", lambda e: e.scalar_tensor_tensor(z[:, hf * 512:(hf + 1) * 512], xb_[:, hf * 512:(hf + 1) * 512], ALPHA, B[bk][:],
                                                                         op0=ALU.mult, op1=ALU.add),
                                 reads=[xn, BN[bk]], writes=[(zn, hf)])

                    def L0c(t):
                        q3 = t % 3
                        z, stats, mv = zs[q3], statss[q3], mvs[q3]
                        zn = "z%d" % q3
                        for hf in range(2):
                            P.op("dve", lambda e: e.bn_stats(stats[:, hf, :], z[:, hf * 512:(hf + 1) * 512]), reads=[(zn, hf)], writes=[("stats", q3, hf)])
                        P.op("dve", lambda e: e.bn_aggr(mv[:, 0:2], stats[:].rearrange("p a b -> p (a b)")), reads=[("stats", q3, 0), ("stats", q3, 1)], writes=[("mv", q3)])
                        P.op("dve", lambda e: e.tensor_scalar(mv[:, 2:3], mv[:, 1:2], LN_EPS, None, op0=ALU.add), reads=[("mv", q3)], writes=[("mv2", q3)])

                    def L1a(t):
                        q3 = t % 3
                        z, mv = zs[q3], mvs[q3]
                        zn = "z%d" % q3
                        hb_, hn = hh[q3], "hh%d" % q3
                        P.op("pool", lambda e: e.tensor_tensor(mv[:, 3:4], mv[:, 2:3], negh[:, 0:1], op=ALU.pow), reads=[("mv2", q3), "negh"], writes=[("mv3", q3)])
                        P.op("dve", lambda e: e.scalar_tensor_tensor(nm[:, q3:q3 + 1], mv[:, 0:1], -1.0, mv[:, 3:4], op0=ALU.mult, op1=ALU.mult),
                             reads=[("mv", q3), ("mv3", q3)], writes=[("nm", q3)])
                        P.op("act", lambda e: e.activation(out=hb_[:], in_=z[:], func=AF.Identity, scale=mv[:, 3:4], bias=nm[:, q3:q3 + 1]),
                             reads=[(zn, 0), (zn, 1), ("mv3", q3), ("nm", q3)], writes=[hn])

                    def L1a2(t):
                        q3 = t % 3
                        hb_, hn = hh[q3], "hh%d" % q3
                        P.op("dve", lambda e: e.tensor_tensor(hb_[:], hb_[:], g1[:], op=ALU.mult), reads=[hn, "g1"], writes=[hn])
                        P.op("dve", lambda e: e.tensor_tensor(hb_[:], hb_[:], b1[:], op=ALU.add), reads=[hn, "b1"], writes=[hn])

                    def L1b(t):
                        gt = s * NT + t
                        r = t % 2
                        r3 = t % 3
                        hb_, hn = hh[r3], "hh%d" % r3
                        P.dma("sp", lambda e: e.dma_start(out=h_scr[gt * 128:(gt + 1) * 128, :], in_=hb_[:]), reads=[hn], writes=[("h_scr", gt)])
                        if "h" in dbg:
                            P.dma("sp", lambda e: e.dma_start(out=dbg["h"][gt * 128:(gt + 1) * 128, :], in_=hb_[:]), reads=[hn], writes=[("dbg_h", gt)])
                        P.op("act", lambda e: e.activation(out=hbf[t % 4][:], in_=hb_[:], func=AF.Copy), reads=[hn], writes=["hbf%d" % (t % 4)])
                        for hf in range(2):
                            bk = 2 + hf
                            for j in range(4):
                                kc = hf * 4 + j
                                P.op("pe", lambda e: e.transpose(B[bk][:, j * 128:(j + 1) * 128], hb_[:, kc * 128:(kc + 1) * 128], ident_f),
                                     reads=[hn, "cm_f"], writes=[BN[bk]])
                            evac(hTs[r][:, hf * 4:(hf + 1) * 4, :], B[bk][:].rearrange("p (a b) -> p a b", a=4), reads=[BN[bk]], writes=[("hT", r, hf)], eng="act")

                    def L2a1(t):
                        gt = s * NT + t
                        r = t % 2
                        hT = hTs[r]
                        rr = rrs[r]
                        elm = elms[r]
                        elm_f = elm[:].rearrange("p a b -> p (a b)")
                        oh1 = oh1s[t % 3]
                        o1n = "oh1_%d" % (t % 3)
                        for kc in range(8):
                            P.op("pe", lambda e: e.matmul(B[4][:, 0:36], lhsT=hT[:, kc, :], rhs=wrt[:, kc, :], start=(kc == 0), stop=(kc == 7)),
                                 reads=[("hT", r, kc // 4), "wrt"], writes=[BN[4]])
                        P.op("dve", lambda e: e.tensor_tensor(lg[:], B[4][:, 0:36], brt[:], op=ALU.add), reads=[BN[4], "brt"], writes=["lg"])
                        P.op("dve", lambda e: e.reduce_max(rr[:, 0:1], lg[:, 0:4], axis=AX.X), reads=["lg"], writes=[("rr0", r)])
                        P.op("dve", lambda e: e.tensor_scalar(rr[:, 1:2], rr[:, 0:1], -1.0, None, op0=ALU.mult), reads=[("rr0", r)], writes=[("rr1", r)])
                        P.op("act", lambda e: e.activation(out=ge[:], in_=lg[:, 0:4], func=AF.Exp, bias=rr[:, 1:2]), reads=["lg", ("rr1", r)], writes=["ge"])
                        P.op("dve", lambda e: e.reduce_sum(rr[:, 2:3], ge[:], axis=AX.X), reads=["ge"], writes=[("rr2", r)])
                        P.op("dve", lambda e: e.reciprocal(rr[:, 3:4], rr[:, 2:3]), reads=[("rr2", r)], writes=[("rr3", r)])
                        P.op("dve", lambda e: e.tensor_scalar(gone[:], lg[:, 0:4], rr[:, 0:1], None, op0=ALU.is_equal), reads=["lg", ("rr0", r)], writes=["gone"])
                        P.op("dve", lambda e: e.tensor_scalar(mneg[:, :, 0], gone[:], -1.0, 1e30, op0=ALU.add, op1=ALU.mult), reads=["gone"], writes=["mneg"])
                        P.op("dve", lambda e: e.tensor_tensor(elm[:], lg[:, 4:36].rearrange("p (a b) -> p a b", a=4), mneg[:].to_broadcast([128, 4, 8]), op=ALU.add),
                             reads=["lg", "mneg"], writes=[("elm", r)])
                        P.op("dve", lambda e: e.reduce_max(rr[:, 4:5], elm_f, axis=AX.X), reads=[("elm", r)], writes=[("rr4", r)])
                        P.op("dve", lambda e: e.tensor_scalar(oh1[:], elm_f, rr[:, 4:5], None, op0=ALU.is_equal), reads=[("elm", r), ("rr4", r)], writes=[o1n])

                    def L2a2(t):
                        gt = s * NT + t
                        r = t % 2
                        rr = rrs[r]
                        elm = elms[r]
                        elm_f = elm[:].rearrange("p a b -> p (a b)")
                        oh1, oh2, Mb = oh1s[t % 3], oh2s[r], Mbs[r]
                        o1n, o2n, mbn = "oh1_%d" % (t % 3), "oh2_%d" % r, "Mb%d" % r
                        P.op("dve", lambda e: e.scalar_tensor_tensor(elm2[:], oh1[:], -1e30, elm_f, op0=ALU.mult, op1=ALU.add), reads=[o1n, ("elm", r)], writes=["elm2"])
                        P.op("dve", lambda e: e.reduce_max(rr[:, 5:6], elm2[:], axis=AX.X), reads=["elm2"], writes=[("rr5", r)])
                        P.op("dve", lambda e: e.tensor_scalar(oh2[:], elm2[:], rr[:, 5:6], None, op0=ALU.is_equal), reads=["elm2", ("rr5", r)], writes=[o2n])
                        P.op("dve", lambda e: e.tensor_tensor(rr[:, 6:7], rr[:, 5:6], rr[:, 4:5], op=ALU.subtract), reads=[("rr4", r), ("rr5", r)], writes=[("rr6", r)])
                        P.op("act", lambda e: e.activation(out=rr[:, 7:8], in_=rr[:, 6:7], func=AF.Exp), reads=[("rr6", r)], writes=[("rr7", r)])
                        P.op("dve", lambda e: e.tensor_scalar(rr[:, 8:9], rr[:, 7:8], 1.0, None, op0=ALU.add), reads=[("rr7", r)], writes=[("rr8", r)])
                        P.op("dve", lambda e: e.reciprocal(rr[:, 9:10], rr[:, 8:9]), reads=[("rr8", r)], writes=[("rr9", r)])
                        P.op("dve", lambda e: e.tensor_tensor(wts[:, gt, 0:1], rr[:, 9:10], rr[:, 3:4], op=ALU.mult), reads=[("rr9", r), ("rr3", r)], writes=[("wts", gt, 0)])
                        P.op("dve", lambda e: e.tensor_tensor(rr[:, 10:11], rr[:, 7:8], rr[:, 9:10], op=ALU.mult), reads=[("rr7", r), ("rr9", r)], writes=[("rr10", r)])
                        P.op("dve", lambda e: e.tensor_tensor(wts[:, gt, 1:2], rr[:, 10:11], rr[:, 3:4], op=ALU.mult), reads=[("rr10", r), ("rr3", r)], writes=[("wts", gt, 1)])
                        P.op("dve", lambda e: e.tensor_tensor(Mb[:], oh1[:], oh2[:], op=ALU.add), reads=[o1n, o2n], writes=[mbn])

                    def L2b(t):
                        gt = s * NT + t
                        r = t % 2
                        r3 = t % 3
                        oh1, oh2, Mb = oh1s[t % 3], oh2s[r], Mbs[r]
                        o1n, o2n, mbn = "oh1_%d" % (t % 3), "oh2_%d" % r, "Mb%d" % r
                        r4 = t % 4
                        P.op("pe", lambda e: e.matmul(B[5][:, 0:32], lhsT=tri_b, rhs=Mb[:], start=True, stop=True, skip_group_check=True),
                             reads=[mbn, "cm_b"], writes=[BN[5]])
                        P.op("pe", lambda e: e.matmul(B[5][:, 32:64], lhsT=ones_b, rhs=Mb[:], start=False, stop=True, skip_group_check=True),
                             reads=[mbn, "cm_b"], writes=[BN[5]])
                        P.op("dve", lambda e: e.tensor_tensor(posin[:], B[5][:, 0:32], cnt[:], op=ALU.add), reads=[BN[5], "cnt"], writes=["posin"])
                        P.op("dve", lambda e: e.tensor_scalar(valid[:], posin[:], CAP - 0.5, None, op0=ALU.is_lt), reads=["posin"], writes=["valid"])
                        P.op("dve", lambda e: e.tensor_tensor(smv[:], posin[:], eoffmd, op=ALU.add), reads=["posin", "cv"], writes=["smv"])
                        P.op("dve", lambda e: e.tensor_tensor(smv[:], smv[:], valid[:], op=ALU.mult), reads=["smv", "valid"], writes=["smv"])
                        for k_, ohk, ohn in ((0, oh1, o1n), (1, oh2, o2n)):
                            P.op("dve", lambda e: e.tensor_tensor(tmp32[:], smv[:], ohk[:], op=ALU.mult), reads=["smv", ohn], writes=["tmp32"])
                            P.op("dve", lambda e: e.reduce_sum(slots_f[:, gt, k_:k_ + 1], tmp32[:], axis=AX.X), reads=["tmp32"], writes=[("slots_f", gt, k_)])
                        P.op("dve", lambda e: e.tensor_tensor(cnt[:], cnt[:], B[5][:, 32:64], op=ALU.add), reads=["cnt", BN[5]], writes=["cnt"])
                        P.op("dve", lambda e: e.tensor_scalar(slots_f[:, gt, :], slots_f[:, gt, :], float(DUMP), None, op0=ALU.add),
                             reads=[("slots_f", gt, 0), ("slots_f", gt, 1)], writes=[("slots_f2", gt)])
                        P.op("dve", lambda e: e.tensor_copy(slots_i[:, gt, :], slots_f[:, gt, :]), reads=[("slots_f2", gt)], writes=[("slots_i", gt)])
                        for k_ in range(2):
                            P.dma("pool", lambda e: e.indirect_dma_start(
                                out=xs[:, :], out_offset=bass.IndirectOffsetOnAxis(ap=slots_i[:, gt, k_:k_ + 1], axis=0),
                                in_=hbf[r4][:], in_offset=None), reads=["hbf%d" % r4, ("slots_i", gt)] + xs_init, writes=[("xs", gt, k_)])

                    pipeline(NT, [L0a, L0b, L0c, L1a, L1a2, L1b, L2a1, L2a2, L2b])
                    P.barrier()
        NTOT = NSEQ * NT
        if stop in ("ln1",):
            P.dma("sp", lambda e: e.dma_start(out=dbg["slots"][:, :, :], in_=slots_i[:]), writes=["dbg1"])
            P.dma("sp", lambda e: e.dma_start(out=dbg["wts"][:, :, :], in_=wts[:]), writes=["dbg2"])
            P.barrier()
            return nc
        if stop is not None and stop not in ("moe",):
            P.barrier()
            return nc

        xs_all = [("xs", gt, k_) for gt in range(NTOT) for k_ in range(2)]

        with contextlib.ExitStack() as s4:
            NWB = 3
            wg = [sb(s4, "wg%d" % i, [128, 8, D], BF16) for i in range(NWB)]
            wd = [sb(s4, "wd%d" % i, [128, 4, D], BF16) for i in range(NWB)]
            xsb3 = [sb(s4, "xsb3_%d" % i, [128, NBLK, D], BF16) for i in range(2)]
            xsT3 = [sb(s4, "xsT3_%d" % i, [128, 8, CAP], BF16) for i in range(2)]
            sg = [sb(s4, "sg%d" % i, [128, CAP], F32) for i in range(2)]
            actT3 = [sb(s4, "actT3_%d" % i, [128, 4, CAP], BF16) for i in range(2)]
            yb = [sb(s4, "yb%d" % i, [128, D], F32) for i in range(2)]

            def load_expert(e_):
                b = e_ % NWB
                for kc2 in range(2):
                    P.dma("sp", lambda e: e.dma_start(out=wg[b][:, kc2 * 4:(kc2 + 1) * 4, :],
                                                      in_=wgu_bf[e_].rearrange("(kc k) n -> k kc n", k=128)[:, kc2 * 4:(kc2 + 1) * 4, :]),
                          reads=[("wcv", e_, kc2)], writes=[("wg%d" % b, kc2)])
                P.dma("sp", lambda e: e.dma_start(out=wd[b][:], in_=wdn_bf[e_].rearrange("(kc k) n -> k kc n", k=128)),
                      reads=[("wcv", e_, 2)], writes=["wd%d" % b])

            for e0 in range(min(NWB, NE)):
                load_expert(e0)
            def E0(e_):
                r = e_ % 2
                for blk in range(NBLK):
                    row0 = e_ * CAP + blk * 128
                    P.dma("sp", lambda e: e.dma_start(out=xsb3[r][:, blk, :], in_=xs[row0:row0 + 128, :]), reads=xs_all, writes=[("xsb3", r, blk)])

            def E1(e_):
                r = e_ % 2
                for blk in range(NBLK):
                    bt = (e_ * NBLK + blk) % 2
                    for kc in range(8):
                        P.op("pe", lambda e: e.transpose(bank_bf(bt)[:, kc * 128:(kc + 1) * 128], xsb3[r][:, blk, kc * 128:(kc + 1) * 128], ident_b),
                             reads=[("xsb3", r, blk), "cm_b"], writes=[BN[bt]])
                    evac(xsT3[r][:, :, blk * 128:(blk + 1) * 128], bank_bf(bt)[:, :].rearrange("p (a b) -> p a b", a=8),
                         reads=[BN[bt]], writes=[("xsT3", r, blk)])

            def E2(e_):
                r = e_ % 2
                eb = e_ % NWB
                xr = [("xsT3", r, blk) for blk in range(NBLK)]
                for fc in range(4):
                    bg, bu = ((2, 3), (4, 5))[fc % 2]
                    for col0, bk in ((fc * 128, bg), (512 + fc * 128, bu)):
                        for kc in range(8):
                            P.op("pe", lambda e: e.matmul(B[bk][:, 0:CAP], lhsT=wg[eb][:, kc, col0:col0 + 128], rhs=xsT3[r][:, kc, :],
                                                          start=(kc == 0), stop=(kc == 7)),
                                 reads=xr + [("wg%d" % eb, kc // 4)], writes=[BN[bk]])
                    q_ = fc % 2
                    P.op("act", lambda e: e.activation(out=sg[q_][:], in_=B[bg][:, 0:CAP], func=AF.Silu), reads=[BN[bg]], writes=["sg%d" % q_])
                    P.op("dve", lambda e: e.tensor_tensor(actT3[r][:, fc, :], B[bu][:, 0:CAP], sg[q_][:], op=ALU.mult),
                         reads=[BN[bu], "sg%d" % q_], writes=[("actT3", r, fc)])

            def E3(e_):
                r = e_ % 2
                eb = e_ % NWB
                ar = [("actT3", r, fc) for fc in range(4)]
                for blk in range(NBLK):
                    q_ = (e_ * NBLK + blk) % 2
                    row0 = e_ * CAP + blk * 128
                    for half, bk in ((0, 6), (1, 7)):
                        for fc in range(4):
                            P.op("pe", lambda e: e.matmul(B[bk][:], lhsT=actT3[r][:, fc, blk * 128:(blk + 1) * 128], rhs=wd[eb][:, fc, half * 512:(half + 1) * 512],
                                                          start=(fc == 0), stop=(fc == 3)),
                                 reads=ar + ["wd%d" % eb], writes=[BN[bk]])
                        evac(yb[q_][:, half * 512:(half + 1) * 512], B[bk][:], reads=[BN[bk]], writes=[("yb%d" % q_, half)])
                    P.dma("pool", lambda e: e.dma_start(out=ys[row0:row0 + 128, :], in_=yb[q_][:]),
                          reads=[("yb%d" % q_, 0), ("yb%d" % q_, 1)], writes=[("ys", e_, blk)])
                if e_ + NWB < NE:
                    load_expert(e_ + NWB)

            pipeline(NE, [E0, E1, E2, E3])
            P.barrier()
        ys_all = [("ys", e_, blk) for e_ in range(NE) for blk in range(NBLK)] + ["ys_dump"]
        if stop == "moe":
            P.dma("sp", lambda e: e.dma_start(out=dbg["ys"][:, :], in_=ys[:, :]), writes=["dbg1"])
            P.dma("sp", lambda e: e.dma_start(out=dbg["slots"][:, :, :], in_=slots_i[:]), writes=["dbg2"])
            P.dma("sp", lambda e: e.dma_start(out=dbg["wts"][:, :, :], in_=wts[:]), writes=["dbg3"])
            P.barrier()
            return nc

        with contextlib.ExitStack() as s5:
            g2 = sb(s5, "g2", [128, D], F32)
            b2 = sb(s5, "b2", [128, D], F32)
            y1 = [sb(s5, "y1_%d" % i, [128, D], F32) for i in range(3)]
            y2 = [sb(s5, "y2_%d" % i, [128, D], F32) for i in range(3)]
            ht = [sb(s5, "ht%d" % i, [128, D], F32) for i in range(3)]
            za = [sb(s5, "za%d" % i, [128, D], F32) for i in range(2)]
            zc = [sb(s5, "zc%d" % i, [128, D], F32) for i in range(2)]
            oo = [sb(s5, "oo%d" % i, [128, D], F32) for i in range(2)]
            stats2 = [sb(s5, "stats2_%d" % i, [128, 2, 6], F32) for i in range(2)]
            mv2 = [sb(s5, "mv2_%d" % i, [128, 4], F32) for i in range(2)]
            nm2 = sb(s5, "nm2", [128, 2], F32)
            P.dma("sp", lambda e: e.dma_start(out=g2[:], in_=ln2_g[0:1, :].to_broadcast([128, D])), writes=["g2"])
            P.dma("sp", lambda e: e.dma_start(out=b2[:], in_=ln2_b[0:1, :].to_broadcast([128, D])), writes=["b2"])

            def c0(gt):
                r = gt % 3
                P.dma("pool", lambda e: e.indirect_dma_start(
                    out=y1[r][:], out_offset=None, in_=ys[:, :],
                    in_offset=bass.IndirectOffsetOnAxis(ap=slots_i[:, gt, 0:1], axis=0)), reads=ys_all, writes=["y1_%d" % r])
                P.dma("pool", lambda e: e.indirect_dma_start(
                    out=y2[r][:], out_offset=None, in_=ys[:, :],
                    in_offset=bass.IndirectOffsetOnAxis(ap=slots_i[:, gt, 1:2], axis=0)), reads=ys_all, writes=["y2_%d" % r])
                P.dma("sp", lambda e: e.dma_start(out=ht[r][:], in_=h_scr[gt * 128:(gt + 1) * 128, :]), reads=[("h_scr", gt)], writes=["ht%d" % r])

            def c1(gt):
                r = gt % 3
                q_ = gt % 2
                P.op("act", lambda e: e.activation(out=za[q_][:], in_=ht[r][:], func=AF.Copy, scale=ALPHA), reads=["ht%d" % r], writes=["za%d" % q_])
                P.op("dve", lambda e: e.scalar_tensor_tensor(za[q_][:], y1[r][:], wts[:, gt, 0:1], za[q_][:], op0=ALU.mult, op1=ALU.add),
                     reads=["y1_%d" % r, "za%d" % q_], writes=["za%d" % q_])
                P.op("dve", lambda e: e.scalar_tensor_tensor(zc[q_][:], y2[r][:], wts[:, gt, 1:2], za[q_][:], op0=ALU.mult, op1=ALU.add),
                     reads=["y2_%d" % r, "za%d" % q_], writes=["zc%d" % q_])
                for hf in range(2):
                    P.op("dve", lambda e: e.bn_stats(stats2[q_][:, hf, :], zc[q_][:, hf * 512:(hf + 1) * 512]), reads=["zc%d" % q_], writes=[("stats2", q_, hf)])
                P.op("dve", lambda e: e.bn_aggr(mv2[q_][:, 0:2], stats2[q_][:].rearrange("p a b -> p (a b)")),
                     reads=[("stats2", q_, 0), ("stats2", q_, 1)], writes=[("mv2a", q_)])
                P.op("dve", lambda e: e.tensor_scalar(mv2[q_][:, 2:3], mv2[q_][:, 1:2], LN_EPS, None, op0=ALU.add), reads=[("mv2a", q_)], writes=[("mv2b", q_)])

            def c2(gt):
                q_ = gt % 2
                s_, t_ = gt // NT, gt % NT
                P.op("pool", lambda e: e.tensor_tensor(mv2[q_][:, 3:4], mv2[q_][:, 2:3], negh[:, 0:1], op=ALU.pow), reads=[("mv2b", q_), "negh"], writes=[("mv2c", q_)])
                P.op("dve", lambda e: e.scalar_tensor_tensor(nm2[:, q_:q_ + 1], mv2[q_][:, 0:1], -1.0, mv2[q_][:, 3:4], op0=ALU.mult, op1=ALU.mult),
                     reads=[("mv2a", q_), ("mv2c", q_)], writes=[("nm2", q_)])
                P.op("act", lambda e: e.activation(out=oo[q_][:], in_=zc[q_][:], func=AF.Identity, scale=mv2[q_][:, 3:4], bias=nm2[:, q_:q_ + 1]),
                     reads=["zc%d" % q_, ("mv2c", q_), ("nm2", q_)], writes=["oo%d" % q_])
                P.op("dve", lambda e: e.tensor_tensor(oo[q_][:], oo[q_][:], g2[:], op=ALU.mult), reads=["oo%d" % q_, "g2"], writes=["oo%d" % q_])
                P.op("dve", lambda e: e.tensor_tensor(oo[q_][:], oo[q_][:], b2[:], op=ALU.add), reads=["oo%d" % q_, "b2"], writes=["oo%d" % q_])
                P.dma("sp", lambda e: e.dma_start(out=out[s_, t_ * 128:(t_ + 1) * 128, :], in_=oo[q_][:]), reads=["oo%d" % q_], writes=[("out", gt)])

            pipeline(NTOT, [c0, c1, c2])
            P.barrier()
    return nc


def _in_maps(inputs):
    f = lambda a: np.ascontiguousarray(np.asarray(a))
    cmat, cvec = _consts()
    common = {
        "w_in": f(inputs["w_in"][0]),
        "gate_bias": f(inputs["gate_bias"][0].reshape(16, 128).T),
        "lam_vecs": f(inputs["lam_vecs"][0].reshape(1, 256)),
        "subln_gain": f(inputs["subln_gain"][0].reshape(1, 128)),
        "w_pool": f(inputs["w_pool"][0]),
        "pool_scale": f(inputs["pool_scale"][0].reshape(4, 128).T),
        "w_a": f(inputs["w_branch_a"][0]),
        "w_b": f(inputs["w_branch_b"][0]),
        "w_out": f(inputs["w_out"][0]),
        "ln1_g": f(inputs["ln1_gain"][0].reshape(1, D)),
        "ln1_b": f(inputs["ln1_bias"][0].reshape(1, D)),
        "w_rt": f(np.concatenate([inputs["w_router_group"][0], inputs["w_router_expert"][0]], axis=1)),
        "b_rt": f(np.concatenate([inputs["b_router_group"][0], inputs["b_router_expert"][0]]).reshape(1, 36)),
        "w_gu": f(inputs["w_gate_up"][0]),
        "w_dn": f(inputs["w_down"][0]),
        "ln2_g": f(inputs["ln2_gain"][0].reshape(1, D)),
        "ln2_b": f(inputs["ln2_bias"][0].reshape(1, D)),
        "cmat": cmat,
        "cvec": cvec,
    }
    maps = []
    xx = np.asarray(inputs["x"])
    pp = np.asarray(inputs["positions"]).astype(np.int32)
    for c in range(NCORES):
        m = dict(common)
        m["x"] = f(xx[c * NSEQ:(c + 1) * NSEQ])
        m["pos"] = f(pp[c * NSEQ:(c + 1) * NSEQ])
        maps.append(m)
    return maps


def kernel(**inputs):
    nc = build_nc()
    res = run_bass_kernel_spmd(nc, _in_maps(inputs), core_ids=list(range(NCORES)))
    return np.concatenate([np.asarray(r["out"]) for r in res.results], axis=0).astype(np.float32)
```

```python
import contextlib
import math
import numpy as np
import concourse.bass as bass
import concourse.mybir as mybir
from concourse.bass_utils import run_bass_kernel_spmd

F32 = mybir.dt.float32
BF16 = mybir.dt.bfloat16
I32 = mybir.dt.int32
AF = mybir.ActivationFunctionType
ALU = mybir.AluOpType
AX = mybir.AxisListType

NCORES = 8
NSEQ = 2
S = 2048
D = 1024
H = 8
NT = S // 128
NE = 32
CAP = 384
NBLK = CAP // 128
NSLOT = NE * CAP + 128
DUMP = NE * CAP
ALPHA = 2.0 ** 0.25
LAMBDA_INIT = 0.2
LN_EPS = 1e-5
RMS_EPS = 1e-5
IN_W = 5632
TWO_PI = 2.0 * math.pi
C1 = 6.28125
C2 = TWO_PI - C1

SEM_LIMIT = 30000
DMA_RING = 6
DMA_RING_Q = {"poolc": 2}
CONV_EVERY = 13


class Prog:
    def __init__(self, nc, stack):
        self.nc = nc
        self.stack = stack
        self.eng = {"pe": nc.tensor, "dve": nc.vector, "act": nc.scalar,
                    "pool": nc.gpsimd, "sp": nc.sync, "poolc": nc.gpsimd}
        self.cnt = {}
        self.sems = {}
        self.seen = {}
        self.res = {}
        self.dma_n = {}
        self.dma_ring = {}
        self.dma_ring_cnt = {}
        self.ninst = 0
        self.recording = None
        for s in ["pe", "dve", "act", "pool"]:
            self.cnt[s] = 0
            self.sems[s] = []
            self._new_sem(s)

    def _alloc_sem(self, name):
        return self.stack.enter_context(self.nc.semaphore(name))

    def _new_sem(self, s):
        sem = self._alloc_sem("s_%s_%d" % (s, len(self.sems[s])))
        self.sems[s].append((sem, self.cnt[s]))

    def _wait(self, engname, tok):
        stream, idx = tok
        key = (engname, stream)
        if self.seen.get(key, 0) >= idx:
            return
        if stream.startswith("dma"):
            self.seen[key] = idx
            q, r = stream.split(":")[1:]
            self.eng[engname].wait_ge(self.dma_ring[q][int(r)], 16 * idx)
            self.ninst += 1
            return
        if stream == engname and engname == "pe":
            return
        self.seen[key] = idx
        for sem, base in reversed(self.sems[stream]):
            if idx > base:
                self.eng[engname].wait_ge(sem, idx - base)
                self.ninst += 1
                return
        raise RuntimeError("bad token")

    def _deps(self, reads, writes):
        deps = []
        for r in reads:
            st = self.res.get(r)
            if st and st[0] is not None:
                deps.append(st[0])
        for w in writes:
            st = self.res.get(w)
            if st:
                if st[0] is not None:
                    deps.append(st[0])
                deps.extend(st[1])
        return deps

    def _commit(self, tok, reads, writes):
        for r in reads:
            st = self.res.setdefault(r, [None, []])
            st[1].append(tok)
        for w in writes:
            self.res[w] = [tok, []]

    def _excl(self, reads, writes):
        r2 = [r for r in reads if not (isinstance(r, str) and r.startswith("B") and r[1:].isdigit())]
        w2 = list(writes) + [r for r in reads if (isinstance(r, str) and r.startswith("B") and r[1:].isdigit())]
        return r2, w2

    def op(self, engname, fn, reads=(), writes=()):
        if self.recording is not None:
            rec = _Rec()
            fn(rec)
            self.recording.append(("op", engname, rec.call, tuple(reads), tuple(writes)))
            return None
        reads, writes = self._excl(reads, writes)
        for d in self._deps(reads, writes):
            self._wait(engname, d)
        inst = fn(self.eng[engname])
        if self.cnt[engname] - self.sems[engname][-1][1] >= SEM_LIMIT:
            self._new_sem(engname)
        sem, base = self.sems[engname][-1]
        inst.then_inc(sem, 1)
        self.cnt[engname] += 1
        self.ninst += 1
        tok = (engname, self.cnt[engname])
        self._commit(tok, reads, writes)
        return tok

    def dma(self, q, fn, reads=(), writes=()):
        if self.recording is not None:
            rec = _Rec()
            fn(rec)
            self.recording.append(("dma", q, rec.call, tuple(reads), tuple(writes)))
            return None
        nring = DMA_RING_Q.get(q, DMA_RING)
        if q not in self.dma_ring:
            self.dma_ring[q] = [self._alloc_sem("d_%s_%d" % (q, i)) for i in range(nring)]
            self.dma_ring_cnt[q] = [0] * nring
            self.dma_n[q] = 0
        r = self.dma_n[q] % nring
        self.dma_n[q] += 1
        stream = "dma:%s:%d" % (q, r)
        if self.dma_ring_cnt[q][r] > 0:
            self._wait(q, (stream, self.dma_ring_cnt[q][r]))
        for d in self._deps(reads, writes):
            self._wait(q, d)
        inst = fn(self.eng[q])
        inst.then_inc(self.dma_ring[q][r], 16)
        self.dma_ring_cnt[q][r] += 1
        self.ninst += 1
        tok = (stream, self.dma_ring_cnt[q][r])
        self._commit(tok, reads, writes)
        return tok

    def record(self, fn, *args):
        assert self.recording is None
        self.recording = []
        try:
            fn(*args)
            return self.recording
        finally:
            self.recording = None

    def replay(self, item):
        kind, eng, call, reads, writes = item
        name, a, kw = call
        f = lambda e: getattr(e, name)(*a, **kw)
        if kind == "op":
            return self.op(eng, f, reads, writes)
        return self.dma(eng, f, reads, writes)

    def barrier(self):
        toks = []
        for s in ["pe", "dve", "act", "pool"]:
            if self.cnt[s] > 0:
                toks.append((s, self.cnt[s]))
        for q in self.dma_ring:
            for r in range(len(self.dma_ring[q])):
                if self.dma_ring_cnt[q][r] > 0:
                    toks.append(("dma:%s:%d" % (q, r), self.dma_ring_cnt[q][r]))
        for e in ["pe", "dve", "act", "pool", "sp"]:
            for t in toks:
                if e == "pe" and t[0] == "pe":
                    continue
                self._wait(e, t)
        self.res = {}


class _Rec:
    def __init__(self):
        self.call = None

    def __getattr__(self, name):
        def f(*a, **kw):
            self.call = (name, a, kw)
            return self
        return f


def _consts():
    ident = np.eye(128, dtype=np.float32)
    prot = np.zeros((128, 128), np.float32)
    for pp in range(128):
        if (pp % 64) < 32:
            prot[pp + 32, pp] = -1.0
        else:
            prot[pp - 32, pp] = 1.0
    tri = np.triu(np.ones((128, 128), np.float32), 1)
    ones = np.ones((128, 128), np.float32)
    cmat = np.stack([ident, prot, tri, ones], axis=1)
    half = 32
    inv_freq = (10000.0 ** (-np.arange(half, dtype=np.float32) * np.float32(2.0 / 64))).astype(np.float32)
    cvec = np.zeros((128, 128), np.float32)
    cvec[:, 0] = inv_freq[np.arange(128) % 32]
    for g, w in enumerate((2, 4, 8, 16)):
        for t in range(16):
            cvec[:, 16 + g * 16 + t] = (w / (t + 1.0)) if t < w - 1 else 1.0
    cvec[:, 80:112] = (np.arange(NE, dtype=np.float32) * CAP - DUMP)[None, :]
    cvec[64:, 112] = -30000.0
    return np.ascontiguousarray(cmat), cvec


def build_nc(stop=None):
    nc = bass.Bass("TRN2", target_bir_lowering=False)

    def dram(name, shape, dt, kind="ExternalInput"):
        return nc.dram_tensor(name, shape, dt, kind=kind).ap()

    x = dram("x", [NSEQ, S, D], F32)
    pos = dram("pos", [NSEQ, S], I32)
    w_in = dram("w_in", [D, IN_W], F32)
    gate_bias = dram("gate_bias", [128, 16], F32)
    lam_vecs = dram("lam_vecs", [1, 256], F32)
    subln_gain = dram("subln_gain", [1, 128], F32)
    w_pool = dram("w_pool", [4, 128, 128], F32)
    pool_scale = dram("pool_scale", [128, 4], F32)
    w_a = dram("w_a", [D, D], F32)
    w_b = dram("w_b", [512, D], F32)
    w_out = dram("w_out", [D, D], F32)
    ln1_g = dram("ln1_g", [1, D], F32)
    ln1_b = dram("ln1_b", [1, D], F32)
    w_rt = dram("w_rt", [D, 36], F32)
    b_rt = dram("b_rt", [1, 36], F32)
    w_gu = dram("w_gu", [NE, D, D], F32)
    w_dn = dram("w_dn", [NE, 512, D], F32)
    ln2_g = dram("ln2_g", [1, D], F32)
    ln2_b = dram("ln2_b", [1, D], F32)
    cmat = dram("cmat", [128, 4, 128], F32)
    cvec = dram("cvec", [128, 128], F32)
    out = dram("out", [NSEQ, S, D], F32, kind="ExternalOutput")
    h_scr = dram("h_scr", [NSEQ * S, D], F32, kind="Internal")
    xs = dram("xs", [NSLOT, D], BF16, kind="Internal")
    ys = dram("ys", [NSLOT, D], F32, kind="Internal")
    wgu_bf = dram("wgu_bf", [NE, D, D], BF16, kind="Internal")
    wdn_bf = dram("wdn_bf", [NE, 512, D], BF16, kind="Internal")
    dbg = {}
    if stop in ("attn", "a1", "rope", "v", "qk"):
        dbg["oT"] = dram("dbg_oT", [NSEQ, 128, H, S], BF16, kind="ExternalOutput")
    if stop == "mix":
        dbg["mergedT"] = dram("dbg_mergedT", [NSEQ, 128, 8, S], BF16, kind="ExternalOutput")
    if stop in ("ln1",):
        dbg["h"] = dram("dbg_h", [NSEQ * S, D], F32, kind="ExternalOutput")
    if stop in ("ln1", "moe"):
        dbg["slots"] = dram("dbg_slots", [128, 32, 2], I32, kind="ExternalOutput")
        dbg["wts"] = dram("dbg_wts", [128, 32, 2], F32, kind="ExternalOutput")
    if stop == "moe":
        dbg["ys"] = dram("dbg_ys", [NSLOT, D], F32, kind="ExternalOutput")

    w_in_k = w_in.rearrange("(kc k) n -> k kc n", k=128)
    w_a_k = w_a.rearrange("(kc k) n -> k kc n", k=128)
    w_b_k = w_b.rearrange("(kc k) n -> k kc n", k=128)
    w_out_k = w_out.rearrange("(kc k) n -> k kc n", k=128)
    w_rt_k = w_rt.rearrange("(kc k) n -> k kc n", k=128)

    with contextlib.ExitStack() as st:
        P = Prog(nc, st)

        uniq = [0]

        def sb(stack, name, shape, dt):
            uniq[0] += 1
            return stack.enter_context(nc.sbuf_tensor("%s_u%d" % (name, uniq[0]), shape, dt))

        B = [st.enter_context(nc.psum_tensor("bank%d" % i, [128, 512], F32)) for i in range(8)]
        BN = ["B%d" % i for i in range(8)]

        def bank_bf(i):
            return B[i][:].bitcast(BF16)

        cm_f = sb(st, "cm_f", [128, 4, 128], F32)
        cm_b = sb(st, "cm_b", [128, 4, 128], BF16)
        cv = sb(st, "cv", [128, 128], F32)
        lvb = sb(st, "lvb", [128, 256], F32)
        gainb = sb(st, "gainb", [128, 128], F32)
        gbias = sb(st, "gbias", [128, 16], F32)
        pscale = sb(st, "pscale", [128, 4], F32)
        wpool_b = sb(st, "wpool_b", [128, 4, 128], BF16)
        wrt = sb(st, "wrt", [128, 8, 36], F32)
        brt = sb(st, "brt", [128, 36], F32)
        small = sb(st, "small", [128, 16], F32)
        negh = sb(st, "negh", [128, 4], F32)
        cnt = sb(st, "cnt", [128, 32], F32)
        slots_f = sb(st, "slots_f", [128, 32, 2], F32)
        slots_i = sb(st, "slots_i", [128, 32, 2], I32)
        wts = sb(st, "wts", [128, 32, 2], F32)
        zrow = sb(st, "zrow", [128, 1024], F32)
        zb3 = sb(st, "zb3", [128, 1, 1024], BF16)
        xT = sb(st, "xT", [128, 8, S], BF16)
        oT = sb(st, "oT", [128, 8, S], BF16)

        ident_f = cm_f[:, 0, :]
        ident_b = cm_b[:, 0, :]
        prot_b = cm_b[:, 1, :]
        tri_b = cm_b[:, 2, :]
        ones_b = cm_b[:, 3, :]
        invf = cv[:, 0:1]
        eoffmd = cv[:, 80:112]
        lamneg = small[:, 0:1]

        P.dma("sp", lambda e: e.dma_start(out=cm_f[:], in_=cmat[:, :, :]), writes=["cm_f"])
        P.dma("pool", lambda e: e.dma_start(out=cm_b[:], in_=cmat[:, :, :]), writes=["cm_b"])
        P.dma("sp", lambda e: e.dma_start(out=cv[:], in_=cvec[:, :]), writes=["cv"])
        P.dma("sp", lambda e: e.dma_start(out=lvb[:], in_=lam_vecs[0:1, :].to_broadcast([128, 256])), writes=["lvb"])
        P.dma("sp", lambda e: e.dma_start(out=gainb[:], in_=subln_gain[0:1, :].to_broadcast([128, 128])), writes=["gainb"])
        P.dma("sp", lambda e: e.dma_start(out=brt[:], in_=b_rt[0:1, :].to_broadcast([128, 36])), writes=["brt"])
        P.dma("sp", lambda e: e.dma_start(out=gbias[:], in_=gate_bias[:, :]), writes=["gbias"])
        P.dma("sp", lambda e: e.dma_start(out=pscale[:], in_=pool_scale[:, :]), writes=["pscale"])
        P.dma("pool", lambda e: e.dma_start(out=wpool_b[:], in_=w_pool.rearrange("g c d -> c g d")), writes=["wpool_b"])
        P.dma("sp", lambda e: e.dma_start(out=wrt[:], in_=w_rt_k), writes=["wrt"])
        P.op("dve", lambda e: e.memset(cnt[:], 0.0), writes=["cnt"])
        P.op("dve", lambda e: e.memset(slots_i[:], 0), writes=["slots_i_init"])
        P.op("dve", lambda e: e.memset(wts[:], 0.0), writes=["wts_init"])
        P.op("dve", lambda e: e.memset(zrow[:], 0.0), writes=["zrow"])
        P.op("dve", lambda e: e.memset(negh[:], -0.5), writes=["negh"])
        P.op("dve", lambda e: e.tensor_scalar(gainb[:], gainb[:], 1.0 - LAMBDA_INIT, None, op0=ALU.mult),
             reads=["gainb"], writes=["gainb"])
        P.op("dve", lambda e: e.memset(zb3[:], 0.0), writes=["zb3"])
        xs_v = xs.rearrange("(n p) d -> p n d", p=128)
        n_tot = NSLOT // 128
        xs_init = [("xs_init", n0) for n0 in range(0, n_tot, 25)]

        def emit_xs_zero_fill():
            for n0 in range(0, n_tot, 25):
                n1 = min(n_tot, n0 + 25)
                P.dma("sp", lambda e: e.dma_start(out=xs_v[:, n0:n1, :], in_=zb3[:].to_broadcast([128, n1 - n0, 1024])),
                      reads=["zb3"], writes=[("xs_init", n0)])

        P.dma("sp", lambda e: e.dma_start(out=ys[DUMP:DUMP + 128, :], in_=zrow[:]), reads=["zrow"], writes=["ys_dump"])
        P.op("dve", lambda e: e.tensor_tensor(lvb[:, 0:64], lvb[:, 0:64], lvb[:, 64:128], op=ALU.mult),
             reads=["lvb"], writes=["lvb"])
        P.op("dve", lambda e: e.tensor_tensor(lvb[:, 128:192], lvb[:, 128:192], lvb[:, 192:256], op=ALU.mult),
             reads=["lvb"], writes=["lvb"])
        P.op("dve", lambda e: e.reduce_sum(small[:, 1:2], lvb[:, 0:64], axis=AX.X), reads=["lvb"], writes=["small"])
        P.op("dve", lambda e: e.reduce_sum(small[:, 2:3], lvb[:, 128:192], axis=AX.X), reads=["lvb", "small"], writes=["small"])
        P.op("act", lambda e: e.activation(out=small[:, 3:5], in_=small[:, 1:3], func=AF.Exp), reads=["small"], writes=["small"])
        P.op("dve", lambda e: e.tensor_tensor(small[:, 5:6], small[:, 4:5], small[:, 3:4], op=ALU.subtract),
             reads=["small"], writes=["small"])
        P.op("dve", lambda e: e.tensor_scalar(small[:, 0:1], small[:, 5:6], -LAMBDA_INIT, None, op0=ALU.add),
             reads=["small"], writes=["small"])

        ev_flip = [0]

        def evac(out_ap, in_ap, reads, writes, eng=None):
            if eng is None:
                eng = "act" if ev_flip[0] % 2 == 0 else "dve"
                ev_flip[0] += 1
            if eng == "act":
                return P.op("act", lambda e: e.activation(out=out_ap, in_=in_ap, func=AF.Copy), reads=reads, writes=writes)
            return P.op("dve", lambda e: e.tensor_copy(out_ap, in_ap), reads=reads, writes=writes)

        def pipeline(n_items, stages):
            for step in range(n_items + len(stages) - 1):
                lists = []
                for si, f in enumerate(stages):
                    k = step - si
                    if 0 <= k < n_items:
                        lists.append(P.record(f, k))
                pos = [0] * len(lists)
                left = sum(len(l) for l in lists)
                while left:
                    for li, l in enumerate(lists):
                        if pos[li] < len(l):
                            P.replay(l[pos[li]])
                            pos[li] += 1
                            left -= 1

        conv_jobs = []
        for e_ in range(NE):
            conv_jobs.append((wgu_bf[e_, 0:512, :], w_gu[e_, 0:512, :], ("wcv", e_, 0)))
            conv_jobs.append((wgu_bf[e_, 512:1024, :], w_gu[e_, 512:1024, :], ("wcv", e_, 1)))
            conv_jobs.append((wdn_bf[e_, :, :], w_dn[e_, :, :], ("wcv", e_, 2)))
        conv_next = [0]
        conv_tick = [0]

        def conv_issue(n=1):
            for _ in range(n):
                if conv_next[0] < len(conv_jobs):
                    o_, i_, nm_ = conv_jobs[conv_next[0]]
                    conv_next[0] += 1
                    P.dma("poolc", lambda e: e.dma_start(out=o_, in_=i_), writes=[nm_])

        for s in range(NSEQ):
            s2 = contextlib.ExitStack()
            with s2:
                V = sb(s2, "V", [128, NT, H, 129], BF16)
                cosT = sb(s2, "cosT", [128, S], F32)
                sinT = sb(s2, "sinT", [128, S], F32)
                with contextlib.ExitStack() as s2a:
                    xt = [sb(s2a, "xt%d" % i, [128, D], F32) for i in range(2)]
                    posi = sb(s2a, "posi", [128, S], I32)
                    ang = sb(s2a, "ang", [128, S], F32)
                    ras = [sb(s2a, "ra%d" % i, [128, S], F32) for i in range(2)]
                    rk = sb(s2a, "rk", [128, S], F32)
                    ki = sb(s2a, "ki", [128, S], I32)
                    wv = sb(s2a, "wv", [128, 8, D], BF16)
                    for kc2 in range(2):
                        P.dma("pool", lambda e: e.dma_start(out=wv[:, kc2 * 4:(kc2 + 1) * 4, :],
                                                            in_=w_in_k[:, kc2 * 4:(kc2 + 1) * 4, 2048:3072]),
                              writes=[("wv", kc2)])
                    P.dma("sp", lambda e: e.dma_start(out=posi[:], in_=pos[s:s + 1, :].to_broadcast([128, S])), writes=["posi"])
                    P.op("pool", lambda e: e.memset(V[:, :, :, 128:129], 1.0), writes=["Vones"])
                    P.op("dve", lambda e: e.tensor_copy(ang[:], posi[:]), reads=["posi"], writes=["ang"])
                    P.op("dve", lambda e: e.tensor_scalar(ang[:], ang[:], invf, None, op0=ALU.mult), reads=["ang", "cv"], writes=["ang"])
                    for which in range(2):
                        ra, ran = ras[which], "ra%d" % which
                        shift = 0.0 if which == 0 else 0.5 * math.pi
                        P.op("dve", lambda e: e.tensor_scalar(ra[:], ang[:], shift, None, op0=ALU.add), reads=["ang"], writes=[ran])
                        P.op("dve", lambda e: e.tensor_scalar(rk[:], ra[:], 1.0 / TWO_PI, None, op0=ALU.mult), reads=[ran], writes=["rk"])
                        P.op("dve", lambda e: e.tensor_copy(ki[:], rk[:]), reads=["rk"], writes=["ki"])
                        P.op("dve", lambda e: e.tensor_copy(rk[:], ki[:]), reads=["ki"], writes=["rk"])
                        P.op("dve", lambda e: e.scalar_tensor_tensor(ra[:], rk[:], -C1, ra[:], op0=ALU.mult, op1=ALU.add),
                             reads=["rk", ran], writes=[ran])
                        P.op("dve", lambda e: e.scalar_tensor_tensor(ra[:], rk[:], -C2, ra[:], op0=ALU.mult, op1=ALU.add),
                             reads=["rk", ran], writes=[ran])
                        P.op("dve", lambda e: e.tensor_scalar(rk[:], ra[:], math.pi, -TWO_PI, op0=ALU.is_gt, op1=ALU.mult),
                             reads=[ran], writes=["rk"])
                        P.op("dve", lambda e: e.tensor_tensor(ra[:], ra[:], rk[:], op=ALU.add), reads=[ran, "rk"], writes=[ran])
                        P.op("dve", lambda e: e.tensor_scalar(rk[:], ra[:], -math.pi, TWO_PI, op0=ALU.is_lt, op1=ALU.mult),
                             reads=[ran], writes=["rk"])
                        P.op("dve", lambda e: e.tensor_tensor(ra[:], ra[:], rk[:], op=ALU.add), reads=[ran, "rk"], writes=[ran])
                        P.op("dve", lambda e: e.tensor_scalar(ra[:], ra[:], 3.1415925, -3.1415925, op0=ALU.min, op1=ALU.max),
                             reads=[ran], writes=[ran])
                    for t in range(NT):
                        xb_ = xt[t % 2]
                        xn = "xt%d" % (t % 2)
                        P.dma("sp", lambda e: e.dma_start(out=xb_[:], in_=x[s, t * 128:(t + 1) * 128, :]), writes=[xn])
                        for hf in range(2):
                            bk = (t * 2 + hf) % 4
                            for j in range(4):
                                kc = hf * 4 + j
                                P.op("pe", lambda e: e.transpose(B[bk][:, j * 128:(j + 1) * 128], xb_[:, kc * 128:(kc + 1) * 128], ident_f),
                                     reads=[xn, "cm_f"], writes=[BN[bk]])
                            evac(xT[:, hf * 4:(hf + 1) * 4, t * 128:(t + 1) * 128],
                                 B[bk][:].rearrange("p (a b) -> p a b", a=4), reads=[BN[bk]], writes=[("xT", t)], eng="act")
                    P.op("act", lambda e: e.activation(out=sinT[:], in_=ras[0][:], func=AF.Sin), reads=["ra0"], writes=["sinT"])
                    P.op("act", lambda e: e.activation(out=cosT[:], in_=ras[1][:], func=AF.Sin), reads=["ra1"], writes=["cosT"])
                    if stop == "a1":
                        P.dma("sp", lambda e: e.dma_start(out=dbg["oT"][s], in_=xT[:]), reads=[("xT", t_) for t_ in range(NT)], writes=["dbg"])
                    for t in range(NT):
                        for hf in range(2):
                            bk = 4 + (t * 2 + hf) % 4
                            for kc in range(8):
                                P.op("pe", lambda e: e.matmul(B[bk][:], lhsT=xT[:, kc, t * 128:(t + 1) * 128],
                                                              rhs=wv[:, kc, hf * 512:(hf + 1) * 512],
                                                              start=(kc == 0), stop=(kc == 7)),
                                     reads=[("xT", t), ("wv", kc // 4)], writes=[BN[bk]])
                            evac(V[:, t, hf * 4:(hf + 1) * 4, 0:128], B[bk][:].rearrange("p (a b) -> p a b", a=4),
                                 reads=[BN[bk]], writes=[("V", t)])
                    P.barrier()
                if s == 0:
                    emit_xs_zero_fill()
                if stop in ("a1", "rope", "v"):
                    P.barrier()
                    continue
                with contextlib.ExitStack() as s2c:
                    wq = [sb(s2c, "wq%d" % i, [128, 8, 128], BF16) for i in range(2)]
                    wk = [sb(s2c, "wk%d" % i, [128, 8, 128], BF16) for i in range(2)]
                    qT = [sb(s2c, "qT%d" % i, [128, S], BF16) for i in range(2)]
                    kz = [[sb(s2c, "kz%d_%d" % (i, m), [128, S], BF16) for m in range(2)] for i in range(2)]
                    qb = [sb(s2c, "qb%d" % i, [128, 512], BF16) for i in range(2)]
                    t1 = [sb(s2c, "t1_%d" % i, [128, 512], F32) for i in range(2)]
                    t2 = [sb(s2c, "t2_%d" % i, [128, 512], F32) for i in range(2)]
                    PT = [sb(s2c, "PT%d" % i, [128, 512], BF16) for i in range(4)]
                    accs = [sb(s2c, "accs%d" % i, [128, 1032], F32) for i in range(2)]
                    rec3 = sb(s2c, "rec3", [128, 4, 2, 1], F32)
                    O4 = sb(s2c, "O4", [128, 4, 128], F32)
                    T4 = sb(s2c, "T4", [128, 4, 128], F32)
                    ss = sb(s2c, "ss", [128, 4], F32)
                    ssv = sb(s2c, "ssv", [128, 4], F32)
                    rstd3 = sb(s2c, "rstd3", [128, 4, 1], F32)
                    onb = [sb(s2c, "onb%d" % i, [128, 4, 128], BF16) for i in range(2)]
                    rope_i = [0]
                    NG = S // 512
                    for i_ in range(2):
                        P.op("pool", lambda e: e.memset(kz[i_][0][64:128, :], 0.0), writes=[("kzz", i_, 0)])
                        P.op("pool", lambda e: e.memset(kz[i_][1][0:64, :], 0.0), writes=[("kzz", i_, 1)])

                    def load_head_w(h):
                        b = h % 2
                        P.dma("pool", lambda e: e.dma_start(out=wq[b][:], in_=w_in_k[:, :, h * 128:(h + 1) * 128]),
                              writes=["wq%d" % b])
                        P.dma("pool", lambda e: e.dma_start(out=wk[b][:], in_=w_in_k[:, :, 1024 + h * 128:1024 + (h + 1) * 128]),
                              writes=["wk%d" % b])

                    def proj_units(h):
                        hb = h % 2
                        units = []
                        for which in range(2):
                            w_t = (wq if which == 0 else wk)[hb]
                            wname = ("wq%d" if which == 0 else "wk%d") % hb
                            for tg in range(NG):
                                st_ = {}

                                def part_a(w_t=w_t, wname=wname, tg=tg, st_=st_):
                                    i = rope_i[0]
                                    rope_i[0] += 1
                                    st_["i"] = i
                                    r = i % 2
                                    pb = 3 if i % 2 == 0 else 7
                                    tsl = slice(tg * 512, (tg + 1) * 512)
                                    for kc in range(8):
                                        P.op("pe", lambda e: e.matmul(B[pb][:], lhsT=w_t[:, kc, :], rhs=xT[:, kc, tsl],
                                                                      start=(kc == 0), stop=(kc == 7)),
                                             reads=[wname], writes=[BN[pb]])
                                    P.op("act", lambda e: e.activation(out=qb[r][:], in_=B[pb][:], func=AF.Copy),
                                         reads=[BN[pb]], writes=["qb%d" % r])
                                    P.op("dve", lambda e: e.tensor_tensor(t1[r][:], B[pb][:], cosT[:, tsl], op=ALU.mult),
                                         reads=[BN[pb], "cosT"], writes=["t1_%d" % r])

                                def part_b(which=which, tg=tg, st_=st_, hb=hb):
                                    i = st_["i"]
                                    r = i % 2
                                    pb = 3 if i % 2 == 0 else 7
                                    tsl = slice(tg * 512, (tg + 1) * 512)
                                    P.op("pe", lambda e: e.matmul(B[pb][:], lhsT=prot_b, rhs=qb[r][:], start=True, stop=True),
                                         reads=["qb%d" % r, "cm_b"], writes=[BN[pb]])
                                    P.op("dve", lambda e: e.tensor_tensor(t2[r][:], B[pb][:], sinT[:, tsl], op=ALU.mult),
                                         reads=[BN[pb], "sinT"], writes=["t2_%d" % r])
                                    if which == 0:
                                        P.op("dve", lambda e: e.tensor_tensor(qT[hb][:, tsl], t1[r][:], t2[r][:], op=ALU.add),
                                             reads=["t1_%d" % r, "t2_%d" % r], writes=[("qT%d" % hb, tg)])
                                    else:
                                        for m in range(2):
                                            ps_ = slice(m * 64, (m + 1) * 64)
                                            P.op("dve", lambda e: e.tensor_tensor(kz[hb][m][ps_, tsl], t1[r][ps_, :], t2[r][ps_, :], op=ALU.add),
                                                 reads=["t1_%d" % r, "t2_%d" % r, ("kzz", hb, m)], writes=[("kz%d_%d" % (hb, m), tg)])
                                units.append(part_a)
                                units.append(part_b)
                        return units

                    LOOK = 2
                    gstep = [0]
                    deferred = []

                    def run_due(force=False):
                        keep = []
                        for due, fn in deferred:
                            if force or due <= gstep[0]:
                                fn()
                            else:
                                keep.append((due, fn))
                        deferred[:] = keep

                    def defer(delay, fn):
                        deferred.append((gstep[0] + delay, fn))

                    grp_i = [0]
                    load_head_w(0)
                    for u in proj_units(0):
                        u()
                    for h in range(H):
                        hb = h % 2
                        if h + 1 < H:
                            load_head_w(h + 1)
                            nxt = proj_units(h + 1)
                        else:
                            nxt = []
                        qn = "qT%d" % hb
                        steps = []
                        for g in range(NG):
                            for j in range(4 * g + 4):
                                for m in range(2):
                                    steps.append((g, j, m))
                        nsteps = len(steps)
                        unit_at = {}
                        if nxt:
                            gap = max(1, (nsteps - 8) // len(nxt))
                            for ui, u in enumerate(nxt):
                                unit_at.setdefault(4 + ui * gap, []).append(u)
                        started = {}
                        pv_pending = []

                        def emit_pv(g, j, m, pt, ptn, q0):
                            for il in range(q0 - 4 * g, 4):
                                a = il * 2 + m
                                ab = 4 + a // 3
                                col = (a % 3) * 129
                                c0 = (il - (q0 - 4 * g)) * 128
                                first = (g, ab) not in started
                                started[(g, ab)] = True
                                P.op("pe", lambda e: e.matmul(B[ab][:, col:col + 129], lhsT=pt[:, c0:c0 + 128],
                                                              rhs=V[:, j, h, :], start=first, stop=(j == 4 * g + il),
                                                              skip_group_check=True),
                                     reads=[ptn, (ptn, "m"), ("V", j), "Vones"], writes=[BN[ab]])
                            if j == 4 * g + 3 and m == 1:
                                emit_group_end(g)

                        def emit_group_end(g, h=h):
                            gi = grp_i[0]
                            grp_i[0] += 1
                            ac = accs[gi % 2]
                            acn = "accs%d" % (gi % 2)
                            evac(ac[:, 0:387], B[4][:, 0:387], reads=[BN[4]], writes=[(acn, 0)], eng="dve")
                            evac(ac[:, 387:774], B[5][:, 0:387], reads=[BN[5]], writes=[(acn, 1)], eng="act")
                            evac(ac[:, 774:1032], B[6][:, 0:258], reads=[BN[6]], writes=[(acn, 2)], eng="dve")
                            acv = ac[:, :].rearrange("p (i m c) -> p i m c", i=4, m=2)
                            acr = [(acn, 0), (acn, 1), (acn, 2)]

                            def norm_1():
                                P.op("dve", lambda e: e.reciprocal(rec3[:, :, :, 0], acv[:, :, :, 128]), reads=acr, writes=["rec"])
                                P.op("dve", lambda e: e.tensor_scalar(rec3[:, :, 1, 0], rec3[:, :, 1, 0], lamneg, None, op0=ALU.mult),
                                     reads=["rec", "small"], writes=["rec"])
                                P.op("dve", lambda e: e.tensor_tensor(O4[:], acv[:, :, 0, 0:128], rec3[:, :, 0, :].to_broadcast([128, 4, 128]), op=ALU.mult),
                                     reads=acr + ["rec"], writes=["O4"])
                                P.op("dve", lambda e: e.tensor_tensor(T4[:], acv[:, :, 1, 0:128], rec3[:, :, 1, :].to_broadcast([128, 4, 128]), op=ALU.mult),
                                     reads=acr + ["rec"], writes=["T4"])

                            def norm_2():
                                P.op("dve", lambda e: e.tensor_tensor(O4[:], O4[:], T4[:], op=ALU.add), reads=["O4", "T4"], writes=["O4"])
                                P.op("dve", lambda e: e.tensor_tensor(T4[:], O4[:], O4[:], op=ALU.mult), reads=["O4"], writes=["T4"])
                                P.op("dve", lambda e: e.reduce_sum(ss[:], T4[:], axis=AX.X), reads=["T4"], writes=["ss"])
                                P.op("dve", lambda e: e.tensor_scalar(ssv[:], ss[:], 1.0 / 128.0, RMS_EPS, op0=ALU.mult, op1=ALU.add),
                                     reads=["ss"], writes=["ssv"])
                                P.op("pool", lambda e: e.tensor_tensor(rstd3[:, :, 0], ssv[:], negh[:], op=ALU.pow), reads=["ssv", "negh"], writes=["rstd"])

                            def norm_on():
                                ob = onb[gi % 2]
                                obn = "onb%d" % (gi % 2)
                                P.op("dve", lambda e: e.tensor_tensor(O4[:], O4[:], rstd3[:].to_broadcast([128, 4, 128]), op=ALU.mult),
                                     reads=["O4", "rstd"], writes=["O4"])
                                P.op("dve", lambda e: e.tensor_tensor(ob[:], O4[:], gainb[:, None, :].to_broadcast([128, 4, 128]), op=ALU.mult),
                                     reads=["O4", "gainb"], writes=[obn])

                            def norm_b():
                                ob = onb[gi % 2]
                                obn = "onb%d" % (gi % 2)
                                for il in range(4):
                                    P.op("pe", lambda e: e.transpose(bank_bf(7)[:, il * 128:(il + 1) * 128], ob[:, il, :], ident_b),
                                         reads=[obn, "cm_b"], writes=[BN[7]])
                                evac(oT[:, h, g * 512:(g + 1) * 512], bank_bf(7)[:, 0:512], reads=[BN[7]], writes=[("oT", g)])

                            defer(1, norm_1)
                            defer(3, norm_2)
                            defer(6, norm_on)
                            defer(11, norm_b)

                        for si, (g, j, m) in enumerate(steps):
                            gstep[0] += 1
                            run_due()
                            conv_tick[0] += 1
                            if conv_tick[0] % CONV_EVERY == 0:
                                conv_issue()
                            for u in unit_at.get(si, []):
                                u()
                            q0 = max(j, 4 * g)
                            N = (4 * g + 4 - q0) * 128
                            qsl = slice(q0 * 128, (4 * g + 4) * 128)
                            i = gstep[0]
                            bk = i % 3
                            pt = PT[i % 4]
                            ptn = "PT%d" % (i % 4)
                            P.op("pe", lambda e: e.matmul(B[bk][:, 0:N], lhsT=kz[hb][m][:, j * 128:(j + 1) * 128],
                                                          rhs=qT[hb][:, qsl], start=True, stop=True),
                                 reads=[("kz%d_%d" % (hb, m), j // 4), ("kzz", hb, m), (qn, g)], writes=[BN[bk]])
                            if j >= 4 * g:
                                P.op("act", lambda e: e.activation(out=pt[:, 0:64], in_=B[bk][:, 0:64], func=AF.Exp, scale=0.125, bias=cv[:, 112:113]),
                                     reads=[BN[bk], "cv"], writes=[(ptn, "m")])
                                P.op("act", lambda e: e.activation(out=pt[:, 64:N], in_=B[bk][:, 64:N], func=AF.Exp, scale=0.125),
                                     reads=[BN[bk]], writes=[ptn])
                            else:
                                P.op("act", lambda e: e.activation(out=pt[:, 0:N], in_=B[bk][:, 0:N], func=AF.Exp, scale=0.125),
                                     reads=[BN[bk]], writes=[ptn, (ptn, "m")])
                            pv_pending.append((g, j, m, pt, ptn, q0))
                            if len(pv_pending) > LOOK:
                                emit_pv(*pv_pending.pop(0))
                        while pv_pending:
                            emit_pv(*pv_pending.pop(0))
                        for si_ in sorted(unit_at):
                            if si_ >= nsteps:
                                for u in unit_at[si_]:
                                    u()
                    run_due(force=True)
                    if s == NSEQ - 1:
                        conv_issue(len(conv_jobs))
                    P.barrier()
            if stop in ("attn", "qk"):
                P.dma("sp", lambda e: e.dma_start(out=dbg["oT"][s], in_=oT[:]), reads=[], writes=["dbg"])
                P.barrier()
                continue
            with contextlib.ExitStack() as s3:
                mergedT = sb(s3, "mergedT", [128, 8, S], BF16)
                mixedT = sb(s3, "mixedT", [128, 4, S], BF16)
                NG = S // 512
                with contextlib.ExitStack() as s3a:
                    wu = sb(s3a, "wu", [128, 8, 512], BF16)
                    uT = sb(s3a, "uT", [128, 4, S], F32)
                    Tp = [sb(s3a, "Tp%d" % i, [128, S], F32) for i in range(2)]
                    pooledT = sb(s3a, "pooledT", [128, 4, S], BF16)
                    for kc2 in range(2):
                        P.dma("pool", lambda e: e.dma_start(out=wu[:, kc2 * 4:(kc2 + 1) * 4, :],
                                                            in_=w_in_k[:, kc2 * 4:(kc2 + 1) * 4, 3072:3584]),
                              writes=[("wu", kc2)])
                    bi = 0
                    for g in range(4):
                        for tg in range(NG):
                            bk = bi % 4
                            bi += 1
                            tsl = slice(tg * 512, (tg + 1) * 512)
                            for kc in range(8):
                                P.op("pe", lambda e: e.matmul(B[bk][:], lhsT=wu[:, kc, g * 128:(g + 1) * 128], rhs=xT[:, kc, tsl],
                                                              start=(kc == 0), stop=(kc == 7)),
                                     reads=[("wu", kc // 4)], writes=[BN[bk]])
                            evac(uT[:, g, tsl], B[bk][:], reads=[BN[bk]], writes=[("uT", g)])
                    for g, w in enumerate((2, 4, 8, 16)):
                        cur, cur_r = uT[:, g, :], [("uT", g)]
                        k = 0
                        sh = 1
                        while sh < w:
                            dst, dstn = Tp[k % 2], "Tp%d" % (k % 2)
                            eng = "dve"
                            P.op(eng, lambda e: e.tensor_tensor(dst[:, sh:S], cur[:, sh:S], cur[:, 0:S - sh], op=ALU.add),
                                 reads=cur_r, writes=[dstn])
                            P.op(eng, lambda e: e.tensor_copy(dst[:, 0:sh], cur[:, 0:sh]), reads=cur_r, writes=[dstn + "h"])
                            cur, cur_r = dst[:, :], [dstn, dstn + "h"]
                            k += 1
                            sh *= 2
                        P.op("dve", lambda e: e.tensor_tensor(cur[:, 0:16], cur[:, 0:16], cv[:, 16 + g * 16:32 + g * 16], op=ALU.mult),
                             reads=cur_r + ["cv"], writes=cur_r)
                        P.op("dve", lambda e: e.scalar_tensor_tensor(pooledT[:, g, :], cur, 1.0 / w, uT[:, g, :], op0=ALU.mult, op1=ALU.subtract),
                             reads=cur_r + [("uT", g)], writes=[("pooledT", g)])
                    for g in range(4):
                        for tg in range(NG):
                            bk = bi % 4
                            bi += 1
                            tsl = slice(tg * 512, (tg + 1) * 512)
                            P.op("pe", lambda e: e.matmul(B[bk][:], lhsT=wpool_b[:, g, :], rhs=pooledT[:, g, tsl], start=True, stop=True),
                                 reads=[("pooledT", g), "wpool_b"], writes=[BN[bk]])
                            P.op("dve", lambda e: e.tensor_scalar(mixedT[:, g, tsl], B[bk][:], pscale[:, g:g + 1], None, op0=ALU.mult),
                                 reads=[BN[bk], "pscale"], writes=[("mixedT", g)])
                    P.barrier()
                with contextlib.ExitStack() as s3b:
                    wa_c = [sb(s3b, "wa_c%d" % i, [128, 8, 128], BF16) for i in range(2)]
                    wb_c = [sb(s3b, "wb_c%d" % i, [128, 4, 128], BF16) for i in range(2)]
                    wga_c = [sb(s3b, "wga_c%d" % i, [128, 8, 128], BF16) for i in range(2)]
                    wgb_c = [sb(s3b, "wgb_c%d" % i, [128, 8, 128], BF16) for i in range(2)]
                    ga = [sb(s3b, "ga%d" % i, [128, 512], F32) for i in range(2)]
                    gb = [sb(s3b, "gb%d" % i, [128, 512], F32) for i in range(2)]
                    m1 = [sb(s3b, "m1_%d" % i, [128, 512], F32) for i in range(2)]
                    m2 = [sb(s3b, "m2_%d" % i, [128, 512], F32) for i in range(2)]

                    def load_chunk_w(c):
                        b = c % 2
                        cs = slice(c * 128, (c + 1) * 128)
                        P.dma("pool", lambda e: e.dma_start(out=wa_c[b][:], in_=w_a_k[:, :, cs]), writes=["wa_c%d" % b])
                        P.dma("pool", lambda e: e.dma_start(out=wb_c[b][:], in_=w_b_k[:, :, cs]), writes=["wb_c%d" % b])
                        P.dma("pool", lambda e: e.dma_start(out=wga_c[b][:], in_=w_in_k[:, :, 3584 + c * 128:3584 + (c + 1) * 128]),
                              writes=["wga_c%d" % b])
                        P.dma("pool", lambda e: e.dma_start(out=wgb_c[b][:], in_=w_in_k[:, :, 4608 + c * 128:4608 + (c + 1) * 128]),
                              writes=["wgb_c%d" % b])

                    load_chunk_w(0)
                    it = 0
                    for c in range(8):
                        cb = c % 2
                        if c + 1 < 8:
                            load_chunk_w(c + 1)
                        for tg in range(NG):
                            r = it % 2
                            it += 1
                            b0 = r * 4
                            tsl = slice(tg * 512, (tg + 1) * 512)
                            for hh_ in range(8):
                                P.op("pe", lambda e: e.matmul(B[b0][:], lhsT=wa_c[cb][:, hh_, :], rhs=oT[:, hh_, tsl],
                                                              start=(hh_ == 0), stop=(hh_ == 7)),
                                     reads=["wa_c%d" % cb], writes=[BN[b0]])
                            for g in range(4):
                                P.op("pe", lambda e: e.matmul(B[b0 + 1][:], lhsT=wb_c[cb][:, g, :], rhs=mixedT[:, g, tsl],
                                                              start=(g == 0), stop=(g == 3)),
                                     reads=["wb_c%d" % cb], writes=[BN[b0 + 1]])
                            for kc in range(8):
                                P.op("pe", lambda e: e.matmul(B[b0 + 2][:], lhsT=wga_c[cb][:, kc, :], rhs=xT[:, kc, tsl],
                                                              start=(kc == 0), stop=(kc == 7)),
                                     reads=["wga_c%d" % cb], writes=[BN[b0 + 2]])
                            for kc in range(8):
                                P.op("pe", lambda e: e.matmul(B[b0 + 3][:], lhsT=wgb_c[cb][:, kc, :], rhs=xT[:, kc, tsl],
                                                              start=(kc == 0), stop=(kc == 7)),
                                     reads=["wgb_c%d" % cb], writes=[BN[b0 + 3]])
                            P.op("act", lambda e: e.activation(out=ga[r][:], in_=B[b0 + 2][:], func=AF.Sigmoid, bias=gbias[:, c:c + 1]),
                                 reads=[BN[b0 + 2], "gbias"], writes=["ga%d" % r])
                            P.op("act", lambda e: e.activation(out=gb[r][:], in_=B[b0 + 3][:], func=AF.Sigmoid, bias=gbias[:, 8 + c:9 + c]),
                                 reads=[BN[b0 + 3], "gbias"], writes=["gb%d" % r])
                            P.op("dve", lambda e: e.tensor_tensor(m1[r][:], B[b0][:], ga[r][:], op=ALU.mult),
                                 reads=[BN[b0], "ga%d" % r], writes=["m1_%d" % r])
                            P.op("dve", lambda e: e.tensor_tensor(m2[r][:], B[b0 + 1][:], gb[r][:], op=ALU.mult),
                                 reads=[BN[b0 + 1], "gb%d" % r], writes=["m2_%d" % r])
                            P.op("pool", lambda e: e.tensor_tensor(mergedT[:, c, tsl], m1[r][:], m2[r][:], op=ALU.add),
                                 reads=["m1_%d" % r, "m2_%d" % r], writes=[("mergedT", c)])
                    P.barrier()
                if stop == "mix":
                    P.dma("sp", lambda e: e.dma_start(out=dbg["mergedT"][s], in_=mergedT[:]), reads=[], writes=["dbg"])
                    P.barrier()
                    continue
                with contextlib.ExitStack() as s3c:
                    wout = sb(s3c, "wout", [128, 8, D], BF16)
                    g1 = sb(s3c, "g1", [128, D], F32)
                    b1 = sb(s3c, "b1", [128, D], F32)
                    xt = [sb(s3c, "xt%d" % i, [128, D], F32) for i in range(2)]
                    zs = [sb(s3c, "z%d" % i, [128, D], F32) for i in range(3)]
                    hh = [sb(s3c, "hh%d" % i, [128, D], F32) for i in range(3)]
                    hbf = [sb(s3c, "hbf%d" % i, [128, D], BF16) for i in range(4)]
                    hTs = [sb(s3c, "hT%d" % i, [128, 8, 128], F32) for i in range(2)]
                    statss = [sb(s3c, "stats%d" % i, [128, 2, 6], F32) for i in range(3)]
                    mvs = [sb(s3c, "mv%d" % i, [128, 4], F32) for i in range(3)]
                    rrs = [sb(s3c, "rr%d" % i, [128, 16], F32) for i in range(2)]
                    nm = sb(s3c, "nm", [128, 3], F32)
                    lg = sb(s3c, "lg", [128, 36], F32)
                    ge = sb(s3c, "ge", [128, 4], F32)
                    gone = sb(s3c, "gone", [128, 4], F32)
                    mneg = sb(s3c, "mneg", [128, 4, 1], F32)
                    elms = [sb(s3c, "elm%d" % i, [128, 4, 8], F32) for i in range(2)]
                    elm2 = sb(s3c, "elm2", [128, 32], F32)
                    oh1s = [sb(s3c, "oh1_%d" % i, [128, 32], F32) for i in range(3)]
                    oh2s = [sb(s3c, "oh2_%d" % i, [128, 32], F32) for i in range(2)]
                    Mbs = [sb(s3c, "Mb%d" % i, [128, 32], BF16) for i in range(2)]
                    posin = sb(s3c, "posin", [128, 32], F32)
                    valid = sb(s3c, "valid", [128, 32], F32)
                    smv = sb(s3c, "smv", [128, 32], F32)
                    tmp32 = sb(s3c, "tmp32", [128, 32], F32)
                    for kc2 in range(2):
                        P.dma("pool", lambda e: e.dma_start(out=wout[:, kc2 * 4:(kc2 + 1) * 4, :], in_=w_out_k[:, kc2 * 4:(kc2 + 1) * 4, :]),
                              writes=[("wout", kc2)])
                    P.dma("sp", lambda e: e.dma_start(out=g1[:], in_=ln1_g[0:1, :].to_broadcast([128, D])), writes=["g1"])
                    P.dma("sp", lambda e: e.dma_start(out=b1[:], in_=ln1_b[0:1, :].to_broadcast([128, D])), writes=["b1"])

                    def L0a(t):
                        r = t % 2
                        xb_, xn = xt[r], "xt%d" % r
                        P.dma("sp", lambda e: e.dma_start(out=xb_[:], in_=x[s, t * 128:(t + 1) * 128, :]), writes=[xn])
                        for hf in range(2):
                            bk = (0, 1)[hf] if r == 0 else (6, 7)[hf]
                            for kc in range(8):
                                P.op("pe", lambda e: e.matmul(B[bk][:], lhsT=mergedT[:, kc, t * 128:(t + 1) * 128],
                                                              rhs=wout[:, kc, hf * 512:(hf + 1) * 512], start=(kc == 0), stop=(kc == 7)),
                                     reads=[("wout", kc // 4)], writes=[BN[bk]])

                    def L0b(t):
                        r = t % 2
                        q3 = t % 3
                        xb_, xn = xt[r], "xt%d" % r
                        z, stats, mv = zs[q3], statss[q3], mvs[q3]
                        zn = "z%d" % q3
                        for hf in range(2):
                            bk = (0, 1)[hf] if r == 0 else (6, 7)[hf]
                            P.op("dve", lambda e: e.scalar_tensor_tensor(z[:, hf * 512:(hf + 1) * 512], xb_[:, hf * 512:(hf + 1) * 512], ALPHA, B[bk][:],
                                                                         op0=ALU.mult, op1=ALU.add),
                                 reads=[xn, BN[bk]], writes=[(zn, hf)])
                            P.op("dve", lambda e: e.bn_stats(stats[:, hf, :], z[:, hf * 512:(hf + 1) * 512]), reads=[(zn, hf)], writes=[("stats", q3, hf)])
                        P.op("dve", lambda e: e.bn_aggr(mv[:, 0:2], stats[:].rearrange("p a b -> p (a b)")), reads=[("stats", q3, 0), ("stats", q3, 1)], writes=[("mv", q3)])
                        P.op("dve", lambda e: e.tensor_scalar(mv[:, 2:3], mv[:, 1:2], LN_EPS, None, op0=ALU.add), reads=[("mv", q3)], writes=[("mv2", q3)])

                    def L1a(t):
                        q3 = t % 3
                        z, mv = zs[q3], mvs[q3]
                        zn = "z%d" % q3
                        hb_, hn = hh[q3], "hh%d" % q3
                        P.op("pool", lambda e: e.tensor_tensor(mv[:, 3:4], mv[:, 2:3], negh[:, 0:1], op=ALU.pow), reads=[("mv2", q3), "negh"], writes=[("mv3", q3)])
                        P.op("dve", lambda e: e.scalar_tensor_tensor(nm[:, q3:q3 + 1], mv[:, 0:1], -1.0, mv[:, 3:4], op0=ALU.mult, op1=ALU.mult),
                             reads=[("mv", q3), ("mv3", q3)], writes=[("nm", q3)])
                        P.op("act", lambda e: e.activation(out=hb_[:], in_=z[:], func=AF.Identity, scale=mv[:, 3:4], bias=nm[:, q3:q3 + 1]),
                             reads=[(zn, 0), (zn, 1), ("mv3", q3), ("nm", q3)], writes=[hn])

                    def L1a2(t):
                        q3 = t % 3
                        hb_, hn = hh[q3], "hh%d" % q3
                        P.op("dve", lambda e: e.tensor_tensor(hb_[:], hb_[:], g1[:], op=ALU.mult), reads=[hn, "g1"], writes=[hn])
                        P.op("dve", lambda e: e.tensor_tensor(hb_[:], hb_[:], b1[:], op=ALU.add), reads=[hn, "b1"], writes=[hn])

                    def L1b(t):
                        gt = s * NT + t
                        r = t % 2
                        r3 = t % 3
                        hb_, hn = hh[r3], "hh%d" % r3
                        P.dma("sp", lambda e: e.dma_start(out=h_scr[gt * 128:(gt + 1) * 128, :], in_=hb_[:]), reads=[hn], writes=[("h_scr", gt)])
                        if "h" in dbg:
                            P.dma("sp", lambda e: e.dma_start(out=dbg["h"][gt * 128:(gt + 1) * 128, :], in_=hb_[:]), reads=[hn], writes=[("dbg_h", gt)])
                        P.op("act", lambda e: e.activation(out=hbf[t % 4][:], in_=hb_[:], func=AF.Copy), reads=[hn], writes=["hbf%d" % (t % 4)])
                        for hf in range(2):
                            bk = 2 + hf
                            for j in range(4):
                                kc = hf * 4 + j
                                P.op("pe", lambda e: e.transpose(B[bk][:, j * 128:(j + 1) * 128], hb_[:, kc * 128:(kc + 1) * 128], ident_f),
                                     reads=[hn, "cm_f"], writes=[BN[bk]])
                            evac(hTs[r][:, hf * 4:(hf + 1) * 4, :], B[bk][:].rearrange("p (a b) -> p a b", a=4), reads=[BN[bk]], writes=[("hT", r, hf)], eng="act")

                    def L2a1(t):
                        gt = s * NT + t
                        r = t % 2
                        hT = hTs[r]
                        rr = rrs[r]
                        elm = elms[r]
                        elm_f = elm[:].rearrange("p a b -> p (a b)")
                        oh1 = oh1s[t % 3]
                        o1n = "oh1_%d" % (t % 3)
                        for kc in range(8):
                            P.op("pe", lambda e: e.matmul(B[4][:, 0:36], lhsT=hT[:, kc, :], rhs=wrt[:, kc, :], start=(kc == 0), stop=(kc == 7)),
                                 reads=[("hT", r, kc // 4), "wrt"], writes=[BN[4]])
                        P.op("dve", lambda e: e.tensor_tensor(lg[:], B[4][:, 0:36], brt[:], op=ALU.add), reads=[BN[4], "brt"], writes=["lg"])
                        P.op("dve", lambda e: e.reduce_max(rr[:, 0:1], lg[:, 0:4], axis=AX.X), reads=["lg"], writes=[("rr0", r)])
                        P.op("dve", lambda e: e.tensor_scalar(rr[:, 1:2], rr[:, 0:1], -1.0, None, op0=ALU.mult), reads=[("rr0", r)], writes=[("rr1", r)])
                        P.op("act", lambda e: e.activation(out=ge[:], in_=lg[:, 0:4], func=AF.Exp, bias=rr[:, 1:2]), reads=["lg", ("rr1", r)], writes=["ge"])
                        P.op("dve", lambda e: e.reduce_sum(rr[:, 2:3], ge[:], axis=AX.X), reads=["ge"], writes=[("rr2", r)])
                        P.op("dve", lambda e: e.reciprocal(rr[:, 3:4], rr[:, 2:3]), reads=[("rr2", r)], writes=[("rr3", r)])
                        P.op("dve", lambda e: e.tensor_scalar(gone[:], lg[:, 0:4], rr[:, 0:1], None, op0=ALU.is_equal), reads=["lg", ("rr0", r)], writes=["gone"])
                        P.op("dve", lambda e: e.tensor_scalar(mneg[:, :, 0], gone[:], -1.0, 1e30, op0=ALU.add, op1=ALU.mult), reads=["gone"], writes=["mneg"])
                        P.op("dve", lambda e: e.tensor_tensor(elm[:], lg[:, 4:36].rearrange("p (a b) -> p a b", a=4), mneg[:].to_broadcast([128, 4, 8]), op=ALU.add),
                             reads=["lg", "mneg"], writes=[("elm", r)])
                        P.op("dve", lambda e: e.reduce_max(rr[:, 4:5], elm_f, axis=AX.X), reads=[("elm", r)], writes=[("rr4", r)])
                        P.op("dve", lambda e: e.tensor_scalar(oh1[:], elm_f, rr[:, 4:5], None, op0=ALU.is_equal), reads=[("elm", r), ("rr4", r)], writes=[o1n])

                    def L2a2(t):
                        gt = s * NT + t
                        r = t % 2
                        rr = rrs[r]
                        elm = elms[r]
                        elm_f = elm[:].rearrange("p a b -> p (a b)")
                        oh1, oh2, Mb = oh1s[t % 3], oh2s[r], Mbs[r]
                        o1n, o2n, mbn = "oh1_%d" % (t % 3), "oh2_%d" % r, "Mb%d" % r
                        P.op("dve", lambda e: e.scalar_tensor_tensor(elm2[:], oh1[:], -1e30, elm_f, op0=ALU.mult, op1=ALU.add), reads=[o1n, ("elm", r)], writes=["elm2"])
                        P.op("dve", lambda e: e.reduce_max(rr[:, 5:6], elm2[:], axis=AX.X), reads=["elm2"], writes=[("rr5", r)])
                        P.op("dve", lambda e: e.tensor_scalar(oh2[:], elm2[:], rr[:, 5:6], None, op0=ALU.is_equal), reads=["elm2", ("rr5", r)], writes=[o2n])
                        P.op("dve", lambda e: e.tensor_tensor(rr[:, 6:7], rr[:, 5:6], rr[:, 4:5], op=ALU.subtract), reads=[("rr4", r), ("rr5", r)], writes=[("rr6", r)])
                        P.op("act", lambda e: e.activation(out=rr[:, 7:8], in_=rr[:, 6:7], func=AF.Exp), reads=[("rr6", r)], writes=[("rr7", r)])
                        P.op("dve", lambda e: e.tensor_scalar(rr[:, 8:9], rr[:, 7:8], 1.0, None, op0=ALU.add), reads=[("rr7", r)], writes=[("rr8", r)])
                        P.op("dve", lambda e: e.reciprocal(rr[:, 9:10], rr[:, 8:9]), reads=[("rr8", r)], writes=[("rr9", r)])
                        P.op("dve", lambda e: e.tensor_tensor(wts[:, gt, 0:1], rr[:, 9:10], rr[:, 3:4], op=ALU.mult), reads=[("rr9", r), ("rr3", r)], writes=[("wts", gt, 0)])
                        P.op("dve", lambda e: e.tensor_tensor(rr[:, 10:11], rr[:, 7:8], rr[:, 9:10], op=ALU.mult), reads=[("rr7", r), ("rr9", r)], writes=[("rr10", r)])
                        P.op("dve", lambda e: e.tensor_tensor(wts[:, gt, 1:2], rr[:, 10:11], rr[:, 3:4], op=ALU.mult), reads=[("rr10", r), ("rr3", r)], writes=[("wts", gt, 1)])
                        P.op("dve", lambda e: e.tensor_tensor(Mb[:], oh1[:], oh2[:], op=ALU.add), reads=[o1n, o2n], writes=[mbn])

                    def L2b(t):
                        gt = s * NT + t
                        r = t % 2
                        r3 = t % 3
                        oh1, oh2, Mb = oh1s[t % 3], oh2s[r], Mbs[r]
                        o1n, o2n, mbn = "oh1_%d" % (t % 3), "oh2_%d" % r, "Mb%d" % r
                        r4 = t % 4
                        P.op("pe", lambda e: e.matmul(B[5][:, 0:32], lhsT=tri_b, rhs=Mb[:], start=True, stop=True, skip_group_check=True),
                             reads=[mbn, "cm_b"], writes=[BN[5]])
                        P.op("pe", lambda e: e.matmul(B[5][:, 32:64], lhsT=ones_b, rhs=Mb[:], start=False, stop=True, skip_group_check=True),
                             reads=[mbn, "cm_b"], writes=[BN[5]])
                        P.op("dve", lambda e: e.tensor_tensor(posin[:], B[5][:, 0:32], cnt[:], op=ALU.add), reads=[BN[5], "cnt"], writes=["posin"])
                        P.op("dve", lambda e: e.tensor_scalar(valid[:], posin[:], CAP - 0.5, None, op0=ALU.is_lt), reads=["posin"], writes=["valid"])
                        P.op("dve", lambda e: e.tensor_tensor(smv[:], posin[:], eoffmd, op=ALU.add), reads=["posin", "cv"], writes=["smv"])
                        P.op("dve", lambda e: e.tensor_tensor(smv[:], smv[:], valid[:], op=ALU.mult), reads=["smv", "valid"], writes=["smv"])
                        for k_, ohk, ohn in ((0, oh1, o1n), (1, oh2, o2n)):
                            P.op("dve", lambda e: e.tensor_tensor(tmp32[:], smv[:], ohk[:], op=ALU.mult), reads=["smv", ohn], writes=["tmp32"])
                            P.op("dve", lambda e: e.reduce_sum(slots_f[:, gt, k_:k_ + 1], tmp32[:], axis=AX.X), reads=["tmp32"], writes=[("slots_f", gt, k_)])
                        P.op("dve", lambda e: e.tensor_tensor(cnt[:], cnt[:], B[5][:, 32:64], op=ALU.add), reads=["cnt", BN[5]], writes=["cnt"])
                        P.op("dve", lambda e: e.tensor_scalar(slots_f[:, gt, :], slots_f[:, gt, :], float(DUMP), None, op0=ALU.add),
                             reads=[("slots_f", gt, 0), ("slots_f", gt, 1)], writes=[("slots_f2", gt)])
                        P.op("dve", lambda e: e.tensor_copy(slots_i[:, gt, :], slots_f[:, gt, :]), reads=[("slots_f2", gt)], writes=[("slots_i", gt)])
                        for k_ in range(2):
                            P.dma("pool", lambda e: e.indirect_dma_start(
                                out=xs[:, :], out_offset=bass.IndirectOffsetOnAxis(ap=slots_i[:, gt, k_:k_ + 1], axis=0),
                                in_=hbf[r4][:], in_offset=None), reads=["hbf%d" % r4, ("slots_i", gt)] + xs_init, writes=[("xs", gt, k_)])

                    pipeline(NT, [L0a, L0b, L1a, L1a2, L1b, L2a1, L2a2, L2b])
                    P.barrier()
        NTOT = NSEQ * NT
        if stop in ("ln1",):
            P.dma("sp", lambda e: e.dma_start(out=dbg["slots"][:, :, :], in_=slots_i[:]), writes=["dbg1"])
            P.dma("sp", lambda e: e.dma_start(out=dbg["wts"][:, :, :], in_=wts[:]), writes=["dbg2"])
            P.barrier()
            return nc
        if stop is not None and stop not in ("moe",):
            P.barrier()
            return nc

        xs_all = [("xs", gt, k_) for gt in range(NTOT) for k_ in range(2)]

        with contextlib.ExitStack() as s4:
            NWB = 3
            wg = [sb(s4, "wg%d" % i, [128, 8, D], BF16) for i in range(NWB)]
            wd = [sb(s4, "wd%d" % i, [128, 4, D], BF16) for i in range(NWB)]
            xsb3 = [sb(s4, "xsb3_%d" % i, [128, NBLK, D], BF16) for i in range(2)]
            xsT3 = [sb(s4, "xsT3_%d" % i, [128, 8, CAP], BF16) for i in range(2)]
            sg = [sb(s4, "sg%d" % i, [128, CAP], F32) for i in range(2)]
            actT3 = [sb(s4, "actT3_%d" % i, [128, 4, CAP], BF16) for i in range(2)]
            yb = [sb(s4, "yb%d" % i, [128, D], F32) for i in range(2)]

            def load_expert(e_):
                b = e_ % NWB
                for kc2 in range(2):
                    P.dma("sp", lambda e: e.dma_start(out=wg[b][:, kc2 * 4:(kc2 + 1) * 4, :],
                                                      in_=wgu_bf[e_].rearrange("(kc k) n -> k kc n", k=128)[:, kc2 * 4:(kc2 + 1) * 4, :]),
                          reads=[("wcv", e_, kc2)], writes=[("wg%d" % b, kc2)])
                P.dma("sp", lambda e: e.dma_start(out=wd[b][:], in_=wdn_bf[e_].rearrange("(kc k) n -> k kc n", k=128)),
                      reads=[("wcv", e_, 2)], writes=["wd%d" % b])

            for e0 in range(min(NWB, NE)):
                load_expert(e0)
            def E0(e_):
                r = e_ % 2
                for blk in range(NBLK):
                    row0 = e_ * CAP + blk * 128
                    P.dma("sp", lambda e: e.dma_start(out=xsb3[r][:, blk, :], in_=xs[row0:row0 + 128, :]), reads=xs_all, writes=[("xsb3", r, blk)])

            def E1(e_):
                r = e_ % 2
                for blk in range(NBLK):
                    bt = (e_ * NBLK + blk) % 2
                    for kc in range(8):
                        P.op("pe", lambda e: e.transpose(bank_bf(bt)[:, kc * 128:(kc + 1) * 128], xsb3[r][:, blk, kc * 128:(kc + 1) * 128], ident_b),
                             reads=[("xsb3", r, blk), "cm_b"], writes=[BN[bt]])
                    evac(xsT3[r][:, :, blk * 128:(blk + 1) * 128], bank_bf(bt)[:, :].rearrange("p (a b) -> p a b", a=8),
                         reads=[BN[bt]], writes=[("xsT3", r, blk)])

            def E2(e_):
                r = e_ % 2
                eb = e_ % NWB
                xr = [("xsT3", r, blk) for blk in range(NBLK)]
                for fc in range(4):
                    bg, bu = ((2, 3), (4, 5))[fc % 2]
                    for col0, bk in ((fc * 128, bg), (512 + fc * 128, bu)):
                        for kc in range(8):
                            P.op("pe", lambda e: e.matmul(B[bk][:, 0:CAP], lhsT=wg[eb][:, kc, col0:col0 + 128], rhs=xsT3[r][:, kc, :],
                                                          start=(kc == 0), stop=(kc == 7)),
                                 reads=xr + [("wg%d" % eb, kc // 4)], writes=[BN[bk]])
                    q_ = fc % 2
                    P.op("act", lambda e: e.activation(out=sg[q_][:], in_=B[bg][:, 0:CAP], func=AF.Silu), reads=[BN[bg]], writes=["sg%d" % q_])
                    P.op("dve", lambda e: e.tensor_tensor(actT3[r][:, fc, :], B[bu][:, 0:CAP], sg[q_][:], op=ALU.mult),
                         reads=[BN[bu], "sg%d" % q_], writes=[("actT3", r, fc)])

            def E3(e_):
                r = e_ % 2
                eb = e_ % NWB
                ar = [("actT3", r, fc) for fc in range(4)]
                for blk in range(NBLK):
                    q_ = (e_ * NBLK + blk) % 2
                    row0 = e_ * CAP + blk * 128
                    for half, bk in ((0, 6), (1, 7)):
                        for fc in range(4):
                            P.op("pe", lambda e: e.matmul(B[bk][:], lhsT=actT3[r][:, fc, blk * 128:(blk + 1) * 128], rhs=wd[eb][:, fc, half * 512:(half + 1) * 512],
                                                          start=(fc == 0), stop=(fc == 3)),
                                 reads=ar + ["wd%d" % eb], writes=[BN[bk]])
                        evac(yb[q_][:, half * 512:(half + 1) * 512], B[bk][:], reads=[BN[bk]], writes=[("yb%d" % q_, half)])
                    P.dma("pool", lambda e: e.dma_start(out=ys[row0:row0 + 128, :], in_=yb[q_][:]),
                          reads=[("yb%d" % q_, 0), ("yb%d" % q_, 1)], writes=[("ys", e_, blk)])
                if e_ + NWB < NE:
                    load_expert(e_ + NWB)

            pipeline(NE, [E0, E1, E2, E3])
            P.barrier()
        ys_all = [("ys", e_, blk) for e_ in range(NE) for blk in range(NBLK)] + ["ys_dump"]
        if stop == "moe":
            P.dma("sp", lambda e: e.dma_start(out=dbg["ys"][:, :], in_=ys[:, :]), writes=["dbg1"])
            P.dma("sp", lambda e: e.dma_start(out=dbg["slots"][:, :, :], in_=slots_i[:]), writes=["dbg2"])
            P.dma("sp", lambda e: e.dma_start(out=dbg["wts"][:, :, :], in_=wts[:]), writes=["dbg3"])
            P.barrier()
            return nc

        with contextlib.ExitStack() as s5:
            g2 = sb(s5, "g2", [128, D], F32)
            b2 = sb(s5, "b2", [128, D], F32)
            y1 = [sb(s5, "y1_%d" % i, [128, D], F32) for i in range(3)]
            y2 = [sb(s5, "y2_%d" % i, [128, D], F32) for i in range(3)]
            ht = [sb(s5, "ht%d" % i, [128, D], F32) for i in range(3)]
            za = [sb(s5, "za%d" % i, [128, D], F32) for i in range(2)]
            zc = [sb(s5, "zc%d" % i, [128, D], F32) for i in range(2)]
            oo = [sb(s5, "oo%d" % i, [128, D], F32) for i in range(2)]
            stats2 = [sb(s5, "stats2_%d" % i, [128, 2, 6], F32) for i in range(2)]
            mv2 = [sb(s5, "mv2_%d" % i, [128, 4], F32) for i in range(2)]
            nm2 = sb(s5, "nm2", [128, 2], F32)
            P.dma("sp", lambda e: e.dma_start(out=g2[:], in_=ln2_g[0:1, :].to_broadcast([128, D])), writes=["g2"])
            P.dma("sp", lambda e: e.dma_start(out=b2[:], in_=ln2_b[0:1, :].to_broadcast([128, D])), writes=["b2"])

            def c0(gt):
                r = gt % 3
                P.dma("pool", lambda e: e.indirect_dma_start(
                    out=y1[r][:], out_offset=None, in_=ys[:, :],
                    in_offset=bass.IndirectOffsetOnAxis(ap=slots_i[:, gt, 0:1], axis=0)), reads=ys_all, writes=["y1_%d" % r])
                P.dma("pool", lambda e: e.indirect_dma_start(
                    out=y2[r][:], out_offset=None, in_=ys[:, :],
                    in_offset=bass.IndirectOffsetOnAxis(ap=slots_i[:, gt, 1:2], axis=0)), reads=ys_all, writes=["y2_%d" % r])
                P.dma("sp", lambda e: e.dma_start(out=ht[r][:], in_=h_scr[gt * 128:(gt + 1) * 128, :]), reads=[("h_scr", gt)], writes=["ht%d" % r])

            def c1(gt):
                r = gt % 3
                q_ = gt % 2
                P.op("act", lambda e: e.activation(out=za[q_][:], in_=ht[r][:], func=AF.Copy, scale=ALPHA), reads=["ht%d" % r], writes=["za%d" % q_])
                P.op("dve", lambda e: e.scalar_tensor_tensor(za[q_][:], y1[r][:], wts[:, gt, 0:1], za[q_][:], op0=ALU.mult, op1=ALU.add),
                     reads=["y1_%d" % r, "za%d" % q_], writes=["za%d" % q_])
                P.op("dve", lambda e: e.scalar_tensor_tensor(zc[q_][:], y2[r][:], wts[:, gt, 1:2], za[q_][:], op0=ALU.mult, op1=ALU.add),
                     reads=["y2_%d" % r, "za%d" % q_], writes=["zc%d" % q_])
                for hf in range(2):
                    P.op("dve", lambda e: e.bn_stats(stats2[q_][:, hf, :], zc[q_][:, hf * 512:(hf + 1) * 512]), reads=["zc%d" % q_], writes=[("stats2", q_, hf)])
                P.op("dve", lambda e: e.bn_aggr(mv2[q_][:, 0:2], stats2[q_][:].rearrange("p a b -> p (a b)")),
                     reads=[("stats2", q_, 0), ("stats2", q_, 1)], writes=[("mv2a", q_)])
                P.op("dve", lambda e: e.tensor_scalar(mv2[q_][:, 2:3], mv2[q_][:, 1:2], LN_EPS, None, op0=ALU.add), reads=[("mv2a", q_)], writes=[("mv2b", q_)])

            def c2(gt):
                q_ = gt % 2
                s_, t_ = gt // NT, gt % NT
                P.op("pool", lambda e: e.tensor_tensor(mv2[q_][:, 3:4], mv2[q_][:, 2:3], negh[:, 0:1], op=ALU.pow), reads=[("mv2b", q_), "negh"], writes=[("mv2c", q_)])
                P.op("dve", lambda e: e.scalar_tensor_tensor(nm2[:, q_:q_ + 1], mv2[q_][:, 0:1], -1.0, mv2[q_][:, 3:4], op0=ALU.mult, op1=ALU.mult),
                     reads=[("mv2a", q_), ("mv2c", q_)], writes=[("nm2", q_)])
                P.op("act", lambda e: e.activation(out=oo[q_][:], in_=zc[q_][:], func=AF.Identity, scale=mv2[q_][:, 3:4], bias=nm2[:, q_:q_ + 1]),
                     reads=["zc%d" % q_, ("mv2c", q_), ("nm2", q_)], writes=["oo%d" % q_])

            def c3(gt):
                q_ = gt % 2
                s_, t_ = gt // NT, gt % NT
                P.op("dve", lambda e: e.tensor_tensor(oo[q_][:], oo[q_][:], g2[:], op=ALU.mult), reads=["oo%d" % q_, "g2"], writes=["oo%d" % q_])
                P.op("dve", lambda e: e.tensor_tensor(oo[q_][:], oo[q_][:], b2[:], op=ALU.add), reads=["oo%d" % q_, "b2"], writes=["oo%d" % q_])
                P.dma("sp", lambda e: e.dma_start(out=out[s_, t_ * 128:(t_ + 1) * 128, :], in_=oo[q_][:]), reads=["oo%d" % q_], writes=[("out", gt)])

            pipeline(NTOT, [c0, c1, c2, c3])
            P.barrier()
    return nc


def _in_maps(inputs):
    f = lambda a: np.ascontiguousarray(np.asarray(a))
    cmat, cvec = _consts()
    common = {
        "w_in": f(inputs["w_in"][0]),
        "gate_bias": f(inputs["gate_bias"][0].reshape(16, 128).T),
        "lam_vecs": f(inputs["lam_vecs"][0].reshape(1, 256)),
        "subln_gain": f(inputs["subln_gain"][0].reshape(1, 128)),
        "w_pool": f(inputs["w_pool"][0]),
        "pool_scale": f(inputs["pool_scale"][0].reshape(4, 128).T),
        "w_a": f(inputs["w_branch_a"][0]),
        "w_b": f(inputs["w_branch_b"][0]),
        "w_out": f(inputs["w_out"][0]),
        "ln1_g": f(inputs["ln1_gain"][0].reshape(1, D)),
        "ln1_b": f(inputs["ln1_bias"][0].reshape(1, D)),
        "w_rt": f(np.concatenate([inputs["w_router_group"][0], inputs["w_router_expert"][0]], axis=1)),
        "b_rt": f(np.concatenate([inputs["b_router_group"][0], inputs["b_router_expert"][0]]).reshape(1, 36)),
        "w_gu": f(inputs["w_gate_up"][0]),
        "w_dn": f(inputs["w_down"][0]),
        "ln2_g": f(inputs["ln2_gain"][0].reshape(1, D)),
        "ln2_b": f(inputs["ln2_bias"][0].reshape(1, D)),
        "cmat": cmat,
        "cvec": cvec,
    }
    maps = []
    xx = np.asarray(inputs["x"])
    pp = np.asarray(inputs["positions"]).astype(np.int32)
    for c in range(NCORES):
        m = dict(common)
        m["x"] = f(xx[c * NSEQ:(c + 1) * NSEQ])
        m["pos"] = f(pp[c * NSEQ:(c + 1) * NSEQ])
        maps.append(m)
    return maps


def kernel(**inputs):
    nc = build_nc()
    res = run_bass_kernel_spmd(nc, _in_maps(inputs), core_ids=list(range(NCORES)))
    return np.concatenate([np.asarray(r["out"]) for r in res.results], axis=0).astype(np.float32)
```
